# Optimizing a Trainium2 kernel written in Bass

```python
import math
import jax
import jax.numpy as jnp
from jax import lax
import numpy as np

D_MODEL = 1024
BATCH = 32
SEQ = 2048
DEPTH = 4

HG_HEADS = 4
HG_DK = 128
HG_DV = 128
HG_F = HG_HEADS * HG_DK
HG_V = HG_HEADS * HG_DV
HG_CHUNK = 64
SB_HEADS = 8
SB_DH = 64
SB_W = SB_HEADS * SB_DH
SB_BLOCK = 128
PEER_HEADS = 8
PEER_NKEYS = 128
PEER_N = PEER_NKEYS * PEER_NKEYS
PEER_DKEY = 128
PEER_TOPK = 16
PEER_TOK_BLOCK = 16
DN_ALPHA = (2.0 * DEPTH) ** 0.25
DN_BETA = (8.0 * DEPTH) ** -0.25
LN_EPS = 1e-5
RMS_EPS = 1e-6
IN_SIZES = (HG_F, HG_F, HG_V, HG_V, SB_W, SB_W, SB_W, D_MODEL, D_MODEL)
IN_WIDTH = sum(IN_SIZES)
IN_SPLITS = tuple(int(s) for s in np.cumsum(IN_SIZES)[:-1])

kernel_name = "hybrid_hgrn2_stickbreak_peer_deepnorm"


def layer_norm(x, g, b):
    xf = x.astype(jnp.float32)
    mu = jnp.mean(xf, axis=-1, keepdims=True)
    var = jnp.mean(jnp.square(xf - mu), axis=-1, keepdims=True)
    return (xf - mu) * lax.rsqrt(var + LN_EPS) * g + b


def head_rms_norm(o, g):
    of = o.astype(jnp.float32)
    return of * lax.rsqrt(jnp.mean(jnp.square(of), axis=-1, keepdims=True) + RMS_EPS) * g


def hgrn2_chunkwise(q, f_logit, v, lb):
    B, S, H, DK = q.shape
    DV = v.shape[-1]
    C = HG_CHUNK
    NC = S // C
    q = q.astype(jnp.float32)
    z = f_logit.astype(jnp.float32)
    v = v.astype(jnp.float32)
    lb = lb.reshape(H, DK)
    log_f = jnp.log(lb + (1.0 - lb) * jax.nn.sigmoid(z))
    k = (1.0 - lb) * jax.nn.sigmoid(-z)

    def chunks(a):
        return a.reshape(B, NC, C, H, a.shape[-1]).transpose(1, 0, 3, 2, 4)

    qc, kc, vc, gc = chunks(q), chunks(k), chunks(v), chunks(log_f)
    bc = jnp.cumsum(gc, axis=3)
    tri = jnp.tril(jnp.ones((C, C), dtype=bool))[:, :, None]

    def step(state, inp):
        q_, k_, v_, b_ = inp
        b_end = b_[:, :, -1:, :]
        inter = jnp.einsum('bhtd,bhde->bhte', q_ * jnp.exp(b_), state)
        diff = b_[:, :, :, None, :] - b_[:, :, None, :, :]
        decay = jnp.where(tri, jnp.exp(jnp.where(tri, diff, 0.0)), 0.0)
        scores = jnp.einsum('bhtd,bhsd,bhtsd->bhts', q_, k_, decay)
        intra = jnp.einsum('bhts,bhse->bhte', scores, v_)
        new_state = (jnp.exp(b_end[:, :, 0, :])[..., None] * state
                     + jnp.einsum('bhsd,bhse->bhde', k_ * jnp.exp(b_end - b_), v_))
        return new_state, inter + intra

    state0 = jnp.zeros((B, H, DK, DV), jnp.float32)
    _, o = lax.scan(step, state0, (qc, kc, vc, bc))
    return o.transpose(1, 0, 3, 2, 4).reshape(B, S, H, DV)


def stick_breaking_attention(q, k, v):
    B, S, H, d = q.shape
    scale = 1.0 / math.sqrt(d)
    qh = q.astype(jnp.float32).transpose(0, 2, 1, 3)
    kh = k.astype(jnp.float32).transpose(0, 2, 1, 3)
    vh = v.astype(jnp.float32).transpose(0, 2, 1, 3)
    outs = []
    for blk in range(S // SB_BLOCK):
        t0 = blk * SB_BLOCK
        t1 = t0 + SB_BLOCK
        z = jnp.einsum('bhtd,bhsd->bhts', qh[:, :, t0:t1], kh[:, :, :t1]) * scale
        tpos = t0 + jnp.arange(SB_BLOCK)[:, None]
        spos = jnp.arange(t1)[None, :]
        causal = spos < tpos
        log_1mb = jnp.where(causal, jax.nn.log_sigmoid(-z), 0.0)
        after = lax.cumsum(log_1mb, axis=3, reverse=True) - log_1mb
        w = jnp.where(causal, jnp.exp(jax.nn.log_sigmoid(z) + after), 0.0)
        outs.append(jnp.einsum('bhts,bhsd->bhtd', w, vh[:, :, :t1]))
    o = jnp.concatenate(outs, axis=2)
    return o.transpose(0, 2, 1, 3)


def peer_ffn(h, w_pq, sub_keys, peer_u, peer_v):
    B, S, D = h.shape
    half = PEER_DKEY // 2
    q = (h @ w_pq).reshape(B, S, PEER_HEADS, 2, half).astype(jnp.float32)
    s1 = jnp.einsum('bshd,hnd->bshn', q[..., 0, :], sub_keys[0].astype(jnp.float32))
    s2 = jnp.einsum('bshd,hnd->bshn', q[..., 1, :], sub_keys[1].astype(jnp.float32))
    v1, i1 = lax.top_k(s1, PEER_TOPK)
    v2, i2 = lax.top_k(s2, PEER_TOPK)
    cand = (v1[..., :, None] + v2[..., None, :]).reshape(B, S, PEER_HEADS, PEER_TOPK * PEER_TOPK)
    cand_idx = (i1[..., :, None] * PEER_NKEYS + i2[..., None, :]).reshape(B, S, PEER_HEADS, PEER_TOPK * PEER_TOPK)
    top, pos = lax.top_k(cand, PEER_TOPK)
    idx = jnp.take_along_axis(cand_idx, pos, axis=-1)
    gates = jax.nn.softmax(top, axis=-1)

    T = PEER_TOK_BLOCK
    nb = S // T

    def blockify(a):
        return a.reshape((B, nb, T) + a.shape[2:]).swapaxes(0, 1)

    def expert_block(args):
        hb, ib, gb = args
        u = peer_u[ib]
        act = jax.nn.gelu(jnp.einsum('btd,bthkd->bthk', hb, u), approximate=False)
        return jnp.einsum('bthk,bthkd->btd', gb * act, peer_v[ib])

    out = lax.map(expert_block, (blockify(h), blockify(idx), blockify(gates)))
    return out.swapaxes(0, 1).reshape(B, S, D)


def setup_inputs(seed: int = 0) -> dict:
    key = jax.random.key(seed)
    ks = jax.random.split(key, 16)
    nrm = jax.random.normal
    f32 = jnp.float32
    D = D_MODEL
    return {
        "x": nrm(ks[0], (BATCH, SEQ, D), f32),
        "c": nrm(ks[1], (BATCH, D), f32),
        "w_ada": nrm(ks[2], (DEPTH, D, 6 * D), f32) * (0.5 * D ** -0.5),
        "b_ada": nrm(ks[3], (DEPTH, 6 * D), f32) * 0.01,
        "w_in": nrm(ks[4], (DEPTH, D, IN_WIDTH), f32) * D ** -0.5,
        "lb_logits": nrm(ks[5], (DEPTH, HG_F), f32) * 0.1,
        "hg_norm_g": 1.0 + 0.02 * nrm(ks[6], (DEPTH, HG_V), f32),
        "w_up_a": nrm(ks[7], (DEPTH, HG_V, D), f32) * HG_V ** -0.5,
        "w_up_b": nrm(ks[8], (DEPTH, SB_W, D), f32) * SB_W ** -0.5,
        "w_o": nrm(ks[9], (DEPTH, D, D), f32) * (DN_BETA * D ** -0.5),
        "w_pq": nrm(ks[10], (DEPTH, D, PEER_HEADS * PEER_DKEY), f32) * D ** -0.5,
        "sub_keys": nrm(ks[11], (DEPTH, 2, PEER_HEADS, PEER_NKEYS, PEER_DKEY // 2), f32) * (PEER_DKEY // 2) ** -0.5,
        "peer_u": nrm(ks[12], (DEPTH, PEER_N, D), f32) * D ** -0.5,
        "peer_v": nrm(ks[13], (DEPTH, PEER_N, D), f32) * DN_BETA,
        "ln_g": 1.0 + 0.02 * nrm(ks[14], (DEPTH, 2, D), f32),
        "ln_b": 0.02 * nrm(ks[15], (DEPTH, 2, D), f32),
    }


def reference(x, c, w_ada, b_ada, w_in, lb_logits, hg_norm_g, w_up_a, w_up_b, w_o,
              w_pq, sub_keys, peer_u, peer_v, ln_g, ln_b):
    B, S, _ = x.shape
    p = jax.nn.softmax(lb_logits.astype(jnp.float32), axis=0)
    lower_bounds = jnp.cumsum(p, axis=0) - p[0:1]
    cond = jax.nn.silu(c)
    for l in range(DEPTH):
        mod = cond @ w_ada[l] + b_ada[l]
        sh1, sc1, gt1, sh2, sc2, gt2 = jnp.split(mod, 6, axis=-1)

        hm = x * (1.0 + sc1[:, None]) + sh1[:, None]
        proj = hm @ w_in[l]
        qa, fa, ia, ga, qb, kb, vb, gate_a, gate_b = jnp.split(proj, IN_SPLITS, axis=-1)
        oa = hgrn2_chunkwise(qa.reshape(B, S, HG_HEADS, HG_DK),
                             fa.reshape(B, S, HG_HEADS, HG_DK),
                             ia.reshape(B, S, HG_HEADS, HG_DV),
                             lower_bounds[l])
        oa = head_rms_norm(oa, hg_norm_g[l].reshape(HG_HEADS, HG_DV)).reshape(B, S, HG_V) * jax.nn.silu(ga)
        ob = stick_breaking_attention(qb.reshape(B, S, SB_HEADS, SB_DH),
                                      kb.reshape(B, S, SB_HEADS, SB_DH),
                                      vb.reshape(B, S, SB_HEADS, SB_DH)).reshape(B, S, SB_W)
        merged = jax.nn.sigmoid(gate_a) * (oa @ w_up_a[l]) + jax.nn.sigmoid(gate_b) * (ob @ w_up_b[l])
        y = (merged @ w_o[l]) * gt1[:, None]
        x = layer_norm(DN_ALPHA * x + y, ln_g[l, 0], ln_b[l, 0])

        hf = x * (1.0 + sc2[:, None]) + sh2[:, None]
        y = peer_ffn(hf, w_pq[l], sub_keys[l], peer_u[l], peer_v[l]) * gt2[:, None]
        x = layer_norm(DN_ALPHA * x + y, ln_g[l, 1], ln_b[l, 1])
    return x
```

```python
from contextlib import ExitStack
import numpy as np
import concourse.bass as bass
import concourse.mybir as mybir
from concourse.bass_utils import run_bass_kernel_spmd

F32 = mybir.dt.float32
BF16 = mybir.dt.bfloat16
U32 = mybir.dt.uint32
I32 = mybir.dt.int32
U8 = mybir.dt.uint8
AF = mybir.ActivationFunctionType
ALU = mybir.AluOpType
AX = mybir.AxisListType

D = 1024
S_LEN = 2048
NCORES = 8
DEPTH = 4
BATCH = 32
ALPHA = (2.0 * DEPTH) ** 0.25
LN_EPS = 1e-5
RMS_EPS = 1e-6
IN_W = 5632
OFF_HG = 0
OFF_QK = OFF_HG + 4 * 4096
OFF_SV = OFF_QK + 4 * 2048
OFF_MG = OFF_SV + 4096
OFF_WO = OFF_MG + 8 * 3072
OFF_PQ = OFF_WO + 8 * 1024
NWS = OFF_PQ + 8192


class Buf:
    def __init__(self, S, name=""):
        self.name = name
        self.w = None
        self.r = {}
        S.bufs.append(self)


class Sched:
    def __init__(self, nc):
        self.nc = nc
        self.eng = {"pe": nc.tensor, "act": nc.scalar, "dve": nc.vector, "pool": nc.gpsimd, "sp": nc.sync}
        self.same = {"act", "dve", "pool"}
        self.es = ExitStack()
        self.semh = {}
        self.cnt = {}
        self.waited = {e: {} for e in self.eng}
        self.bufs = []
        self.n = 0
        self.pool = [self.es.enter_context(nc.semaphore("sp%d" % i)) for i in range(48)]
        self.npool = 0
        for e in self.eng:
            self._sem(e)

    def sb(self, name, shape, dt):
        return self.es.enter_context(self.nc.sbuf_tensor(name, list(shape), dt))

    def ps(self, name, shape, dt):
        return self.es.enter_context(self.nc.psum_tensor(name, list(shape), dt))

    def _sem(self, key):
        if key not in self.semh:
            self.semh[key] = self.pool[self.npool]
            self.npool += 1
            self.cnt[key] = 0
        return self.semh[key]

    def _waits(self, e, reads, writes):
        waits = {}

        def need(k, v):
            if waits.get(k, 0) < v:
                waits[k] = v
        for b in reads:
            if b.w is not None:
                need(*b.w)
        for b in writes:
            if b.w is not None:
                need(*b.w)
            for k, v in b.r.items():
                if k != e:
                    need(k, v)
        eng = self.eng[e]
        for k, v in waits.items():
            if k == e and e not in self.same:
                continue
            if self.waited[e].get(k, 0) >= v:
                continue
            self.waited[e][k] = v
            eng.wait_ge(self.semh[k], v)
            self.n += 1

    def op(self, e, fn, reads=(), writes=()):
        self._sem(e)
        self._waits(e, reads, writes)
        ins = fn(self.eng[e])
        ins.then_inc(self.semh[e], 1)
        self.cnt[e] += 1
        idx = self.cnt[e]
        for b in reads:
            b.r[e] = idx
        for b in writes:
            b.w = (e, idx)
            b.r = {}
        self.n += 1

    def dma(self, q, fn, key_buf, reads=(), writes=()):
        key = ("dma", id(key_buf))
        self._sem(key)
        self._sem(q)
        self._waits(q, reads, writes)
        ins = fn(self.eng[q])
        ins.then_inc(self.semh[key], 16)
        self.cnt[key] += 16
        val = self.cnt[key]
        for b in reads:
            b.r[key] = val
        for b in writes:
            b.w = (key, val)
            b.r = {}
        self.n += 1

    def sync_all(self):
        nc = self.nc
        for key, h in self.semh.items():
            if isinstance(key, tuple) and self.cnt[key] > 0:
                nc.sync.wait_ge(h, self.cnt[key])
        nc.all_engine_barrier()
        for h in self.pool:
            nc.gpsimd.sem_clear(h)
        nc.all_engine_barrier()
        for k in self.cnt:
            self.cnt[k] = 0
        for e in self.waited:
            self.waited[e] = {}
        for b in self.bufs:
            b.w = None
            b.r = {}


class MK:
    def __init__(self, nseq=4, nl=4, hw_loops=True, ntiles=8, dbg=None, stop_after=None):
        self.nseq, self.nl, self.hw, self.ntiles, self.dbg, self.stop_after = nseq, nl, hw_loops, ntiles, dbg, stop_after
        self.nc = bass.Bass("TRN2", target_bir_lowering=False)
        self.S = Sched(self.nc)
        self.build()

    def B(self, name=""):
        return Buf(self.S, name)

    def chain(self, out, pairs, r, w, first=True, last=True):
        def fn(e):
            n = len(pairs)
            ins = None
            for i, (a, b) in enumerate(pairs):
                ins = e.matmul(out, lhsT=a, rhs=b, start=(first and i == 0), stop=(last and i == n - 1))
            return ins
        self.S.op("pe", fn, r, w)

    def ACT(self, out, in_, func, r, w, bias=None, scale=None):
        kw = {}
        if bias is not None:
            kw["bias"] = bias
        if scale is not None:
            kw["scale"] = scale
        self.S.op("act", lambda e: e.activation(out=out, in_=in_, func=func, **kw), r, w)

    def TT(self, eng, out, in0, in1, op, r, w):
        self.S.op(eng, lambda e: e.tensor_tensor(out=out, in0=in0, in1=in1, op=op), r, w)

    def TS(self, eng, out, in0, s1, s2, op0, op1, r, w):
        if s2 is None:
            self.S.op(eng, lambda e: e.tensor_scalar(out=out, in0=in0, scalar1=s1, scalar2=None, op0=op0), r, w)
        else:
            self.S.op(eng, lambda e: e.tensor_scalar(out=out, in0=in0, scalar1=s1, scalar2=s2, op0=op0, op1=op1), r, w)

    def STT(self, eng, out, in0, scalar, in1, op0, op1, r, w):
        self.S.op(eng, lambda e: e.scalar_tensor_tensor(out=out, in0=in0, scalar=scalar, in1=in1, op0=op0, op1=op1), r, w)

    def CP(self, eng, out, in_, r, w):
        if eng == "act":
            self.S.op("act", lambda e: e.copy(out=out, in_=in_), r, w)
        else:
            self.S.op(eng, lambda e: e.tensor_copy(out=out, in_=in_), r, w)

    def MS(self, eng, ap, val, w):
        self.S.op(eng, lambda e: e.memset(ap, val), (), w)

    def LD(self, out, in_, key, r=(), w=(), q="sp"):
        self.S.dma(q, lambda e: e.dma_start(out=out, in_=in_), key, r, w)

    def cv(self, arena, off, shape, dt):
        nb = {F32: 4, BF16: 2, U32: 4, I32: 4}[dt]
        n = int(np.prod(shape[1:])) * nb
        ap = arena[0:shape[0], off:off + n].bitcast(dt)
        if len(shape) == 3:
            ap = ap.rearrange("p (a b) -> p a b", a=shape[1])
        elif len(shape) == 4:
            ap = ap.rearrange("p (a b c) -> p a b c", a=shape[1], b=shape[2])
        return ap

    def run_loop(self, n, body):
        if self.hw and n > 1:
            with self.nc.Fori(0, n) as i:
                self.S.sync_all()
                body(i)
            self.S.sync_all()
        else:
            for i in range(n):
                self.S.sync_all()
                body(i)
            self.S.sync_all()

    def build(self):
        nc, S, nseq, nl = self.nc, self.S, self.nseq, self.nl
        dram = lambda name, shape, dt, kind: nc.dram_tensor(name, list(shape), dt, kind=kind).ap()
        self.xT = dram("xT", [nseq, 128, 8, S_LEN], F32, "ExternalInput")
        self.cT = dram("cT", [128, 8, nseq], F32, "ExternalInput")
        self.w_ada = dram("w_ada", [nl, D, 6 * D], F32, "ExternalInput")
        self.bada = dram("bada", [128, nl, 48], F32, "ExternalInput")
        self.w_in = dram("w_in", [nl, D, IN_W], F32, "ExternalInput")
        self.lbl = dram("lbl", [128, nl, 4], F32, "ExternalInput")
        self.hgg = dram("hgg", [128, nl, 4], F32, "ExternalInput")
        self.w_up_a = dram("w_up_a", [nl, 512, D], F32, "ExternalInput")
        self.w_up_b = dram("w_up_b", [nl, 512, D], F32, "ExternalInput")
        self.w_o = dram("w_o", [nl, D, D], F32, "ExternalInput")
        self.w_pq = dram("w_pq", [nl, D, D], F32, "ExternalInput")
        self.skT = dram("skT", [nl, 2, 8, 64, 128], F32, "ExternalInput")
        self.uT = dram("uT", [nl, 128, 128 * 1024], F32, "ExternalInput")
        self.pv = dram("pv", [nl, 128, 128 * 1024], F32, "ExternalInput")
        self.lng = dram("lng", [128, nl, 2, 8], F32, "ExternalInput")
        self.lnb = dram("lnb", [128, nl, 2, 8], F32, "ExternalInput")
        self.outT = dram("outT", [nseq, 128, 8, S_LEN], F32, "ExternalOutput")
        if self.dbg:
            self.dbg_out = dram("dbg_out", [128, 8, S_LEN], F32, "ExternalOutput")
        self.WSC = dram("wsc", [nl, 128, NWS], BF16, "Internal")
        self.UTS = dram("uts", [nl, 128, 128 * 1024], BF16, "Internal")
        self.VS = dram("vs", [nl, 128, 128 * 1024], BF16, "Internal")
        self.bWSC = self.B("wsc")
        self.bUV = self.B("uvs")
        self.Xt = S.sb("X", [128, 8, S_LEN], F32)
        self.bX = [self.B("X%d" % i) for i in range(4)]
        self.AA = S.sb("arenaA", [128, 65536], U8)
        self.AB = S.sb("arenaB", [128, 69 * 1024], U8)
        self.PS = S.ps("psum", [128, 4096], F32)
        self.bPS = [self.B("ps%d" % i) for i in range(8)]
        c = lambda n, sh, dt=F32: S.sb(n, sh, dt)
        self.IDENT = c("ident", [128, 128]); self.IOTA = c("iota", [128, 128])
        self.TRI = c("tri", [128, 128]); self.TRIC = c("tric", [128, 128])
        self.ONESD = c("onesd", [128, 128]); self.ONESH = c("onesh", [128, 128])
        self.MASKH = c("maskh", [128, 128]); self.MASKU = c("masku", [128, 128], U32)
        self.ZEROB = c("zerob", [128, 128], BF16)
        self.MODT = c("modt", [128, nseq, nl, 48])
        self.LNG = c("lngs", [128, nl, 2, 8]); self.LNB = c("lnbs", [128, nl, 2, 8])
        self.HGG = c("hggs", [128, nl, 4]); self.LBT = c("lbt", [128, nl, 4, 3])
        self.CUR = c("cur", [128, 48 + 16 + 16 + 4 + 12])
        self.bCUR = self.B("cur")
        self.bCONST = self.B("const")
        self.prologue()
        S.sync_all()
        if self.stop_after == "pro":
            return
        self.run_loop(nseq, self.seq_body)

    def PSb(self, i, lo=0, hi=512):
        return self.PS[:, i * 512 + lo:i * 512 + hi]

    def prologue(self):
        nc, S, nseq, nl = self.nc, self.S, self.nseq, self.nl
        bC = self.bCONST
        io = lambda out, base, cm, pat: S.op("pool", lambda e: e.iota(out, pattern=pat, base=base, channel_multiplier=cm,
                                                                     allow_small_or_imprecise_dtypes=True), (), [bC])
        io(self.IOTA[:], 0, 0, [[1, 128]])
        io(self.IDENT[:], 0, -1, [[1, 128]])
        self.TS("dve", self.IDENT[:], self.IDENT[:], 0.0, None, ALU.is_equal, None, [bC], [bC])
        io(self.TRI[:], 0, 1, [[-1, 128]])
        self.TS("dve", self.TRIC[:], self.TRI[:], 0.0, None, ALU.is_le, None, [bC], [bC])
        self.TS("dve", self.TRI[:], self.TRI[:], 0.0, None, ALU.is_gt, None, [bC], [bC])
        self.MS("pool", self.ZEROB[:], 0.0, [bC])
        self.MS("pool", self.ONESD[:], 1.0 / D, [bC])
        self.MS("pool", self.ONESH[:], 1.0 / 128, [bC])
        io(self.MASKH[:], 0, -1, [[1, 128]])
        self.TS("dve", self.MASKH[:], self.MASKH[:], 0.0, None, ALU.is_ge, None, [bC], [bC])
        self.MS("dve", self.MASKH[0:64, 64:128], 0.0, [bC])
        self.CP("dve", self.MASKU[:], self.MASKH[:], [bC], [bC])
        self.LD(self.LNG[:], self.lng, bC, w=[bC]); self.LD(self.LNB[:], self.lnb, bC, w=[bC])
        self.LD(self.HGG[:], self.hgg, bC, w=[bC])
        STI = [self.AA[:, i * 16384:(i + 1) * 16384].bitcast(F32) for i in range(2)]
        STO = [self.AA[:, 32768 + i * 8192:32768 + (i + 1) * 8192].bitcast(BF16) for i in range(2)]
        bSTI = [self.B("sti0"), self.B("sti1")]
        bSTO = [self.B("sto0"), self.B("sto1")]
        jobs = []
        for l in range(nl):
            W = self.WSC[l]
            win = self.w_in[l].rearrange("(c p) j -> p c j", p=128)
            wua = self.w_up_a[l].rearrange("(h p) j -> p h j", p=128)
            wub = self.w_up_b[l].rearrange("(h p) j -> p h j", p=128)
            wo = self.w_o[l].rearrange("(c p) j -> p c j", p=128)
            wpq = self.w_pq[l].rearrange("(c p) j -> p c j", p=128)

            def part(off, n_c, w, j0, src):
                return (lambda st, off=off, n_c=n_c, w=w, j0=j0: st[:, off:off + n_c * w].rearrange("p (c j) -> p c j", c=n_c)[:, :, j0:j0 + src.shape[2]], src)
            for h in range(4):
                jobs.append((W[:, OFF_HG + h * 4096:OFF_HG + (h + 1) * 4096], 4096,
                             [part(0, 8, 512, k * 128, win[:, :, k * 512 + h * 128:k * 512 + (h + 1) * 128]) for k in range(4)], self.bWSC))
            for hp in range(4):
                jobs.append((W[:, OFF_QK + hp * 2048:OFF_QK + (hp + 1) * 2048], 2048,
                             [part(0, 8, 256, k * 128, win[:, :, 2048 + k * 512 + hp * 128:2048 + k * 512 + (hp + 1) * 128]) for k in range(2)], self.bWSC))
            jobs.append((W[:, OFF_SV:OFF_SV + 4096], 4096, [part(0, 8, 512, 0, win[:, :, 3072:3584])], self.bWSC))
            for j in range(8):
                jobs.append((W[:, OFF_MG + j * 3072:OFF_MG + (j + 1) * 3072], 3072,
                             [part(0, 8, 128, 0, win[:, :, 3584 + j * 128:3584 + (j + 1) * 128]),
                              part(1024, 8, 128, 0, win[:, :, 4608 + j * 128:4608 + (j + 1) * 128]),
                              part(2048, 4, 128, 0, wua[:, :, j * 128:(j + 1) * 128]),
                              part(2560, 4, 128, 0, wub[:, :, j * 128:(j + 1) * 128])], self.bWSC))
            for off, wsrc in ((OFF_WO, wo), (OFF_PQ, wpq)):
                for jj in range(2):
                    jobs.append((W[:, off + jj * 4096:off + (jj + 1) * 4096], 4096,
                                 [part(q * 1024, 8, 128, 0, wsrc[:, :, (jj * 4 + q) * 128:(jj * 4 + q + 1) * 128]) for q in range(4)], self.bWSC))
            if self.dbg not in ("oa", "ob", "ln1", "nouv", "hm", "proj", "ew", "blk"):
                for k in range(32):
                    sl = slice(k * 4096, (k + 1) * 4096)
                    jobs.append((self.UTS[l][:, sl], 4096, [(lambda st: st[:, 0:4096], self.uT[l][:, sl])], self.bUV))
                    jobs.append((self.VS[l][:, sl], 4096, [(lambda st: st[:, 0:4096], self.pv[l][:, sl])], self.bUV))

        def job_load(i):
            dst, n, parts, db = jobs[i]
            for vf, src in parts:
                self.LD(vf(STI[i % 2]), src, bSTI[i % 2], w=[bSTI[i % 2]])

        def job_rest(i):
            dst, n, parts, db = jobs[i]
            eng = ("act", "dve", "pool")[i % 3]
            self.CP(eng, STO[i % 2][:, 0:n], STI[i % 2][:, 0:n], [bSTI[i % 2]], [bSTO[i % 2]])
            self.LD(dst, STO[i % 2][:, 0:n], bSTO[i % 2], r=[bSTO[i % 2]], w=[db])
        if jobs:
            job_load(0)
            for i in range(len(jobs)):
                if i + 1 < len(jobs):
                    job_load(i + 1)
                job_rest(i)
        LBL = self.cv(self.AB, 0, [128, nl, 4], F32)
        LBE = self.cv(self.AB, 256, [128, nl, 4], F32)
        LBM = self.cv(self.AB, 512, [128, 4], F32)
        bT = self.B("lbtmp")
        self.LD(LBL, self.lbl, bT, w=[bT])
        S.op("dve", lambda e: e.tensor_reduce(out=LBM, in_=LBL.rearrange("p l h -> p h l"), axis=AX.X, op=ALU.max), [bT], [bT])
        self.TT("dve", LBE, LBL, LBM[:, None, :].broadcast_to([128, nl, 4]), ALU.subtract, [bT], [bT])
        self.ACT(LBE, LBE, AF.Exp, [bT], [bT])
        S.op("dve", lambda e: e.tensor_reduce(out=LBM, in_=LBE.rearrange("p l h -> p h l"), axis=AX.X, op=ALU.add), [bT], [bT])
        S.op("dve", lambda e: e.reciprocal(out=LBM, in_=LBM), [bT], [bT])
        self.TT("dve", LBE, LBE, LBM[:, None, :].broadcast_to([128, nl, 4]), ALU.mult, [bT], [bT])
        self.MS("dve", self.LBT[:, 0, :, 0], 0.0, [bC])
        for l in range(1, nl):
            self.TT("dve", self.LBT[:, l, :, 0], self.LBT[:, l - 1, :, 0], LBE[:, l, :], ALU.add, [bT, bC], [bC])
        self.TS("dve", self.LBT[:, :, :, 1], self.LBT[:, :, :, 0], -1.0, 1.0, ALU.mult, ALU.add, [bC], [bC])
        self.TS("dve", self.LBT[:, :, :, 2], self.LBT[:, :, :, 0], -1.0, None, ALU.add, None, [bC], [bC])
        COND = self.cv(self.AB, 1024, [128, 8, nseq], F32)
        BADA = self.cv(self.AB, 2048, [128, nl, 48], F32)
        bCo = self.B("cond")
        self.LD(COND, self.cT, bCo, w=[bCo])
        self.LD(BADA, self.bada, bCo, w=[bCo])
        self.ACT(COND, COND, AF.Silu, [bCo], [bCo])
        WA = [self.cv(self.AB, 8192 + i * 16384, [128, 8, 512], F32) for i in range(2)]
        bWA = [self.B("wa0"), self.B("wa1")]
        it = 0
        for l in range(nl):
            wa = self.w_ada[l].rearrange("(c p) j -> p c j", p=128)
            for g in range(12):
                s = it % 2
                self.LD(WA[s], wa[:, :, g * 512:(g + 1) * 512], bWA[s], w=[bWA[s]])
                bank = self.PSb(it % 4, 0, 4 * nseq)
                for jl in range(4):
                    self.chain(bank[:, jl * nseq:(jl + 1) * nseq],
                               [(WA[s][:, cc, jl * 128:(jl + 1) * 128], COND[:, cc, :]) for cc in range(8)],
                               [bWA[s], bCo], [self.bPS[it % 4]])
                self.TT("dve", self.MODT[:, :, l, g * 4:(g + 1) * 4], bank.rearrange("p (j b) -> p b j", b=nseq),
                        BADA[:, l, g * 4:(g + 1) * 4][:, None, :].broadcast_to([128, nseq, 4]), ALU.add,
                        [self.bPS[it % 4], bCo], [bC])
                it += 1
        for k in (1, 4):
            self.TS("dve", self.MODT[:, :, :, k * 8:(k + 1) * 8], self.MODT[:, :, :, k * 8:(k + 1) * 8], 1.0, None, ALU.add, None, [bC], [bC])
        for k in (2, 5):
            self.TS("dve", self.MODT[:, :, :, k * 8:(k + 1) * 8], self.MODT[:, :, :, k * 8:(k + 1) * 8], 1.0 / ALPHA, None, ALU.mult, None, [bC], [bC])

    def seq_body(self, b):
        self.b = b
        for cc in range(8):
            self.LD(self.Xt[:, cc, :], self.xT[b][:, cc, :], self.bX[0], w=self.bX)
        for l in range(self.nl):
            self.S.sync_all()
            self.layer_body(l)
        self.S.sync_all()
        for cc in range(8):
            self.LD(self.outT[b][:, cc, :], self.Xt[:, cc, :], self.bX[0], r=self.bX)
        if self.dbg:
            pass

    def layer_body(self, l):
        self.l = l
        b = self.b
        C = self.CUR
        bc = self.bCUR
        self.CP("pool", C[:, 0:48], self.MODT[:, b, l, :], [self.bCONST], [bc])
        self.CP("dve", C[:, 48:64], self.LNG[:, l].rearrange("p a b -> p (a b)"), [self.bCONST], [bc])
        self.CP("dve", C[:, 64:80], self.LNB[:, l].rearrange("p a b -> p (a b)"), [self.bCONST], [bc])
        self.CP("dve", C[:, 80:84], self.HGG[:, l, :], [self.bCONST], [bc])
        self.CP("dve", C[:, 84:96], self.LBT[:, l].rearrange("p a b -> p (a b)"), [self.bCONST], [bc])
        self.phase1()
        if self.stop_after == "p1":
            return
        self.S.sync_all()
        self.phase2()

    def mod(self, k, cc):
        return self.CUR[:, k * 8 + cc:k * 8 + cc + 1]

    def layer_norm(self, which, base):
        SQ = self.cv(self.AB, base, [128, 8, 512], F32)
        MEAN = self.cv(self.AB, base + 16384, [128, 512], F32)
        M2 = self.cv(self.AB, base + 18432, [128, 512], F32)
        RSTD = self.cv(self.AB, base + 20480, [128, 512], F32)
        bSQ, bM, bM2, bR = self.B(), self.B(), self.B(), self.B()
        for tt in range(4):
            bx = self.bX[tt]
            cs = slice(tt * 512, (tt + 1) * 512)
            Xv = self.Xt[:, :, cs]
            self.ACT(SQ, Xv, AF.Square, [bx], [bSQ])
            pm, pq = (0, 1) if tt % 2 == 0 else (2, 3)
            self.chain(self.PSb(pm), [(self.ONESD[:], self.Xt[:, cc, cs]) for cc in range(8)], [bx, self.bCONST], [self.bPS[pm]])
            self.chain(self.PSb(pq), [(self.ONESD[:], SQ[:, cc, :]) for cc in range(8)], [bSQ, self.bCONST], [self.bPS[pq]])
            self.CP("act", MEAN, self.PSb(pm), [self.bPS[pm]], [bM])
            self.TT("dve", M2, MEAN, MEAN, ALU.mult, [bM], [bM2])
            self.TT("dve", M2, self.PSb(pq), M2, ALU.subtract, [self.bPS[pq], bM2], [bM2])
            self.ACT(M2, M2, AF.Ln, [bM2], [bM2], bias=LN_EPS / (ALPHA * ALPHA))
            self.ACT(RSTD, M2, AF.Exp, [bM2], [bR], scale=-0.5)
            self.TT("dve", Xv, Xv, MEAN[:, None, :].broadcast_to([128, 8, 512]), ALU.subtract, [bx, bM], [bx])
            self.TT("pool", Xv, Xv, RSTD[:, None, :].broadcast_to([128, 8, 512]), ALU.mult, [bx, bR], [bx])
            for cc in range(8):
                g = self.CUR[:, 48 + which * 8 + cc:48 + which * 8 + cc + 1]
                bb = self.CUR[:, 64 + which * 8 + cc:64 + which * 8 + cc + 1]
                self.TS("dve" if cc % 2 == 0 else "pool", self.Xt[:, cc, cs], self.Xt[:, cc, cs], g, bb, ALU.mult, ALU.add, [bx, self.bCUR], [bx])

    def phase1(self):
        nc, S = self.nc, self.S
        l = self.l
        AA, AB = self.AA, self.AB
        HM = self.cv(AA, 0, [128, 8, S_LEN], BF16); bHM = self.B("hm")
        OA = self.cv(AA, 32768, [128, 4, S_LEN], BF16); bOA = self.B("oa")
        OB = self.cv(AA, 49152, [128, 4, S_LEN], BF16); bOB = self.B("ob")
        WS = [AB[:, 0:8192].bitcast(BF16), AB[:, 8192:16384].bitcast(BF16)]
        bWS = [self.B("ws0"), self.B("ws1")]
        self.ws_i = 0
        W = self.WSC[l]
        bPS = self.bPS

        def load_ws(off, n):
            s = self.ws_i % 2
            self.ws_i += 1
            o = 0
            for piece in (4096, 2048, 1024):
                while n - o >= piece:
                    self.LD(WS[s][:, o:o + piece], W[:, off + o:off + o + piece], bWS[s], r=[self.bWSC], w=[bWS[s]])
                    o += piece
            assert o == n
            return WS[s], bWS[s]

        for cc in range(8):
            if cc % 2 == 0:
                self.ACT(HM[:, cc, :], self.Xt[:, cc, :], AF.Identity, self.bX + [self.bCUR], [bHM], bias=self.mod(0, cc), scale=self.mod(1, cc))
            else:
                self.TS("dve", HM[:, cc, :], self.Xt[:, cc, :], self.mod(1, cc), self.mod(0, cc), ALU.mult, ALU.add, self.bX + [self.bCUR], [bHM])

        if self.dbg == "hm":
            self.dump_bf(HM, [bHM], 8)
            return
        SC = 16384
        P = [self.cv(AB, SC + i * 8192, [128, S_LEN], F32) for i in range(4)]
        bP = [self.B("P%d" % i) for i in range(4)]
        VH = self.cv(AB, SC + 32768, [128, 16, 128], F32); bVH = self.B("vh")
        RM = self.cv(AB, SC + 40960, [128, S_LEN], F32); bRM = self.B("rm")
        MI = SC + 49152
        SCM = [self.cv(AB, MI + i * 512, [128, 128], F32) for i in range(2)]; bSCM = [self.B(), self.B()]
        KDT = [self.cv(AB, MI + 1024 + i * 512, [128, 128], F32) for i in range(2)]; bKDT = [self.B(), self.B()]
        ST = [self.cv(AB, MI + 2048 + i * 512, [128, 128], F32) for i in range(2)]; bST = [self.B(), self.B()]
        EBE = self.cv(AB, MI + 3072, [128, 32], F32); bEBE = self.B()
        BMD = self.cv(AB, MI + 3200, [128, 32], F32)
        self.MS("pool", RM, 1.0, [bRM])
        self.MS("pool", RM.rearrange("p (a b) -> p a b", b=64)[:, :, 0:1], 0.0, [bRM])
        SCP = [self.PSb(i, 0, 128) for i in range(2)]; bSCP = [self.bPS[0], self.bPS[1]]
        KTP = [self.PSb(2 + i, 0, 128) for i in range(2)]; bKTP = [self.bPS[2], self.bPS[3]]
        STP = [self.PSb(6 + i, 0, 128) for i in range(2)]; bSTP = [self.bPS[6], self.bPS[7]]
        pr = [0]

        def proj_fm(ws, bws, col0, evac):
            for tt in range(4):
                bk = pr[0] % 4
                pr[0] += 1
                self.chain(self.PSb(bk), [(ws.rearrange("p (c j) -> p c j", c=8)[:, cc, col0:col0 + 128], HM[:, cc, tt * 512:(tt + 1) * 512]) for cc in range(8)],
                           [bws, bHM], [bPS[bk]])
                evac(tt, self.PSb(bk), bPS[bk])

        for h in range(4):
            ws, bws = load_ws(OFF_HG + h * 4096, 4096)
            ws3 = ws[:, 0:4096].rearrange("p (c j) -> p c j", c=8)
            lb = self.CUR[:, 84 + h * 3:84 + h * 3 + 1]
            omlb = self.CUR[:, 84 + h * 3 + 1:84 + h * 3 + 2]
            nomlb = self.CUR[:, 84 + h * 3 + 2:84 + h * 3 + 3]
            wsf = ws[:, 0:4096]
            proj_fm(wsf, bws, 0, lambda tt, ps, bps: self.CP("act", P[0][:, tt * 512:(tt + 1) * 512], ps, [bps], [bP[0]]))
            proj_fm(wsf, bws, 128, lambda tt, ps, bps: self.ACT(P[1][:, tt * 512:(tt + 1) * 512], ps, AF.Sigmoid, [bps], [bP[1]]))
            for g in range(4):
                bk = pr[0] % 4
                pr[0] += 1
                for j in range(4):
                    t = g * 4 + j
                    self.chain(self.PSb(bk, j * 128, (j + 1) * 128), [(HM[:, cc, t * 128:(t + 1) * 128], ws3[:, cc, 256:384]) for cc in range(8)],
                               [bws, bHM], [bPS[bk]])
                self.CP("dve", VH[:, g * 4:(g + 1) * 4, :], self.PSb(bk).rearrange("p (a b) -> p a b", a=4), [bPS[bk]], [bVH])
            if self.dbg == "proj":
                self.S.sync_all()
                for i in range(2):
                    self.LD(self.dbg_out[:, i, :], P[i], bP[i], r=[bP[i]])
                self.LD(self.dbg_out[:, 2, :], VH.rearrange("p a b -> p (a b)"), bVH, r=[bVH])
                return
            self.TS("dve", P[2], P[1], nomlb, omlb, ALU.mult, ALU.add, [bP[1], self.bCUR], [bP[2]])
            self.TS("dve", P[1], P[1], omlb, lb, ALU.mult, ALU.add, [bP[1], self.bCUR], [bP[1]])
            self.ACT(P[1], P[1], AF.Ln, [bP[1]], [bP[1]])
            S.op("dve", lambda e: e.tensor_tensor_scan(out=P[3], data0=RM, data1=P[1], initial=0.0, op0=ALU.mult, op1=ALU.add), [bRM, bP[1]], [bP[3]])
            P3v = P[3].rearrange("p (a b) -> p a b", b=64)
            self.CP("dve", BMD, P3v[:, :, 31], [bP[3]], [bEBE])
            self.TT("dve", P3v, P3v, BMD[:, :, None].broadcast_to([128, 32, 64]), ALU.subtract, [bP[3], bEBE], [bP[3]])
            self.TS("dve", P[3], P[3], -80.0, 80.0, ALU.max, ALU.min, [bP[3]], [bP[3]])
            self.TT("dve", EBE[:, 0:31], P3v[:, 0:31, 63], BMD[:, 1:32], ALU.add, [bP[3], bEBE], [bEBE])
            self.CP("dve", EBE[:, 31:32], P3v[:, 31:32, 63], [bP[3]], [bEBE])
            self.ACT(EBE, EBE, AF.Exp, [bEBE], [bEBE])
            self.ACT(P[1], P[3], AF.Exp, [bP[3]], [bP[1]])
            self.TT("pool", P[0], P[0], P[1], ALU.mult, [bP[0], bP[1]], [bP[0]])
            self.ACT(P[1], P[3], AF.Exp, [bP[3], bP[1]], [bP[1]], scale=-1.0)
            self.TT("pool", P[2], P[2], P[1], ALU.mult, [bP[2], bP[1]], [bP[2]])
            if self.dbg == "ew":
                self.S.sync_all()
                for i in range(4):
                    self.LD(self.dbg_out[:, i, :], P[i], bP[i], r=[bP[i]])
                return
            self.MS("dve", ST[0], 0.0, [bST[0]])
            cur = 0

            def pre(blk):
                import os as _os
                _pm = _os.environ.get("PRE_MODE", "")
                s = blk % 2
                cs = slice(blk * 128, (blk + 1) * 128)
                if "a" in _pm:
                    s = 0
                if "b" in _pm:
                    cs = slice(0, 128)
                sp_, sm_ = s, s
                if "c" in _pm:
                    sm_ = 0
                if "d" in _pm:
                    sp_ = 0
                self.chain(SCP[sp_], [(P[2][:, cs], P[0][:, cs])], [bP[2], bP[0]], [bSCP[sp_]])
                if "t" not in _os.environ.get("BLK_FL", "t"):
                    self.TT("dve", SCM[sm_], SCP[sp_], self.MASKH[:], ALU.mult, [bSCP[sp_], self.bCONST], [bSCM[sm_]])
                    return
                import os as _os
                if "t" in _os.environ.get("BLK_FL", "t"):
                    S.op("pe", lambda e: e.transpose(KTP[s], P[2][:, cs], self.IDENT[:]), [bP[2], self.bCONST], [bKTP[s]])
                    self.CP("act", KDT[s], KTP[s], [bKTP[s]], [bKDT[s]])
                self.MS("pool", SCM[s], 0.0, [bSCM[s]])
                S.op("dve", lambda e: e.copy_predicated(out=SCM[s], mask=self.MASKU[:], data=SCP[s]), [bSCP[s], self.bCONST, bSCM[s]], [bSCM[s]])
            pre(0)
            import os as _os
            _NB = int(_os.environ.get("BLK_N", "16")); _FL = _os.environ.get("BLK_FL", "osu")
            for blk in range(_NB):
                s = blk % 2
                if blk + 1 < 16:
                    pre(blk + 1)
                ob = 4 + (blk // 4) % 2
                for ch in range(2):
                    c0 = blk * 128 + ch * 64
                    oc = (blk % 4) * 128 + ch * 64
                    rows = slice(ch * 64, (ch + 1) * 64)
                    if "o" in _FL:
                        self.chain(self.PSb(ob, oc, oc + 64),
                               [(ST[cur], P[0][:, c0:c0 + 64]), (VH[:, blk, :], SCM[s][:, ch * 64:(ch + 1) * 64])],
                               [bST[cur], bP[0], bVH, bSCM[s]], [bPS[ob]])
                    if "s" not in _FL:
                        continue
                    self.chain(STP[cur], [(self.IDENT[:], ST[cur]), (KDT[s][rows, :], VH[rows, blk, :])],
                               [self.bCONST, bST[cur], bKDT[s], bVH], [bSTP[cur]])
                    eb = EBE[:, 2 * blk + ch:2 * blk + ch + 1]
                    if "u" not in _FL:
                        continue
                    if ch == 0:
                        self.ACT(ST[1 - cur], STP[cur], AF.Identity, [bSTP[cur], bEBE], [bST[1 - cur]], scale=eb)
                    else:
                        self.TS("dve", ST[1 - cur], STP[cur], eb, None, ALU.mult, None, [bSTP[cur], bEBE], [bST[1 - cur]])
                    cur = 1 - cur
                if blk % 4 == 3 and "o" in _FL:
                    tt = blk // 4
                    self.CP("act", P[3][:, tt * 512:(tt + 1) * 512], self.PSb(ob), [bPS[ob]], [bP[3]])
            if self.dbg == "blk":
                self.S.sync_all()
                self.LD(self.dbg_out[:, 0, :], P[3], bP[3], r=[bP[3]])
                return
            self.ACT(P[1], P[3], AF.Square, [bP[3]], [bP[1]])
            for tt in range(4):
                bk = pr[0] % 4
                pr[0] += 1
                cs = slice(tt * 512, (tt + 1) * 512)
                self.chain(self.PSb(bk), [(self.ONESH[:], P[1][:, cs])], [bP[1], self.bCONST], [bPS[bk]])
                self.ACT(P[2][:, cs], self.PSb(bk), AF.Ln, [bPS[bk]], [bP[2]], bias=RMS_EPS)
            self.ACT(P[2], P[2], AF.Exp, [bP[2]], [bP[2]], scale=-0.5)
            proj_fm(wsf, bws, 384, lambda tt, ps, bps: self.ACT(P[0][:, tt * 512:(tt + 1) * 512], ps, AF.Silu, [bps], [bP[0]]))
            self.STT("dve", P[1], P[3], self.CUR[:, 80 + h:81 + h], P[2], ALU.mult, ALU.mult, [bP[3], bP[2], self.bCUR], [bP[1]])
            self.TT("pool", OA[:, h, :], P[1], P[0], ALU.mult, [bP[1], bP[0]], [bOA])

        if self.dbg == "oa":
            self.dump_bf(OA, [bOA], 4)
            return
        QT2 = self.cv(AB, SC, [128, S_LEN], BF16); KT2 = self.cv(AB, SC + 4096, [128, S_LEN], BF16)
        VSB = self.cv(AB, SC + 8192, [128, 16, 512], BF16)
        bQK = bP[0]
        bVS = bP[1]
        bST2 = [bP[2], bP[3]]
        EE = [self.cv(AB, SC + 24576 + i * 2048, [128, 512], F32) for i in range(2)]
        SPp = [self.cv(AB, SC + 28672 + i * 2048, [128, 512], F32) for i in range(2)]
        LW = [self.cv(AB, SC + 32768 + i * 2048, [128, 512], F32) for i in range(2)]
        WT = [self.cv(AB, SC + 36864 + i * 1024, [128, 512], BF16) for i in range(2)]
        bEE = [bVH, bRM]; bSP = [bSCM[0], bSCM[1]]; bLW = [bKDT[0], bKDT[1]]; bWT = [bST[0], bST[1]]
        S.sync_all()
        ws, bws = load_ws(OFF_SV, 4096)
        ws3 = ws[:, 0:4096].rearrange("p (c j) -> p c j", c=8)
        for t in range(16):
            bk = 4 + t % 4
            self.chain(self.PSb(bk), [(HM[:, cc, t * 128:(t + 1) * 128], ws3[:, cc, :]) for cc in range(8)], [bws, bHM], [bPS[bk]])
            self.CP("act" if t % 2 == 0 else "dve", VSB[:, t, :], self.PSb(bk), [bPS[bk]], [bVS])
        step = [0]
        for hp in range(4):
            ws, bws = load_ws(OFF_QK + hp * 2048, 2048)
            wsf = ws[:, 0:2048]
            for k, dst in ((0, QT2), (1, KT2)):
                for tt in range(4):
                    bk = 4 + pr[0] % 4
                    pr[0] += 1
                    self.chain(self.PSb(bk), [(wsf.rearrange("p (c j) -> p c j", c=8)[:, cc, k * 128:(k + 1) * 128], HM[:, cc, tt * 512:(tt + 1) * 512]) for cc in range(8)],
                               [bws, bHM], [bPS[bk]])
                    self.CP("act" if tt % 2 == 0 else "dve", dst[:, tt * 512:(tt + 1) * 512], self.PSb(bk), [bPS[bk]], [bQK])
            for hh in range(2):
                h = 2 * hp + hh
                rows = slice(hh * 64, (hh + 1) * 64)
                for ct in range(4):
                    A = self.PSb(2); bA = bPS[2]
                    O = self.PS[0:64, 3 * 512:4 * 512]; bO = bPS[3]
                    self.chain(A, [(self.ZEROB[:], HM[:, 0, 0:512])], [self.bCONST, bHM], [bA])
                    self.chain(O, [(self.ZEROB[:, 0:64], HM[:, 0, 0:512])], [self.bCONST, bHM], [bO])
                    for kb in range(4 * ct + 3, -1, -1):
                        s = step[0] % 2
                        step[0] += 1
                        first = max(kb, 4 * ct)
                        c0, c1 = first * 128, (4 * ct + 4) * 128
                        w = c1 - c0
                        lo = c0 - ct * 512
                        diag = kb >= 4 * ct
                        Z = self.PSb(s, 0, w); bZ = bPS[s]
                        self.chain(Z, [(KT2[rows, kb * 128:(kb + 1) * 128], QT2[rows, c0:c1])], [bQK], [bZ])
                        self.ACT(EE[s][:, 0:w], Z, AF.Exp, [bZ], [bEE[s]], scale=0.125)
                        self.ACT(SPp[s][:, 0:w], EE[s][:, 0:w], AF.Ln, [bEE[s]], [bSP[s]], bias=1.0)
                        if diag:
                            S.op("pool", lambda e: e.affine_select(out=SPp[s][:, 0:128], in_=SPp[s][:, 0:128], pattern=[[1, 128]],
                                                                   compare_op=ALU.is_gt, fill=0.0, base=0, channel_multiplier=-1), [bSP[s]], [bSP[s]])

                        def acc(dst, lhsT, rhs_t, r, wb, lo=lo, w=w, diag=diag, force_nostart=False):
                            def fn(e):
                                return e.matmul(dst[:, lo:lo + w], lhsT=lhsT, rhs=rhs_t[:, 0:w], start=False, stop=True)
                            S.op("pe", fn, r, wb)
                        acc(A, self.TRI[:], SPp[s], [bSP[s], self.bCONST], [bA])
                        self.STT("dve", LW[s][:, 0:w], Z, 0.125, SPp[s][:, 0:w], ALU.mult, ALU.subtract, [bZ, bSP[s]], [bLW[s]])
                        self.TT("dve", LW[s][:, 0:w], LW[s][:, 0:w], A[:, lo:lo + w], ALU.subtract, [bLW[s], bA], [bLW[s]])
                        self.ACT(WT[s][:, 0:w], LW[s][:, 0:w], AF.Exp, [bLW[s]], [bWT[s]])
                        if diag:
                            S.op("pool", lambda e: e.affine_select(out=WT[s][:, 0:128], in_=WT[s][:, 0:128], pattern=[[1, 128]],
                                                                   compare_op=ALU.is_gt, fill=0.0, base=0, channel_multiplier=-1), [bWT[s]], [bWT[s]])
                        acc(O, VSB[:, kb, h * 64:(h + 1) * 64], WT[s], [bVS, bWT[s]], [bO])
                        if kb > 0:
                            acc(A, self.TRIC[:], SPp[s], [bSP[s], self.bCONST], [bA], force_nostart=True)
                    self.CP("act", OB[hh * 64:(hh + 1) * 64, hp, ct * 512:(ct + 1) * 512], O, [bO], [bOB])
        if self.dbg == "ob":
            self.dump_bf(OB, [bOB], 4)
            return
        S.sync_all()
        MT = self.cv(AB, SC, [128, 8, S_LEN], BF16); bMT = self.B("mt")
        SG = [self.cv(AB, SC + 32768 + i * 2048, [128, 512], F32) for i in range(4)]
        bSG = [self.B() for _ in range(4)]
        it = 0
        for j in range(8):
            ws, bws = load_ws(OFF_MG + j * 3072, 3072)
            ga = ws[:, 0:1024].rearrange("p (c j) -> p c j", c=8)
            gb = ws[:, 1024:2048].rearrange("p (c j) -> p c j", c=8)
            ua = ws[:, 2048:2560].rearrange("p (c j) -> p c j", c=4)
            ub = ws[:, 2560:3072].rearrange("p (c j) -> p c j", c=4)
            for tt in range(4):
                cs = slice(tt * 512, (tt + 1) * 512)
                pb = (it % 2) * 4
                it += 1
                self.chain(self.PSb(pb), [(ga[:, cc, :], HM[:, cc, cs]) for cc in range(8)], [bws, bHM], [bPS[pb]])
                self.chain(self.PSb(pb + 1), [(ua[:, hh, :], OA[:, hh, cs]) for hh in range(4)], [bws, bOA], [bPS[pb + 1]])
                self.chain(self.PSb(pb + 2), [(gb[:, cc, :], HM[:, cc, cs]) for cc in range(8)], [bws, bHM], [bPS[pb + 2]])
                self.chain(self.PSb(pb + 3), [(ub[:, hh, :], OB[:, hh, cs]) for hh in range(4)], [bws, bOB], [bPS[pb + 3]])
                self.ACT(SG[0], self.PSb(pb), AF.Sigmoid, [bPS[pb]], [bSG[0]])
                self.ACT(SG[1], self.PSb(pb + 2), AF.Sigmoid, [bPS[pb + 2]], [bSG[1]])
                self.TT("dve", SG[2], SG[0], self.PSb(pb + 1), ALU.mult, [bSG[0], bPS[pb + 1]], [bSG[2]])
                self.TT("dve", SG[3], SG[1], self.PSb(pb + 3), ALU.mult, [bSG[1], bPS[pb + 3]], [bSG[3]])
                self.TT("pool", MT[:, j, cs], SG[2], SG[3], ALU.add, [bSG[2], bSG[3]], [bMT])
        it = 0
        for jo in range(8):
            ws, bws = load_ws(OFF_WO + jo * 1024, 1024)
            wo = ws[:, 0:1024].rearrange("p (c j) -> p c j", c=8)
            for tt in range(4):
                cs = slice(tt * 512, (tt + 1) * 512)
                pb = it % 4
                it += 1
                self.chain(self.PSb(pb), [(wo[:, jj, :], MT[:, jj, cs]) for jj in range(8)], [bws, bMT], [bPS[pb]])
                self.STT("dve", self.Xt[:, jo, cs], self.PSb(pb), self.mod(2, jo), self.Xt[:, jo, cs], ALU.mult, ALU.add,
                         [bPS[pb], self.bCUR, self.bX[tt]], [self.bX[tt]])
        S.sync_all()
        self.layer_norm(0, SC)
        if self.dbg == "ln1":
            self.dump_x()

    def dump_x(self):
        for cc in range(8):
            self.LD(self.dbg_out[:, cc, :], self.Xt[:, cc, :], self.bX[0], r=self.bX)

    def dump_bf(self, src, bufs, n):
        T = self.cv(self.AB, 16384, [128, S_LEN], F32)
        bT = self.B()
        self.S.sync_all()
        for i in range(n):
            self.CP("dve", T, src[:, i, :], bufs, [bT])
            self.LD(self.dbg_out[:, i, :], T, bT, r=[bT])

    def phase2(self):
        nc, S = self.nc, self.S
        l = self.l
        AB = self.AB
        W = self.WSC[l]
        self.G = self.cv(self.AA, 0, [128, 256, 128], BF16); self.bG = self.B("G")
        self.UT = [self.cv(AB, i * 8192, [128, 4, 8, 128], BF16) for i in range(2)]
        self.VV = [self.cv(AB, 16384 + i * 8192, [128, 4, 1024], BF16) for i in range(2)]
        self.bUT = [self.B("ut0"), self.B("ut1")]
        self.bVV = [self.B("v0"), self.B("v1")]
        self.HFT = self.cv(AB, 32768, [128, 8, 256], BF16); self.bHFT = self.B("hft")
        self.QT = self.cv(AB, 36864, [128, 8, 256], BF16); self.bQT = self.B("qt")
        self.WQ = self.cv(AB, 16384, [128, 8, 8, 128], BF16)
        self.KBD = self.cv(AB, 40960, [128, 8, 256], BF16); self.bKBD = self.B("kbd")
        KBF = self.cv(AB, 0, [128, 8, 256], F32)
        bKF = self.bUT[0]
        self.MS("pool", KBF, 0.0, [bKF])
        self.LD(KBF[0:64, :, 0:128], self.skT[l, 0].rearrange("h d n -> d h n"), bKF, w=[bKF])
        self.LD(KBF[64:128, :, 128:256], self.skT[l, 1].rearrange("h d n -> d h n"), bKF, w=[bKF])
        self.CP("dve", self.KBD, KBF, [bKF], [self.bKBD])
        S.sync_all()
        self.run_loop(self.ntiles, self.peer_tile)
        if self.dbg == "peer":
            self.dump_x()
            return
        self.layer_norm(1, 0)
        if self.dbg == "ln2":
            self.dump_x()

    def peer_tile(self, tq):
        nc, S = self.nc, self.S
        l = self.l
        AB = self.AB
        bPS = self.bPS
        bXa = self.bX
        tsl = bass.ts(tq, 256)
        HFT, QT, WQ, KBD, G = self.HFT, self.QT, self.WQ, self.KBD, self.G
        XC = self.cv(AB, 8192, [128, 8, 256], F32)
        self.CP("pool", XC, self.Xt[:, :, tsl], bXa, [self.bUT[1]])
        for cc in range(8):
            if cc % 2 == 0:
                self.ACT(HFT[:, cc, :], XC[:, cc, :], AF.Identity, [self.bUT[1], self.bCUR], [self.bHFT], bias=self.mod(3, cc), scale=self.mod(4, cc))
            else:
                self.TS("dve", HFT[:, cc, :], XC[:, cc, :], self.mod(4, cc), self.mod(3, cc), ALU.mult, ALU.add, [self.bUT[1], self.bCUR], [self.bHFT])
        self.LD(WQ.rearrange("p a b c -> p (a b c)"), self.WSC[l][:, OFF_PQ:OFF_PQ + 8192], self.bVV[0], r=[self.bWSC], w=self.bVV)
        for h in range(8):
            bk = 4 + h % 4
            self.chain(self.PSb(bk, 0, 256), [(WQ[:, h, cc, :], HFT[:, cc, :]) for cc in range(8)], self.bVV + [self.bHFT], [bPS[bk]])
            self.CP("act" if h % 2 == 0 else "dve", QT[:, h, :], self.PSb(bk, 0, 256), [bPS[bk]], [self.bQT])
        SS = self.cv(AB, 0, [128, 2048], F32); bSS = self.bUT[0]
        EQ = self.cv(AB, 8192, [128, 2048], F32); bEQ = self.bUT[1]
        T0 = 45056
        TOPV = self.cv(AB, T0, [128, 16, 16], F32)
        TOPI = self.cv(AB, T0 + 1024, [128, 16, 16], U32)
        TOPIF = self.cv(AB, T0 + 2048, [128, 16, 16], F32)
        SR = self.cv(AB, T0 + 3072, [128, 256], F32)
        CTOP = self.cv(AB, T0 + 4096, [128, 8, 16], F32)
        CPOS = self.cv(AB, T0 + 4608, [128, 128], U32)
        CAI = self.cv(AB, T0 + 5120, [128, 128], I32)
        CBI = self.cv(AB, T0 + 5632, [128, 128], I32)
        CAF = self.cv(AB, T0 + 6144, [128, 128], F32)
        CBF = self.cv(AB, T0 + 6656, [128, 128], F32)
        IDX = [self.cv(AB, T0 + 7168 + i * 512, [128, 128], F32) for i in range(3)]
        GSUM = self.cv(AB, T0 + 8704, [128, 8], F32)
        IDT = [self.cv(AB, T0 + 9216 + i * 1024, [128, 256], F32) for i in range(3)]
        OH0 = T0 + 12288
        EQ1 = self.cv(AB, OH0, [128, 8, 128], BF16)
        OH1 = self.cv(AB, OH0 + 2048, [128, 8, 128], BF16)
        OH2 = self.cv(AB, OH0 + 4096, [128, 8, 128], BF16)
        AG0 = OH0 + 6144
        AG = [self.cv(AB, AG0 + i * 1024, [128, 256], F32) for i in range(2)]
        WW = [self.cv(AB, AG0 + 2048 + i * 512, [128, 256], BF16) for i in range(2)]
        bTK = self.B("topk"); bSR = self.B("sr"); bIDX = self.B("idx"); bIDT = self.B("idt")
        bE1, bO1, bO2 = self.B(), self.B(), self.B()
        bAG = [self.B(), self.B()]; bWW = [self.B(), self.B()]
        dv = lambda fn, r, w: S.op("dve", fn, r, w)
        for st in range(2):
            tcs = slice(st * 128, (st + 1) * 128)
            for h in range(8):
                bk = 4 + h // 2
                self.chain(self.PSb(bk, (h % 2) * 256, (h % 2) * 256 + 256), [(QT[:, h, tcs], KBD[:, h, :])], [self.bQT, self.bKBD], [bPS[bk]])
            for k in range(4):
                self.CP("act" if k % 2 == 0 else "dve", SS[:, k * 512:(k + 1) * 512], self.PSb(4 + k), [bPS[4 + k]], [bSS])
            for g in range(16):
                v = SS[:, g * 128:(g + 1) * 128]
                dv(lambda e, g=g, v=v: e.max(out=TOPV[:, g, 0:8], in_=v), [bSS], [bTK])
                dv(lambda e, g=g, v=v: e.match_replace(out=SR[:, 0:128], in_to_replace=TOPV[:, g, 0:8], in_values=v, imm_value=-1e30), [bSS, bTK], [bSR])
                dv(lambda e, g=g: e.max(out=TOPV[:, g, 8:16], in_=SR[:, 0:128]), [bSR], [bTK])
                dv(lambda e, g=g, v=v: e.max_index(out=TOPI[:, g, 0:8], in_max=TOPV[:, g, 0:8], in_values=v), [bSS, bTK], [bTK])
                dv(lambda e, g=g, v=v: e.max_index(out=TOPI[:, g, 8:16], in_max=TOPV[:, g, 8:16], in_values=v), [bSS, bTK], [bTK])
            self.CP("dve", TOPIF, TOPI, [bTK], [bTK])
            TV4 = TOPV.rearrange("p (h two) k -> p h two k", two=2)
            TI4 = TOPIF.rearrange("p (h two) k -> p h two k", two=2)
            self.TT("dve", SS.rearrange("p (h a b) -> p h a b", h=8, a=16),
                    TV4[:, :, 0, :, None].broadcast_to([128, 8, 16, 16]), TV4[:, :, 1, None, :].broadcast_to([128, 8, 16, 16]), ALU.add, [bTK], [bSS])
            for h in range(8):
                v = SS[:, h * 256:(h + 1) * 256]
                dv(lambda e, h=h, v=v: e.max(out=CTOP[:, h, 0:8], in_=v), [bSS], [bTK])
                dv(lambda e, h=h, v=v: e.match_replace(out=SR[:, 0:256], in_to_replace=CTOP[:, h, 0:8], in_values=v, imm_value=-1e30), [bSS, bTK], [bSR])
                dv(lambda e, h=h: e.max(out=CTOP[:, h, 8:16], in_=SR[:, 0:256]), [bSR], [bTK])
                dv(lambda e, h=h, v=v: e.max_index(out=CPOS[:, h * 16:h * 16 + 8], in_max=CTOP[:, h, 0:8], in_values=v), [bSS, bTK], [bTK])
                dv(lambda e, h=h, v=v: e.max_index(out=CPOS[:, h * 16 + 8:h * 16 + 16], in_max=CTOP[:, h, 8:16], in_values=v), [bSS, bTK], [bTK])
            dv(lambda e: e.tensor_single_scalar(out=CAI, in_=CPOS.bitcast(I32), scalar=4, op=ALU.arith_shift_right), [bTK], [bTK])
            dv(lambda e: e.tensor_single_scalar(out=CBI, in_=CPOS.bitcast(I32), scalar=15, op=ALU.bitwise_and), [bTK], [bTK])
            self.CP("dve", CAF, CAI, [bTK], [bTK])
            self.CP("dve", CBF, CBI, [bTK], [bTK])
            for half, src in ((0, CAF), (1, CBF)):
                self.TT("dve", EQ.rearrange("p (k a) -> p k a", a=16), src[:, :, None].broadcast_to([128, 128, 16]),
                        self.IOTA[:, None, 0:16].broadcast_to([128, 128, 16]), ALU.is_equal, [bTK, self.bCONST], [bEQ])
                self.TT("dve", EQ.rearrange("p (h k a) -> p h k a", h=8, k=16), EQ.rearrange("p (h k a) -> p h k a", h=8, k=16),
                        TI4[:, :, half, None, :].broadcast_to([128, 8, 16, 16]), ALU.mult, [bEQ, bTK], [bEQ])
                dv(lambda e, half=half: e.tensor_reduce(out=IDX[half], in_=EQ.rearrange("p (k a) -> p k a", a=16), axis=AX.X, op=ALU.add), [bEQ], [bIDX])
            G3 = IDX[2].rearrange("p (h k) -> p h k", k=16)
            self.TT("dve", G3, CTOP, CTOP[:, :, 0:1].broadcast_to([128, 8, 16]), ALU.subtract, [bTK], [bIDX])
            self.ACT(IDX[2], IDX[2], AF.Exp, [bIDX], [bIDX])
            dv(lambda e: e.tensor_reduce(out=GSUM, in_=G3, axis=AX.X, op=ALU.add), [bIDX], [bTK])
            dv(lambda e: e.reciprocal(out=GSUM, in_=GSUM), [bTK], [bTK])
            self.TT("dve", G3, G3, GSUM[:, :, None].broadcast_to([128, 8, 16]), ALU.mult, [bIDX, bTK], [bIDX])
            for i in range(3):
                bk = 4 + i
                S.op("pe", lambda e, i=i, bk=bk: e.transpose(self.PSb(bk, 0, 128), IDX[i], self.IDENT[:]), [bIDX, self.bCONST], [bPS[bk]])
                self.CP("act" if i % 2 == 0 else "dve", IDT[i][:, tcs], self.PSb(bk, 0, 128), [bPS[bk]], [bIDT])
        for tb in range(32):
            t0 = tb * 8
            pb = 4 + (tb % 2) * 2
            iob = self.IOTA[:, None, :].broadcast_to([128, 8, 128])
            self.TT("dve", EQ1, iob, IDT[0][:, t0:t0 + 8, None].broadcast_to([128, 8, 128]), ALU.is_equal, [bIDT, self.bCONST], [bE1])
            self.TT("dve", OH1, EQ1, IDT[2][:, t0:t0 + 8, None].broadcast_to([128, 8, 128]), ALU.mult, [bE1, bIDT], [bO1])
            self.TT("dve", OH2, iob, IDT[1][:, t0:t0 + 8, None].broadcast_to([128, 8, 128]), ALU.is_equal, [bIDT, self.bCONST], [bO2])

            def gmm(e, pb=pb):
                ins = None
                for j in range(8):
                    ins = e.matmul(self.PS[:, pb * 512 + j * 128:pb * 512 + (j + 1) * 128], lhsT=OH1[:, j, :], rhs=OH2[:, j, :], start=True, stop=True)
                return ins
            S.op("pe", gmm, [bO1, bO2], [bPS[pb], bPS[pb + 1]])
            self.CP("act", G[:, t0:t0 + 4, :], self.PSb(pb).rearrange("p (a b) -> p a b", a=4), [bPS[pb]], [self.bG])
            self.CP("dve", G[:, t0 + 4:t0 + 8, :], self.PSb(pb + 1).rearrange("p (a b) -> p a b", a=4), [bPS[pb + 1]], [self.bG])
        UTS, VS = self.UTS[l], self.VS[l]

        def load_stage(s):
            sl = s % 2
            self.LD(self.UT[sl].rearrange("p a b c -> p (a b c)"), UTS[:, s * 4096:(s + 1) * 4096], self.bUT[sl], r=[self.bUV], w=[self.bUT[sl]])
            self.LD(self.VV[sl].rearrange("p a b -> p (a b)"), VS[:, s * 4096:(s + 1) * 4096], self.bVV[sl], r=[self.bUV], w=[self.bVV[sl]])

        def a_chain(i2):
            s, q = i2 // 4, i2 % 4
            bk = 4 + i2 % 4
            self.chain(self.PSb(bk, 0, 256), [(self.UT[s % 2][:, q, cc, :], HFT[:, cc, :]) for cc in range(8)], [self.bUT[s % 2], self.bHFT], [bPS[bk]])
        for bk in range(4):
            self.chain(self.PSb(bk), [(self.ZEROB[:], G[:, 0:4, :].rearrange("p a b -> p (a b)"))], [self.bCONST, self.bG], [bPS[bk]])
        load_stage(0)
        a_chain(0)
        for i2 in range(128):
            s, q = i2 // 4, i2 % 4
            if q == 0 and s + 1 < 32:
                load_stage(s + 1)
            if i2 + 1 < 128:
                a_chain(i2 + 1)
            bk = 4 + i2 % 4
            a = i2 % 2
            self.ACT(AG[a], self.PSb(bk, 0, 256), AF.Gelu, [bPS[bk]], [bAG[a]])
            self.TT("dve", WW[a], AG[a], G[:, :, i2], ALU.mult, [bAG[a], self.bG], [bWW[a]])

            def ymm(e, i2=i2, s=s, q=q, a=a):
                ins = None
                for dc in range(8):
                    ins = e.matmul(self.PS[:, dc * 256:(dc + 1) * 256], lhsT=self.VV[s % 2][:, q, dc * 128:(dc + 1) * 128], rhs=WW[a],
                                   start=False, stop=True)
                return ins
            S.op("pe", ymm, [self.bVV[s % 2], bWW[a]], bPS[0:4])
        YT = self.cv(AB, 0, [128, 8, 256], F32)
        for dc in range(8):
            if dc % 2 == 0:
                self.TS("dve", YT[:, dc, :], self.PS[:, dc * 256:(dc + 1) * 256], self.mod(5, dc), None, ALU.mult, None, [bPS[dc // 2], self.bCUR], [self.bUT[0]])
            else:
                self.ACT(YT[:, dc, :], self.PS[:, dc * 256:(dc + 1) * 256], AF.Identity, [bPS[dc // 2], self.bCUR], [self.bUT[0]], scale=self.mod(5, dc))
        self.TT("dve", self.Xt[:, :, tsl], self.Xt[:, :, tsl], YT, ALU.add, [self.bUT[0]] + bXa, bXa)


_CACHE = {}


def _prep_weights(w_ada, b_ada, w_in, lb_logits, hg_norm_g, w_up_a, w_up_b, w_o, w_pq, sub_keys, peer_u, peer_v, ln_g, ln_b):
    nl = w_ada.shape[0]
    f = lambda a: np.ascontiguousarray(np.asarray(a, dtype=np.float32))
    d = {}
    d["w_ada"] = f(w_ada); d["w_in"] = f(w_in); d["w_up_a"] = f(w_up_a); d["w_up_b"] = f(w_up_b)
    d["w_o"] = f(w_o); d["w_pq"] = f(w_pq)
    d["bada"] = f(np.asarray(b_ada).reshape(nl, 48, 128).transpose(2, 0, 1))
    d["lbl"] = f(np.asarray(lb_logits).reshape(nl, 4, 128).transpose(2, 0, 1))
    d["hgg"] = f(np.asarray(hg_norm_g).reshape(nl, 4, 128).transpose(2, 0, 1))
    d["lng"] = f(np.asarray(ln_g).reshape(nl, 2, 8, 128).transpose(3, 0, 1, 2))
    d["lnb"] = f(np.asarray(ln_b).reshape(nl, 2, 8, 128).transpose(3, 0, 1, 2))
    d["skT"] = f(np.asarray(sub_keys).transpose(0, 1, 2, 4, 3))
    d["uT"] = f(np.asarray(peer_u).reshape(nl, 128, 128, 8, 128).transpose(0, 4, 2, 3, 1)).reshape(nl, 128, 128 * 1024)
    d["pv"] = f(peer_v).reshape(nl, 128, 128 * 1024)
    return d


def kernel(x, c, w_ada, b_ada, w_in, lb_logits, hg_norm_g, w_up_a, w_up_b, w_o, w_pq, sub_keys, peer_u, peer_v, ln_g, ln_b):
    x = np.asarray(x, dtype=np.float32)
    c = np.asarray(c, dtype=np.float32)
    B = x.shape[0]
    nseq = B // NCORES
    key = ("full", nseq)
    if key not in _CACHE:
        _CACHE[key] = MK(nseq=nseq, nl=DEPTH)
    mk = _CACHE[key]
    wd = _prep_weights(w_ada, b_ada, w_in, lb_logits, hg_norm_g, w_up_a, w_up_b, w_o, w_pq, sub_keys, peer_u, peer_v, ln_g, ln_b)
    in_maps = []
    for i in range(NCORES):
        xs = x[i * nseq:(i + 1) * nseq]
        xT = np.ascontiguousarray(xs.reshape(nseq, S_LEN, 8, 128).transpose(0, 3, 2, 1))
        cs = c[i * nseq:(i + 1) * nseq]
        cT = np.ascontiguousarray(cs.reshape(nseq, 8, 128).transpose(2, 1, 0))
        m = dict(wd)
        m["xT"] = xT
        m["cT"] = cT
        in_maps.append(m)
    res = run_bass_kernel_spmd(mk.nc, in_maps, core_ids=list(range(NCORES)))
    out = np.empty((B, S_LEN, D), dtype=np.float32)
    for i in range(NCORES):
        oT = np.asarray(res.results[i]["outT"])
        out[i * nseq:(i + 1) * nseq] = oT.transpose(0, 3, 2, 1).reshape(nseq, S_LEN, D)
    return out
```

```python
from contextlib import ExitStack
import numpy as np
import concourse.bass as bass
import concourse.mybir as mybir
from concourse.bass_utils import run_bass_kernel_spmd

F32 = mybir.dt.float32
BF16 = mybir.dt.bfloat16
U32 = mybir.dt.uint32
I32 = mybir.dt.int32
U8 = mybir.dt.uint8
AF = mybir.ActivationFunctionType
ALU = mybir.AluOpType
AX = mybir.AxisListType

D = 1024
S_LEN = 2048
NCORES = 8
DEPTH = 4
BATCH = 32
ALPHA = (2.0 * DEPTH) ** 0.25
LN_EPS = 1e-5
RMS_EPS = 1e-6
IN_W = 5632
OFF_HG = 0
OFF_QK = OFF_HG + 4 * 4096
OFF_SV = OFF_QK + 4 * 2048
OFF_MG = OFF_SV + 4096
OFF_WO = OFF_MG + 8 * 3072
OFF_PQ = OFF_WO + 8 * 1024
NWS = OFF_PQ + 8192


class Buf:
    def __init__(self, S, name=""):
        self.name = name
        self.w = None
        self.r = {}
        S.bufs.append(self)


class Sched:
    def __init__(self, nc):
        self.nc = nc
        self.eng = {"pe": nc.tensor, "act": nc.scalar, "dve": nc.vector, "pool": nc.gpsimd, "sp": nc.sync}
        self.same = {"act", "dve", "pool"}
        self.es = ExitStack()
        self.semh = {}
        self.cnt = {}
        self.waited = {e: {} for e in self.eng}
        self.bufs = []
        self.n = 0
        self.pool = [self.es.enter_context(nc.semaphore("sp%d" % i)) for i in range(48)]
        self.npool = 0
        for e in self.eng:
            self._sem(e)

    def sb(self, name, shape, dt):
        return self.es.enter_context(self.nc.sbuf_tensor(name, list(shape), dt))

    def ps(self, name, shape, dt):
        return self.es.enter_context(self.nc.psum_tensor(name, list(shape), dt))

    def _sem(self, key):
        if key not in self.semh:
            self.semh[key] = self.pool[self.npool]
            self.npool += 1
            self.cnt[key] = 0
        return self.semh[key]

    def _waits(self, e, reads, writes):
        waits = {}

        def need(k, v):
            if waits.get(k, 0) < v:
                waits[k] = v
        for b in reads:
            if b.w is not None:
                need(*b.w)
        for b in writes:
            if b.w is not None:
                need(*b.w)
            for k, v in b.r.items():
                if k != e:
                    need(k, v)
        eng = self.eng[e]
        for k, v in waits.items():
            if k == e and e not in self.same:
                continue
            if self.waited[e].get(k, 0) >= v:
                continue
            self.waited[e][k] = v
            eng.wait_ge(self.semh[k], v)
            self.n += 1

    def op(self, e, fn, reads=(), writes=()):
        self._sem(e)
        self._waits(e, reads, writes)
        ins = fn(self.eng[e])
        ins.then_inc(self.semh[e], 1)
        self.cnt[e] += 1
        idx = self.cnt[e]
        for b in reads:
            b.r[e] = idx
        for b in writes:
            b.w = (e, idx)
            b.r = {}
        self.n += 1

    def dma(self, q, fn, key_buf, reads=(), writes=()):
        key = ("dma", id(key_buf))
        self._sem(key)
        self._sem(q)
        self._waits(q, reads, writes)
        ins = fn(self.eng[q])
        ins.then_inc(self.semh[key], 16)
        self.cnt[key] += 16
        val = self.cnt[key]
        for b in reads:
            b.r[key] = val
        for b in writes:
            b.w = (key, val)
            b.r = {}
        self.n += 1

    def sync_all(self):
        nc = self.nc
        for key, h in self.semh.items():
            if isinstance(key, tuple) and self.cnt[key] > 0:
                nc.sync.wait_ge(h, self.cnt[key])
        nc.all_engine_barrier()
        for h in self.pool:
            nc.gpsimd.sem_clear(h)
        nc.all_engine_barrier()
        for k in self.cnt:
            self.cnt[k] = 0
        for e in self.waited:
            self.waited[e] = {}
        for b in self.bufs:
            b.w = None
            b.r = {}


class MK:
    def __init__(self, nseq=4, nl=4, hw_loops=True, ntiles=8, dbg=None, stop_after=None):
        self.nseq, self.nl, self.hw, self.ntiles, self.dbg, self.stop_after = nseq, nl, hw_loops, ntiles, dbg, stop_after
        self.nc = bass.Bass("TRN2", target_bir_lowering=False)
        self.S = Sched(self.nc)
        self.build()

    def B(self, name=""):
        return Buf(self.S, name)

    def chain(self, out, pairs, r, w, first=True, last=True):
        def fn(e):
            n = len(pairs)
            ins = None
            for i, (a, b) in enumerate(pairs):
                ins = e.matmul(out, lhsT=a, rhs=b, start=(first and i == 0), stop=(last and i == n - 1))
            return ins
        self.S.op("pe", fn, r, w)

    def ACT(self, out, in_, func, r, w, bias=None, scale=None):
        kw = {}
        if bias is not None:
            kw["bias"] = bias
        if scale is not None:
            kw["scale"] = scale
        self.S.op("act", lambda e: e.activation(out=out, in_=in_, func=func, **kw), r, w)

    def TT(self, eng, out, in0, in1, op, r, w):
        self.S.op(eng, lambda e: e.tensor_tensor(out=out, in0=in0, in1=in1, op=op), r, w)

    def TS(self, eng, out, in0, s1, s2, op0, op1, r, w):
        if s2 is None:
            self.S.op(eng, lambda e: e.tensor_scalar(out=out, in0=in0, scalar1=s1, scalar2=None, op0=op0), r, w)
        else:
            self.S.op(eng, lambda e: e.tensor_scalar(out=out, in0=in0, scalar1=s1, scalar2=s2, op0=op0, op1=op1), r, w)

    def STT(self, eng, out, in0, scalar, in1, op0, op1, r, w):
        self.S.op(eng, lambda e: e.scalar_tensor_tensor(out=out, in0=in0, scalar=scalar, in1=in1, op0=op0, op1=op1), r, w)

    def CP(self, eng, out, in_, r, w):
        if eng == "act":
            self.S.op("act", lambda e: e.copy(out=out, in_=in_), r, w)
        else:
            self.S.op(eng, lambda e: e.tensor_copy(out=out, in_=in_), r, w)

    def MS(self, eng, ap, val, w):
        self.S.op(eng, lambda e: e.memset(ap, val), (), w)

    def LD(self, out, in_, key, r=(), w=(), q="sp"):
        self.S.dma(q, lambda e: e.dma_start(out=out, in_=in_), key, r, w)

    def cv(self, arena, off, shape, dt):
        nb = {F32: 4, BF16: 2, U32: 4, I32: 4}[dt]
        n = int(np.prod(shape[1:])) * nb
        ap = arena[0:shape[0], off:off + n].bitcast(dt)
        if len(shape) == 3:
            ap = ap.rearrange("p (a b) -> p a b", a=shape[1])
        elif len(shape) == 4:
            ap = ap.rearrange("p (a b c) -> p a b c", a=shape[1], b=shape[2])
        return ap

    def run_loop(self, n, body):
        if self.hw and n > 1:
            with self.nc.Fori(0, n) as i:
                self.S.sync_all()
                body(i)
            self.S.sync_all()
        else:
            for i in range(n):
                self.S.sync_all()
                body(i)
            self.S.sync_all()

    def build(self):
        nc, S, nseq, nl = self.nc, self.S, self.nseq, self.nl
        dram = lambda name, shape, dt, kind: nc.dram_tensor(name, list(shape), dt, kind=kind).ap()
        self.xT = dram("xT", [nseq, 128, 8, S_LEN], F32, "ExternalInput")
        self.cT = dram("cT", [128, 8, nseq], F32, "ExternalInput")
        self.w_ada = dram("w_ada", [nl, D, 6 * D], F32, "ExternalInput")
        self.bada = dram("bada", [128, nl, 48], F32, "ExternalInput")
        self.w_in = dram("w_in", [nl, D, IN_W], F32, "ExternalInput")
        self.lbl = dram("lbl", [128, nl, 4], F32, "ExternalInput")
        self.hgg = dram("hgg", [128, nl, 4], F32, "ExternalInput")
        self.w_up_a = dram("w_up_a", [nl, 512, D], F32, "ExternalInput")
        self.w_up_b = dram("w_up_b", [nl, 512, D], F32, "ExternalInput")
        self.w_o = dram("w_o", [nl, D, D], F32, "ExternalInput")
        self.w_pq = dram("w_pq", [nl, D, D], F32, "ExternalInput")
        self.skT = dram("skT", [nl, 2, 8, 64, 128], F32, "ExternalInput")
        self.uT = dram("uT", [nl, 128, 128 * 1024], F32, "ExternalInput")
        self.pv = dram("pv", [nl, 128, 128 * 1024], F32, "ExternalInput")
        self.lng = dram("lng", [128, nl, 2, 8], F32, "ExternalInput")
        self.lnb = dram("lnb", [128, nl, 2, 8], F32, "ExternalInput")
        self.outT = dram("outT", [nseq, 128, 8, S_LEN], F32, "ExternalOutput")
        if self.dbg:
            self.dbg_out = dram("dbg_out", [128, 8, S_LEN], F32, "ExternalOutput")
        self.WSC = dram("wsc", [nl, 128, NWS], BF16, "Internal")
        self.UTS = dram("uts", [nl, 128, 128 * 1024], BF16, "Internal")
        self.VS = dram("vs", [nl, 128, 128 * 1024], BF16, "Internal")
        self.bWSC = self.B("wsc")
        self.bUV = self.B("uvs")
        self.Xt = S.sb("X", [128, 8, S_LEN], F32)
        self.bX = [self.B("X%d" % i) for i in range(4)]
        self.AA = S.sb("arenaA", [128, 65536], U8)
        self.AB = S.sb("arenaB", [128, 69 * 1024], U8)
        self.PS = S.ps("psum", [128, 4096], F32)
        self.bPS = [self.B("ps%d" % i) for i in range(8)]
        c = lambda n, sh, dt=F32: S.sb(n, sh, dt)
        self.IDENT = c("ident", [128, 128]); self.IOTA = c("iota", [128, 128])
        self.TRI = c("tri", [128, 128]); self.TRIC = c("tric", [128, 128])
        self.ONESD = c("onesd", [128, 128]); self.ONESH = c("onesh", [128, 128])
        self.MASKH = c("maskh", [128, 128]); self.MASKU = c("masku", [128, 128], U32)
        self.ZEROB = c("zerob", [128, 128], BF16)
        self.MODT = c("modt", [128, nseq, nl, 48])
        self.LNG = c("lngs", [128, nl, 2, 8]); self.LNB = c("lnbs", [128, nl, 2, 8])
        self.HGG = c("hggs", [128, nl, 4]); self.LBT = c("lbt", [128, nl, 4, 3])
        self.CUR = c("cur", [128, 48 + 16 + 16 + 4 + 12])
        self.bCUR = self.B("cur")
        self.bCONST = self.B("const")
        self.prologue()
        S.sync_all()
        if self.stop_after == "pro":
            return
        self.run_loop(nseq, self.seq_body)

    def PSb(self, i, lo=0, hi=512):
        return self.PS[:, i * 512 + lo:i * 512 + hi]

    def prologue(self):
        nc, S, nseq, nl = self.nc, self.S, self.nseq, self.nl
        bC = self.bCONST
        io = lambda out, base, cm, pat: S.op("pool", lambda e: e.iota(out, pattern=pat, base=base, channel_multiplier=cm,
                                                                     allow_small_or_imprecise_dtypes=True), (), [bC])
        io(self.IOTA[:], 0, 0, [[1, 128]])
        io(self.IDENT[:], 0, -1, [[1, 128]])
        self.TS("dve", self.IDENT[:], self.IDENT[:], 0.0, None, ALU.is_equal, None, [bC], [bC])
        io(self.TRI[:], 0, 1, [[-1, 128]])
        self.TS("dve", self.TRIC[:], self.TRI[:], 0.0, None, ALU.is_le, None, [bC], [bC])
        self.TS("dve", self.TRI[:], self.TRI[:], 0.0, None, ALU.is_gt, None, [bC], [bC])
        self.MS("pool", self.ZEROB[:], 0.0, [bC])
        self.MS("pool", self.ONESD[:], 1.0 / D, [bC])
        self.MS("pool", self.ONESH[:], 1.0 / 128, [bC])
        io(self.MASKH[:], 0, -1, [[1, 128]])
        self.TS("dve", self.MASKH[:], self.MASKH[:], 0.0, None, ALU.is_ge, None, [bC], [bC])
        self.MS("dve", self.MASKH[0:64, 64:128], 0.0, [bC])
        self.CP("dve", self.MASKU[:], self.MASKH[:], [bC], [bC])
        self.LD(self.LNG[:], self.lng, bC, w=[bC]); self.LD(self.LNB[:], self.lnb, bC, w=[bC])
        self.LD(self.HGG[:], self.hgg, bC, w=[bC])
        STI = [self.AA[:, i * 16384:(i + 1) * 16384].bitcast(F32) for i in range(2)]
        STO = [self.AA[:, 32768 + i * 8192:32768 + (i + 1) * 8192].bitcast(BF16) for i in range(2)]
        bSTI = [self.B("sti0"), self.B("sti1")]
        bSTO = [self.B("sto0"), self.B("sto1")]
        jobs = []
        for l in range(nl):
            W = self.WSC[l]
            win = self.w_in[l].rearrange("(c p) j -> p c j", p=128)
            wua = self.w_up_a[l].rearrange("(h p) j -> p h j", p=128)
            wub = self.w_up_b[l].rearrange("(h p) j -> p h j", p=128)
            wo = self.w_o[l].rearrange("(c p) j -> p c j", p=128)
            wpq = self.w_pq[l].rearrange("(c p) j -> p c j", p=128)

            def part(off, n_c, w, j0, src):
                return (lambda st, off=off, n_c=n_c, w=w, j0=j0: st[:, off:off + n_c * w].rearrange("p (c j) -> p c j", c=n_c)[:, :, j0:j0 + src.shape[2]], src)
            for h in range(4):
                jobs.append((W[:, OFF_HG + h * 4096:OFF_HG + (h + 1) * 4096], 4096,
                             [part(0, 8, 512, k * 128, win[:, :, k * 512 + h * 128:k * 512 + (h + 1) * 128]) for k in range(4)], self.bWSC))
            for hp in range(4):
                jobs.append((W[:, OFF_QK + hp * 2048:OFF_QK + (hp + 1) * 2048], 2048,
                             [part(0, 8, 256, k * 128, win[:, :, 2048 + k * 512 + hp * 128:2048 + k * 512 + (hp + 1) * 128]) for k in range(2)], self.bWSC))
            jobs.append((W[:, OFF_SV:OFF_SV + 4096], 4096, [part(0, 8, 512, 0, win[:, :, 3072:3584])], self.bWSC))
            for j in range(8):
                jobs.append((W[:, OFF_MG + j * 3072:OFF_MG + (j + 1) * 3072], 3072,
                             [part(0, 8, 128, 0, win[:, :, 3584 + j * 128:3584 + (j + 1) * 128]),
                              part(1024, 8, 128, 0, win[:, :, 4608 + j * 128:4608 + (j + 1) * 128]),
                              part(2048, 4, 128, 0, wua[:, :, j * 128:(j + 1) * 128]),
                              part(2560, 4, 128, 0, wub[:, :, j * 128:(j + 1) * 128])], self.bWSC))
            for off, wsrc in ((OFF_WO, wo), (OFF_PQ, wpq)):
                for jj in range(2):
                    jobs.append((W[:, off + jj * 4096:off + (jj + 1) * 4096], 4096,
                                 [part(q * 1024, 8, 128, 0, wsrc[:, :, (jj * 4 + q) * 128:(jj * 4 + q + 1) * 128]) for q in range(4)], self.bWSC))
            if self.dbg not in ("oa", "ob", "ln1", "nouv", "hm", "proj", "ew", "blk"):
                for k in range(32):
                    sl = slice(k * 4096, (k + 1) * 4096)
                    jobs.append((self.UTS[l][:, sl], 4096, [(lambda st: st[:, 0:4096], self.uT[l][:, sl])], self.bUV))
                    jobs.append((self.VS[l][:, sl], 4096, [(lambda st: st[:, 0:4096], self.pv[l][:, sl])], self.bUV))

        def job_load(i):
            dst, n, parts, db = jobs[i]
            for vf, src in parts:
                self.LD(vf(STI[i % 2]), src, bSTI[i % 2], w=[bSTI[i % 2]])

        def job_rest(i):
            dst, n, parts, db = jobs[i]
            eng = ("act", "dve", "pool")[i % 3]
            self.CP(eng, STO[i % 2][:, 0:n], STI[i % 2][:, 0:n], [bSTI[i % 2]], [bSTO[i % 2]])
            self.LD(dst, STO[i % 2][:, 0:n], bSTO[i % 2], r=[bSTO[i % 2]], w=[db])
        if jobs:
            job_load(0)
            for i in range(len(jobs)):
                if i + 1 < len(jobs):
                    job_load(i + 1)
                job_rest(i)
        LBL = self.cv(self.AB, 0, [128, nl, 4], F32)
        LBE = self.cv(self.AB, 256, [128, nl, 4], F32)
        LBM = self.cv(self.AB, 512, [128, 4], F32)
        bT = self.B("lbtmp")
        self.LD(LBL, self.lbl, bT, w=[bT])
        S.op("dve", lambda e: e.tensor_reduce(out=LBM, in_=LBL.rearrange("p l h -> p h l"), axis=AX.X, op=ALU.max), [bT], [bT])
        self.TT("dve", LBE, LBL, LBM[:, None, :].broadcast_to([128, nl, 4]), ALU.subtract, [bT], [bT])
        self.ACT(LBE, LBE, AF.Exp, [bT], [bT])
        S.op("dve", lambda e: e.tensor_reduce(out=LBM, in_=LBE.rearrange("p l h -> p h l"), axis=AX.X, op=ALU.add), [bT], [bT])
        S.op("dve", lambda e: e.reciprocal(out=LBM, in_=LBM), [bT], [bT])
        self.TT("dve", LBE, LBE, LBM[:, None, :].broadcast_to([128, nl, 4]), ALU.mult, [bT], [bT])
        self.MS("dve", self.LBT[:, 0, :, 0], 0.0, [bC])
        for l in range(1, nl):
            self.TT("dve", self.LBT[:, l, :, 0], self.LBT[:, l - 1, :, 0], LBE[:, l, :], ALU.add, [bT, bC], [bC])
        self.TS("dve", self.LBT[:, :, :, 1], self.LBT[:, :, :, 0], -1.0, 1.0, ALU.mult, ALU.add, [bC], [bC])
        self.TS("dve", self.LBT[:, :, :, 2], self.LBT[:, :, :, 0], -1.0, None, ALU.add, None, [bC], [bC])
        COND = self.cv(self.AB, 1024, [128, 8, nseq], F32)
        BADA = self.cv(self.AB, 2048, [128, nl, 48], F32)
        bCo = self.B("cond")
        self.LD(COND, self.cT, bCo, w=[bCo])
        self.LD(BADA, self.bada, bCo, w=[bCo])
        self.ACT(COND, COND, AF.Silu, [bCo], [bCo])
        WA = [self.cv(self.AB, 8192 + i * 16384, [128, 8, 512], F32) for i in range(2)]
        bWA = [self.B("wa0"), self.B("wa1")]
        it = 0
        for l in range(nl):
            wa = self.w_ada[l].rearrange("(c p) j -> p c j", p=128)
            for g in range(12):
                s = it % 2
                self.LD(WA[s], wa[:, :, g * 512:(g + 1) * 512], bWA[s], w=[bWA[s]])
                bank = self.PSb(it % 4, 0, 4 * nseq)
                for jl in range(4):
                    self.chain(bank[:, jl * nseq:(jl + 1) * nseq],
                               [(WA[s][:, cc, jl * 128:(jl + 1) * 128], COND[:, cc, :]) for cc in range(8)],
                               [bWA[s], bCo], [self.bPS[it % 4]])
                self.TT("dve", self.MODT[:, :, l, g * 4:(g + 1) * 4], bank.rearrange("p (j b) -> p b j", b=nseq),
                        BADA[:, l, g * 4:(g + 1) * 4][:, None, :].broadcast_to([128, nseq, 4]), ALU.add,
                        [self.bPS[it % 4], bCo], [bC])
                it += 1
        for k in (1, 4):
            self.TS("dve", self.MODT[:, :, :, k * 8:(k + 1) * 8], self.MODT[:, :, :, k * 8:(k + 1) * 8], 1.0, None, ALU.add, None, [bC], [bC])
        for k in (2, 5):
            self.TS("dve", self.MODT[:, :, :, k * 8:(k + 1) * 8], self.MODT[:, :, :, k * 8:(k + 1) * 8], 1.0 / ALPHA, None, ALU.mult, None, [bC], [bC])

    def seq_body(self, b):
        self.b = b
        for cc in range(8):
            self.LD(self.Xt[:, cc, :], self.xT[b][:, cc, :], self.bX[0], w=self.bX)
        for l in range(self.nl):
            self.S.sync_all()
            self.layer_body(l)
        self.S.sync_all()
        for cc in range(8):
            self.LD(self.outT[b][:, cc, :], self.Xt[:, cc, :], self.bX[0], r=self.bX)
        if self.dbg:
            pass

    def layer_body(self, l):
        self.l = l
        b = self.b
        C = self.CUR
        bc = self.bCUR
        self.CP("pool", C[:, 0:48], self.MODT[:, b, l, :], [self.bCONST], [bc])
        self.CP("dve", C[:, 48:64], self.LNG[:, l].rearrange("p a b -> p (a b)"), [self.bCONST], [bc])
        self.CP("dve", C[:, 64:80], self.LNB[:, l].rearrange("p a b -> p (a b)"), [self.bCONST], [bc])
        self.CP("dve", C[:, 80:84], self.HGG[:, l, :], [self.bCONST], [bc])
        self.CP("dve", C[:, 84:96], self.LBT[:, l].rearrange("p a b -> p (a b)"), [self.bCONST], [bc])
        self.phase1()
        if self.stop_after == "p1":
            return
        self.S.sync_all()
        self.phase2()

    def mod(self, k, cc):
        return self.CUR[:, k * 8 + cc:k * 8 + cc + 1]

    def layer_norm(self, which, base):
        SQ = self.cv(self.AB, base, [128, 8, 512], F32)
        MEAN = self.cv(self.AB, base + 16384, [128, 512], F32)
        M2 = self.cv(self.AB, base + 18432, [128, 512], F32)
        RSTD = self.cv(self.AB, base + 20480, [128, 512], F32)
        bSQ, bM, bM2, bR = self.B(), self.B(), self.B(), self.B()
        for tt in range(4):
            bx = self.bX[tt]
            cs = slice(tt * 512, (tt + 1) * 512)
            Xv = self.Xt[:, :, cs]
            self.ACT(SQ, Xv, AF.Square, [bx], [bSQ])
            pm, pq = (0, 1) if tt % 2 == 0 else (2, 3)
            self.chain(self.PSb(pm), [(self.ONESD[:], self.Xt[:, cc, cs]) for cc in range(8)], [bx, self.bCONST], [self.bPS[pm]])
            self.chain(self.PSb(pq), [(self.ONESD[:], SQ[:, cc, :]) for cc in range(8)], [bSQ, self.bCONST], [self.bPS[pq]])
            self.CP("act", MEAN, self.PSb(pm), [self.bPS[pm]], [bM])
            self.TT("dve", M2, MEAN, MEAN, ALU.mult, [bM], [bM2])
            self.TT("dve", M2, self.PSb(pq), M2, ALU.subtract, [self.bPS[pq], bM2], [bM2])
            self.ACT(M2, M2, AF.Ln, [bM2], [bM2], bias=LN_EPS / (ALPHA * ALPHA))
            self.ACT(RSTD, M2, AF.Exp, [bM2], [bR], scale=-0.5)
            self.TT("dve", Xv, Xv, MEAN[:, None, :].broadcast_to([128, 8, 512]), ALU.subtract, [bx, bM], [bx])
            self.TT("pool", Xv, Xv, RSTD[:, None, :].broadcast_to([128, 8, 512]), ALU.mult, [bx, bR], [bx])
            for cc in range(8):
                g = self.CUR[:, 48 + which * 8 + cc:48 + which * 8 + cc + 1]
                bb = self.CUR[:, 64 + which * 8 + cc:64 + which * 8 + cc + 1]
                self.TS("dve" if cc % 2 == 0 else "pool", self.Xt[:, cc, cs], self.Xt[:, cc, cs], g, bb, ALU.mult, ALU.add, [bx, self.bCUR], [bx])

    def phase1(self):
        nc, S = self.nc, self.S
        l = self.l
        AA, AB = self.AA, self.AB
        HM = self.cv(AA, 0, [128, 8, S_LEN], BF16); bHM = self.B("hm")
        OA = self.cv(AA, 32768, [128, 4, S_LEN], BF16); bOA = self.B("oa")
        OB = self.cv(AA, 49152, [128, 4, S_LEN], BF16); bOB = self.B("ob")
        WS = [AB[:, 0:8192].bitcast(BF16), AB[:, 8192:16384].bitcast(BF16)]
        bWS = [self.B("ws0"), self.B("ws1")]
        self.ws_i = 0
        W = self.WSC[l]
        bPS = self.bPS

        def load_ws(off, n):
            s = self.ws_i % 2
            self.ws_i += 1
            o = 0
            for piece in (4096, 2048, 1024):
                while n - o >= piece:
                    self.LD(WS[s][:, o:o + piece], W[:, off + o:off + o + piece], bWS[s], r=[self.bWSC], w=[bWS[s]])
                    o += piece
            assert o == n
            return WS[s], bWS[s]

        for cc in range(8):
            if cc % 2 == 0:
                self.ACT(HM[:, cc, :], self.Xt[:, cc, :], AF.Identity, self.bX + [self.bCUR], [bHM], bias=self.mod(0, cc), scale=self.mod(1, cc))
            else:
                self.TS("dve", HM[:, cc, :], self.Xt[:, cc, :], self.mod(1, cc), self.mod(0, cc), ALU.mult, ALU.add, self.bX + [self.bCUR], [bHM])

        if self.dbg == "hm":
            self.dump_bf(HM, [bHM], 8)
            return
        SC = 16384
        P = [self.cv(AB, SC + i * 8192, [128, S_LEN], F32) for i in range(4)]
        bP = [self.B("P%d" % i) for i in range(4)]
        VH = self.cv(AB, SC + 32768, [128, 16, 128], F32); bVH = self.B("vh")
        RM = self.cv(AB, SC + 40960, [128, S_LEN], F32); bRM = self.B("rm")
        MI = SC + 49152
        SCM = [self.cv(AB, MI + i * 512, [128, 128], F32) for i in range(2)]; bSCM = [self.B(), self.B()]
        KDT = [self.cv(AB, MI + 1024 + i * 512, [128, 128], F32) for i in range(2)]; bKDT = [self.B(), self.B()]
        ST = [self.cv(AB, MI + 2048 + i * 512, [128, 128], F32) for i in range(2)]; bST = [self.B(), self.B()]
        EBE = self.cv(AB, MI + 3072, [128, 32], F32); bEBE = self.B()
        BMD = self.cv(AB, MI + 3200, [128, 32], F32)
        self.MS("pool", RM, 1.0, [bRM])
        self.MS("pool", RM.rearrange("p (a b) -> p a b", b=64)[:, :, 0:1], 0.0, [bRM])
        SCP = [self.PSb(i, 0, 128) for i in range(2)]; bSCP = [self.bPS[0], self.bPS[1]]
        KTP = [self.PSb(2 + i, 0, 128) for i in range(2)]; bKTP = [self.bPS[2], self.bPS[3]]
        STP = [self.PSb(6 + i, 0, 128) for i in range(2)]; bSTP = [self.bPS[6], self.bPS[7]]
        pr = [0]

        def proj_fm(ws, bws, col0, evac):
            for tt in range(4):
                bk = pr[0] % 4
                pr[0] += 1
                self.chain(self.PSb(bk), [(ws.rearrange("p (c j) -> p c j", c=8)[:, cc, col0:col0 + 128], HM[:, cc, tt * 512:(tt + 1) * 512]) for cc in range(8)],
                           [bws, bHM], [bPS[bk]])
                evac(tt, self.PSb(bk), bPS[bk])

        for h in range(4):
            ws, bws = load_ws(OFF_HG + h * 4096, 4096)
            ws3 = ws[:, 0:4096].rearrange("p (c j) -> p c j", c=8)
            lb = self.CUR[:, 84 + h * 3:84 + h * 3 + 1]
            omlb = self.CUR[:, 84 + h * 3 + 1:84 + h * 3 + 2]
            nomlb = self.CUR[:, 84 + h * 3 + 2:84 + h * 3 + 3]
            wsf = ws[:, 0:4096]
            proj_fm(wsf, bws, 0, lambda tt, ps, bps: self.CP("act", P[0][:, tt * 512:(tt + 1) * 512], ps, [bps], [bP[0]]))
            proj_fm(wsf, bws, 128, lambda tt, ps, bps: self.ACT(P[1][:, tt * 512:(tt + 1) * 512], ps, AF.Sigmoid, [bps], [bP[1]]))
            for g in range(4):
                bk = pr[0] % 4
                pr[0] += 1
                for j in range(4):
                    t = g * 4 + j
                    self.chain(self.PSb(bk, j * 128, (j + 1) * 128), [(HM[:, cc, t * 128:(t + 1) * 128], ws3[:, cc, 256:384]) for cc in range(8)],
                               [bws, bHM], [bPS[bk]])
                self.CP("dve", VH[:, g * 4:(g + 1) * 4, :], self.PSb(bk).rearrange("p (a b) -> p a b", a=4), [bPS[bk]], [bVH])
            if self.dbg == "proj":
                self.S.sync_all()
                for i in range(2):
                    self.LD(self.dbg_out[:, i, :], P[i], bP[i], r=[bP[i]])
                self.LD(self.dbg_out[:, 2, :], VH.rearrange("p a b -> p (a b)"), bVH, r=[bVH])
                return
            self.TS("dve", P[2], P[1], nomlb, omlb, ALU.mult, ALU.add, [bP[1], self.bCUR], [bP[2]])
            self.TS("dve", P[1], P[1], omlb, lb, ALU.mult, ALU.add, [bP[1], self.bCUR], [bP[1]])
            self.ACT(P[1], P[1], AF.Ln, [bP[1]], [bP[1]])
            S.op("dve", lambda e: e.tensor_tensor_scan(out=P[3], data0=RM, data1=P[1], initial=0.0, op0=ALU.mult, op1=ALU.add), [bRM, bP[1]], [bP[3]])
            P3v = P[3].rearrange("p (a b) -> p a b", b=64)
            self.CP("dve", BMD, P3v[:, :, 31], [bP[3]], [bEBE])
            self.TT("dve", P3v, P3v, BMD[:, :, None].broadcast_to([128, 32, 64]), ALU.subtract, [bP[3], bEBE], [bP[3]])
            self.TS("dve", P[3], P[3], -80.0, 80.0, ALU.max, ALU.min, [bP[3]], [bP[3]])
            self.TT("dve", EBE[:, 0:31], P3v[:, 0:31, 63], BMD[:, 1:32], ALU.add, [bP[3], bEBE], [bEBE])
            self.CP("dve", EBE[:, 31:32], P3v[:, 31:32, 63], [bP[3]], [bEBE])
            self.ACT(EBE, EBE, AF.Exp, [bEBE], [bEBE])
            self.ACT(P[1], P[3], AF.Exp, [bP[3]], [bP[1]])
            self.TT("pool", P[0], P[0], P[1], ALU.mult, [bP[0], bP[1]], [bP[0]])
            self.ACT(P[1], P[3], AF.Exp, [bP[3], bP[1]], [bP[1]], scale=-1.0)
            self.TT("pool", P[2], P[2], P[1], ALU.mult, [bP[2], bP[1]], [bP[2]])
            if self.dbg == "ew":
                self.S.sync_all()
                for i in range(4):
                    self.LD(self.dbg_out[:, i, :], P[i], bP[i], r=[bP[i]])
                return
            self.MS("dve", ST[0], 0.0, [bST[0]])
            cur = 0

            def pre(blk):
                import os as _os
                _pm = _os.environ.get("PRE_MODE", "")
                s = blk % 2
                cs = slice(blk * 128, (blk + 1) * 128)
                if "a" in _pm:
                    s = 0
                if "b" in _pm:
                    cs = slice(0, 128)
                sp_, sm_ = s, s
                if "c" in _pm:
                    sm_ = 0
                if "d" in _pm:
                    sp_ = 0
                self.chain(SCP[sp_], [(P[2][:, cs], P[0][:, cs])], [bP[2], bP[0]], [bSCP[sp_]])
                if "t" not in _os.environ.get("BLK_FL", "t"):
                    self.TT("dve", SCM[sm_], SCP[sp_], self.MASKH[:], ALU.mult, [bSCP[sp_], self.bCONST], [bSCM[sm_]])
                    return
                import os as _os
                if "t" in _os.environ.get("BLK_FL", "t"):
                    S.op("pe", lambda e: e.transpose(KTP[s], P[2][:, cs], self.IDENT[:]), [bP[2], self.bCONST], [bKTP[s]])
                    self.CP("act", KDT[s], KTP[s], [bKTP[s]], [bKDT[s]])
                self.MS("pool", SCM[s], 0.0, [bSCM[s]])
                S.op("dve", lambda e: e.copy_predicated(out=SCM[s], mask=self.MASKU[:], data=SCP[s]), [bSCP[s], self.bCONST, bSCM[s]], [bSCM[s]])
            pre(0)
            import os as _os
            _NB = int(_os.environ.get("BLK_N", "16")); _FL = _os.environ.get("BLK_FL", "osu")
            for blk in range(_NB):
                s = blk % 2
                if blk + 1 < 16:
                    pre(blk + 1)
                ob = 4 + (blk // 4) % 2
                for ch in range(2):
                    c0 = blk * 128 + ch * 64
                    oc = (blk % 4) * 128 + ch * 64
                    rows = slice(ch * 64, (ch + 1) * 64)
                    if "o" in _FL:
                        self.chain(self.PSb(ob, oc, oc + 64),
                               [(ST[cur], P[0][:, c0:c0 + 64]), (VH[:, blk, :], SCM[s][:, ch * 64:(ch + 1) * 64])],
                               [bST[cur], bP[0], bVH, bSCM[s]], [bPS[ob]])
                    if "s" not in _FL:
                        continue
                    self.chain(STP[cur], [(self.IDENT[:], ST[cur]), (KDT[s][rows, :], VH[rows, blk, :])],
                               [self.bCONST, bST[cur], bKDT[s], bVH], [bSTP[cur]])
                    eb = EBE[:, 2 * blk + ch:2 * blk + ch + 1]
                    if "u" not in _FL:
                        continue
                    if ch == 0:
                        self.ACT(ST[1 - cur], STP[cur], AF.Identity, [bSTP[cur], bEBE], [bST[1 - cur]], scale=eb)
                    else:
                        self.TS("dve", ST[1 - cur], STP[cur], eb, None, ALU.mult, None, [bSTP[cur], bEBE], [bST[1 - cur]])
                    cur = 1 - cur
                if blk % 4 == 3 and "o" in _FL:
                    tt = blk // 4
                    self.CP("act", P[3][:, tt * 512:(tt + 1) * 512], self.PSb(ob), [bPS[ob]], [bP[3]])
            if self.dbg == "blk":
                self.S.sync_all()
                self.LD(self.dbg_out[:, 0, :], P[3], bP[3], r=[bP[3]])
                return
            self.ACT(P[1], P[3], AF.Square, [bP[3]], [bP[1]])
            for tt in range(4):
                bk = pr[0] % 4
                pr[0] += 1
                cs = slice(tt * 512, (tt + 1) * 512)
                self.chain(self.PSb(bk), [(self.ONESH[:], P[1][:, cs])], [bP[1], self.bCONST], [bPS[bk]])
                self.ACT(P[2][:, cs], self.PSb(bk), AF.Ln, [bPS[bk]], [bP[2]], bias=RMS_EPS)
            self.ACT(P[2], P[2], AF.Exp, [bP[2]], [bP[2]], scale=-0.5)
            proj_fm(wsf, bws, 384, lambda tt, ps, bps: self.ACT(P[0][:, tt * 512:(tt + 1) * 512], ps, AF.Silu, [bps], [bP[0]]))
            self.STT("dve", P[1], P[3], self.CUR[:, 80 + h:81 + h], P[2], ALU.mult, ALU.mult, [bP[3], bP[2], self.bCUR], [bP[1]])
            self.TT("pool", OA[:, h, :], P[1], P[0], ALU.mult, [bP[1], bP[0]], [bOA])

        if self.dbg == "oa":
            self.dump_bf(OA, [bOA], 4)
            return
        QT2 = self.cv(AB, SC, [128, S_LEN], BF16); KT2 = self.cv(AB, SC + 4096, [128, S_LEN], BF16)
        VSB = self.cv(AB, SC + 8192, [128, 16, 512], BF16)
        bQK = bP[0]
        bVS = bP[1]
        bST2 = [bP[2], bP[3]]
        EE = [self.cv(AB, SC + 24576 + i * 2048, [128, 512], F32) for i in range(2)]
        SPp = [self.cv(AB, SC + 28672 + i * 2048, [128, 512], F32) for i in range(2)]
        LW = [self.cv(AB, SC + 32768 + i * 2048, [128, 512], F32) for i in range(2)]
        WT = [self.cv(AB, SC + 36864 + i * 1024, [128, 512], BF16) for i in range(2)]
        bEE = [bVH, bRM]; bSP = [bSCM[0], bSCM[1]]; bLW = [bKDT[0], bKDT[1]]; bWT = [bST[0], bST[1]]
        S.sync_all()
        ws, bws = load_ws(OFF_SV, 4096)
        ws3 = ws[:, 0:4096].rearrange("p (c j) -> p c j", c=8)
        for t in range(16):
            bk = 4 + t % 4
            self.chain(self.PSb(bk), [(HM[:, cc, t * 128:(t + 1) * 128], ws3[:, cc, :]) for cc in range(8)], [bws, bHM], [bPS[bk]])
            self.CP("act" if t % 2 == 0 else "dve", VSB[:, t, :], self.PSb(bk), [bPS[bk]], [bVS])
        step = [0]
        for hp in range(4):
            ws, bws = load_ws(OFF_QK + hp * 2048, 2048)
            wsf = ws[:, 0:2048]
            for k, dst in ((0, QT2), (1, KT2)):
                for tt in range(4):
                    bk = 4 + pr[0] % 2
                    pr[0] += 1
                    self.chain(self.PSb(bk), [(wsf.rearrange("p (c j) -> p c j", c=8)[:, cc, k * 128:(k + 1) * 128], HM[:, cc, tt * 512:(tt + 1) * 512]) for cc in range(8)],
                               [bws, bHM], [bPS[bk]])
                    self.CP("act" if tt % 2 == 0 else "dve", dst[:, tt * 512:(tt + 1) * 512], self.PSb(bk), [bPS[bk]], [bQK])
            for ct in range(4):
                Ah = [self.PSb(2), self.PSb(3)]; bAh = [bPS[2], bPS[3]]
                Oh = [self.PS[0:64, 6 * 512:7 * 512], self.PS[0:64, 7 * 512:8 * 512]]; bOh = [bPS[6], bPS[7]]
                for hh in range(2):
                    self.chain(Ah[hh], [(self.ZEROB[:], HM[:, 0, 0:512])], [self.bCONST, bHM], [bAh[hh]])
                    self.chain(Oh[hh], [(self.ZEROB[:, 0:64], HM[:, 0, 0:512])], [self.bCONST, bHM], [bOh[hh]])
                for kb in range(4 * ct + 3, -1, -1):
                    first = max(kb, 4 * ct)
                    c0, c1 = first * 128, (4 * ct + 4) * 128
                    w = c1 - c0
                    lo = c0 - ct * 512
                    diag = kb >= 4 * ct

                    def mk(hh):
                        h = 2 * hp + hh
                        rows = slice(hh * 64, (hh + 1) * 64)
                        sl = hh
                        A, bA, O, bO = Ah[hh], bAh[hh], Oh[hh], bOh[hh]
                        Z = self.PSb(sl, 0, w); bZ = bPS[sl]

                        def acc(dst, lhsT, rhs_t, r, wb):
                            S.op("pe", lambda e: e.matmul(dst[:, lo:lo + w], lhsT=lhsT, rhs=rhs_t[:, 0:w], start=False, stop=True), r, wb)

                        def st0():
                            self.chain(Z, [(KT2[rows, kb * 128:(kb + 1) * 128], QT2[rows, c0:c1])], [bQK], [bZ])

                        def st1():
                            self.ACT(EE[sl][:, 0:w], Z, AF.Exp, [bZ], [bEE[sl]], scale=0.125)

                        def st2():
                            self.ACT(SPp[sl][:, 0:w], EE[sl][:, 0:w], AF.Ln, [bEE[sl]], [bSP[sl]], bias=1.0)

                        def st3():
                            if diag:
                                S.op("pool", lambda e: e.affine_select(out=SPp[sl][:, 0:128], in_=SPp[sl][:, 0:128], pattern=[[1, 128]],
                                                                       compare_op=ALU.is_gt, fill=0.0, base=0, channel_multiplier=-1), [bSP[sl]], [bSP[sl]])

                        def st4():
                            acc(A, self.TRI[:], SPp[sl], [bSP[sl], self.bCONST], [bA])

                        def st5():
                            self.STT("dve", LW[sl][:, 0:w], Z, 0.125, SPp[sl][:, 0:w], ALU.mult, ALU.subtract, [bZ, bSP[sl]], [bLW[sl]])

                        def st6():
                            self.TT("dve", LW[sl][:, 0:w], LW[sl][:, 0:w], A[:, lo:lo + w], ALU.subtract, [bLW[sl], bA], [bLW[sl]])

                        def st7():
                            self.ACT(WT[sl][:, 0:w], LW[sl][:, 0:w], AF.Exp, [bLW[sl]], [bWT[sl]])

                        def st8():
                            if diag:
                                S.op("pool", lambda e: e.affine_select(out=WT[sl][:, 0:128], in_=WT[sl][:, 0:128], pattern=[[1, 128]],
                                                                       compare_op=ALU.is_gt, fill=0.0, base=0, channel_multiplier=-1), [bWT[sl]], [bWT[sl]])

                        def st9():
                            acc(O, VSB[:, kb, h * 64:(h + 1) * 64], WT[sl], [bVS, bWT[sl]], [bO])
                            if kb > 0:
                                acc(A, self.TRIC[:], SPp[sl], [bSP[sl], self.bCONST], [bA])
                        return [st0, st1, st2, st3, st4, st5, st6, st7, st8, st9]
                    stg = [mk(0), mk(1)]
                    for si in range(10):
                        for hh in range(2):
                            stg[hh][si]()
                for hh in range(2):
                    self.CP("act", OB[hh * 64:(hh + 1) * 64, hp, ct * 512:(ct + 1) * 512], Oh[hh], [bOh[hh]], [bOB])
        if self.dbg == "ob":
            self.dump_bf(OB, [bOB], 4)
            return
        S.sync_all()
        MT = self.cv(AB, SC, [128, 8, S_LEN], BF16); bMT = self.B("mt")
        SG = [self.cv(AB, SC + 32768 + i * 2048, [128, 512], F32) for i in range(4)]
        bSG = [self.B() for _ in range(4)]
        it = 0
        for j in range(8):
            ws, bws = load_ws(OFF_MG + j * 3072, 3072)
            ga = ws[:, 0:1024].rearrange("p (c j) -> p c j", c=8)
            gb = ws[:, 1024:2048].rearrange("p (c j) -> p c j", c=8)
            ua = ws[:, 2048:2560].rearrange("p (c j) -> p c j", c=4)
            ub = ws[:, 2560:3072].rearrange("p (c j) -> p c j", c=4)
            for tt in range(4):
                cs = slice(tt * 512, (tt + 1) * 512)
                pb = (it % 2) * 4
                it += 1
                self.chain(self.PSb(pb), [(ga[:, cc, :], HM[:, cc, cs]) for cc in range(8)], [bws, bHM], [bPS[pb]])
                self.chain(self.PSb(pb + 1), [(ua[:, hh, :], OA[:, hh, cs]) for hh in range(4)], [bws, bOA], [bPS[pb + 1]])
                self.chain(self.PSb(pb + 2), [(gb[:, cc, :], HM[:, cc, cs]) for cc in range(8)], [bws, bHM], [bPS[pb + 2]])
                self.chain(self.PSb(pb + 3), [(ub[:, hh, :], OB[:, hh, cs]) for hh in range(4)], [bws, bOB], [bPS[pb + 3]])
                self.ACT(SG[0], self.PSb(pb), AF.Sigmoid, [bPS[pb]], [bSG[0]])
                self.ACT(SG[1], self.PSb(pb + 2), AF.Sigmoid, [bPS[pb + 2]], [bSG[1]])
                self.TT("dve", SG[2], SG[0], self.PSb(pb + 1), ALU.mult, [bSG[0], bPS[pb + 1]], [bSG[2]])
                self.TT("dve", SG[3], SG[1], self.PSb(pb + 3), ALU.mult, [bSG[1], bPS[pb + 3]], [bSG[3]])
                self.TT("pool", MT[:, j, cs], SG[2], SG[3], ALU.add, [bSG[2], bSG[3]], [bMT])
        it = 0
        for jo in range(8):
            ws, bws = load_ws(OFF_WO + jo * 1024, 1024)
            wo = ws[:, 0:1024].rearrange("p (c j) -> p c j", c=8)
            for tt in range(4):
                cs = slice(tt * 512, (tt + 1) * 512)
                pb = it % 4
                it += 1
                self.chain(self.PSb(pb), [(wo[:, jj, :], MT[:, jj, cs]) for jj in range(8)], [bws, bMT], [bPS[pb]])
                self.STT("dve", self.Xt[:, jo, cs], self.PSb(pb), self.mod(2, jo), self.Xt[:, jo, cs], ALU.mult, ALU.add,
                         [bPS[pb], self.bCUR, self.bX[tt]], [self.bX[tt]])
        S.sync_all()
        self.layer_norm(0, SC)
        if self.dbg == "ln1":
            self.dump_x()

    def dump_x(self):
        for cc in range(8):
            self.LD(self.dbg_out[:, cc, :], self.Xt[:, cc, :], self.bX[0], r=self.bX)

    def dump_bf(self, src, bufs, n):
        T = self.cv(self.AB, 16384, [128, S_LEN], F32)
        bT = self.B()
        self.S.sync_all()
        for i in range(n):
            self.CP("dve", T, src[:, i, :], bufs, [bT])
            self.LD(self.dbg_out[:, i, :], T, bT, r=[bT])

    def phase2(self):
        nc, S = self.nc, self.S
        l = self.l
        AB = self.AB
        W = self.WSC[l]
        self.G = self.cv(self.AA, 0, [128, 256, 128], BF16); self.bG = self.B("G")
        self.UT = [self.cv(AB, i * 8192, [128, 4, 8, 128], BF16) for i in range(2)]
        self.VV = [self.cv(AB, 16384 + i * 8192, [128, 4, 1024], BF16) for i in range(2)]
        self.bUT = [self.B("ut0"), self.B("ut1")]
        self.bVV = [self.B("v0"), self.B("v1")]
        self.HFT = self.cv(AB, 32768, [128, 8, 256], BF16); self.bHFT = self.B("hft")
        self.QT = self.cv(AB, 36864, [128, 8, 256], BF16); self.bQT = self.B("qt")
        self.WQ = self.cv(AB, 16384, [128, 8, 8, 128], BF16)
        self.KBD = self.cv(AB, 40960, [128, 8, 256], BF16); self.bKBD = self.B("kbd")
        KBF = self.cv(AB, 0, [128, 8, 256], F32)
        bKF = self.bUT[0]
        self.MS("pool", KBF, 0.0, [bKF])
        self.LD(KBF[0:64, :, 0:128], self.skT[l, 0].rearrange("h d n -> d h n"), bKF, w=[bKF])
        self.LD(KBF[64:128, :, 128:256], self.skT[l, 1].rearrange("h d n -> d h n"), bKF, w=[bKF])
        self.CP("dve", self.KBD, KBF, [bKF], [self.bKBD])
        S.sync_all()
        self.run_loop(self.ntiles, self.peer_tile)
        if self.dbg == "peer":
            self.dump_x()
            return
        self.layer_norm(1, 0)
        if self.dbg == "ln2":
            self.dump_x()

    def peer_tile(self, tq):
        nc, S = self.nc, self.S
        l = self.l
        AB = self.AB
        bPS = self.bPS
        bXa = self.bX
        tsl = bass.ts(tq, 256)
        HFT, QT, WQ, KBD, G = self.HFT, self.QT, self.WQ, self.KBD, self.G
        XC = self.cv(AB, 8192, [128, 8, 256], F32)
        self.CP("pool", XC, self.Xt[:, :, tsl], bXa, [self.bUT[1]])
        for cc in range(8):
            if cc % 2 == 0:
                self.ACT(HFT[:, cc, :], XC[:, cc, :], AF.Identity, [self.bUT[1], self.bCUR], [self.bHFT], bias=self.mod(3, cc), scale=self.mod(4, cc))
            else:
                self.TS("dve", HFT[:, cc, :], XC[:, cc, :], self.mod(4, cc), self.mod(3, cc), ALU.mult, ALU.add, [self.bUT[1], self.bCUR], [self.bHFT])
        self.LD(WQ.rearrange("p a b c -> p (a b c)"), self.WSC[l][:, OFF_PQ:OFF_PQ + 8192], self.bVV[0], r=[self.bWSC], w=self.bVV)
        for h in range(8):
            bk = 4 + h % 4
            self.chain(self.PSb(bk, 0, 256), [(WQ[:, h, cc, :], HFT[:, cc, :]) for cc in range(8)], self.bVV + [self.bHFT], [bPS[bk]])
            self.CP("act" if h % 2 == 0 else "dve", QT[:, h, :], self.PSb(bk, 0, 256), [bPS[bk]], [self.bQT])
        SS = self.cv(AB, 0, [128, 2048], F32); bSS = self.bUT[0]
        EQ = self.cv(AB, 8192, [128, 2048], F32); bEQ = self.bUT[1]
        T0 = 45056
        TOPV = self.cv(AB, T0, [128, 16, 16], F32)
        TOPI = self.cv(AB, T0 + 1024, [128, 16, 16], U32)
        TOPIF = self.cv(AB, T0 + 2048, [128, 16, 16], F32)
        SR = self.cv(AB, T0 + 3072, [128, 256], F32)
        CTOP = self.cv(AB, T0 + 4096, [128, 8, 16], F32)
        CPOS = self.cv(AB, T0 + 4608, [128, 128], U32)
        CAI = self.cv(AB, T0 + 5120, [128, 128], I32)
        CBI = self.cv(AB, T0 + 5632, [128, 128], I32)
        CAF = self.cv(AB, T0 + 6144, [128, 128], F32)
        CBF = self.cv(AB, T0 + 6656, [128, 128], F32)
        IDX = [self.cv(AB, T0 + 7168 + i * 512, [128, 128], F32) for i in range(3)]
        GSUM = self.cv(AB, T0 + 8704, [128, 8], F32)
        IDT = [self.cv(AB, T0 + 9216 + i * 1024, [128, 256], F32) for i in range(3)]
        OH0 = T0 + 12288
        EQ1 = self.cv(AB, OH0, [128, 8, 128], BF16)
        OH1 = self.cv(AB, OH0 + 2048, [128, 8, 128], BF16)
        OH2 = self.cv(AB, OH0 + 4096, [128, 8, 128], BF16)
        AG0 = OH0 + 6144
        AG = [self.cv(AB, AG0 + i * 1024, [128, 256], F32) for i in range(2)]
        WW = [self.cv(AB, AG0 + 2048 + i * 512, [128, 256], BF16) for i in range(2)]
        bTK = self.B("topk"); bSR = self.B("sr"); bIDX = self.B("idx"); bIDT = self.B("idt")
        bE1, bO1, bO2 = self.B(), self.B(), self.B()
        bAG = [self.B(), self.B()]; bWW = [self.B(), self.B()]
        dv = lambda fn, r, w: S.op("dve", fn, r, w)
        for st in range(2):
            tcs = slice(st * 128, (st + 1) * 128)
            for h in range(8):
                bk = 4 + h // 2
                self.chain(self.PSb(bk, (h % 2) * 256, (h % 2) * 256 + 256), [(QT[:, h, tcs], KBD[:, h, :])], [self.bQT, self.bKBD], [bPS[bk]])
            for k in range(4):
                self.CP("act" if k % 2 == 0 else "dve", SS[:, k * 512:(k + 1) * 512], self.PSb(4 + k), [bPS[4 + k]], [bSS])
            for g in range(16):
                v = SS[:, g * 128:(g + 1) * 128]
                dv(lambda e, g=g, v=v: e.max(out=TOPV[:, g, 0:8], in_=v), [bSS], [bTK])
                dv(lambda e, g=g, v=v: e.match_replace(out=SR[:, 0:128], in_to_replace=TOPV[:, g, 0:8], in_values=v, imm_value=-1e30), [bSS, bTK], [bSR])
                dv(lambda e, g=g: e.max(out=TOPV[:, g, 8:16], in_=SR[:, 0:128]), [bSR], [bTK])
                dv(lambda e, g=g, v=v: e.max_index(out=TOPI[:, g, 0:8], in_max=TOPV[:, g, 0:8], in_values=v), [bSS, bTK], [bTK])
                dv(lambda e, g=g, v=v: e.max_index(out=TOPI[:, g, 8:16], in_max=TOPV[:, g, 8:16], in_values=v), [bSS, bTK], [bTK])
            self.CP("dve", TOPIF, TOPI, [bTK], [bTK])
            TV4 = TOPV.rearrange("p (h two) k -> p h two k", two=2)
            TI4 = TOPIF.rearrange("p (h two) k -> p h two k", two=2)
            self.TT("dve", SS.rearrange("p (h a b) -> p h a b", h=8, a=16),
                    TV4[:, :, 0, :, None].broadcast_to([128, 8, 16, 16]), TV4[:, :, 1, None, :].broadcast_to([128, 8, 16, 16]), ALU.add, [bTK], [bSS])
            for h in range(8):
                v = SS[:, h * 256:(h + 1) * 256]
                dv(lambda e, h=h, v=v: e.max(out=CTOP[:, h, 0:8], in_=v), [bSS], [bTK])
                dv(lambda e, h=h, v=v: e.match_replace(out=SR[:, 0:256], in_to_replace=CTOP[:, h, 0:8], in_values=v, imm_value=-1e30), [bSS, bTK], [bSR])
                dv(lambda e, h=h: e.max(out=CTOP[:, h, 8:16], in_=SR[:, 0:256]), [bSR], [bTK])
                dv(lambda e, h=h, v=v: e.max_index(out=CPOS[:, h * 16:h * 16 + 8], in_max=CTOP[:, h, 0:8], in_values=v), [bSS, bTK], [bTK])
                dv(lambda e, h=h, v=v: e.max_index(out=CPOS[:, h * 16 + 8:h * 16 + 16], in_max=CTOP[:, h, 8:16], in_values=v), [bSS, bTK], [bTK])
            dv(lambda e: e.tensor_single_scalar(out=CAI, in_=CPOS.bitcast(I32), scalar=4, op=ALU.arith_shift_right), [bTK], [bTK])
            dv(lambda e: e.tensor_single_scalar(out=CBI, in_=CPOS.bitcast(I32), scalar=15, op=ALU.bitwise_and), [bTK], [bTK])
            self.CP("dve", CAF, CAI, [bTK], [bTK])
            self.CP("dve", CBF, CBI, [bTK], [bTK])
            for half, src in ((0, CAF), (1, CBF)):
                self.TT("dve", EQ.rearrange("p (k a) -> p k a", a=16), src[:, :, None].broadcast_to([128, 128, 16]),
                        self.IOTA[:, None, 0:16].broadcast_to([128, 128, 16]), ALU.is_equal, [bTK, self.bCONST], [bEQ])
                self.TT("dve", EQ.rearrange("p (h k a) -> p h k a", h=8, k=16), EQ.rearrange("p (h k a) -> p h k a", h=8, k=16),
                        TI4[:, :, half, None, :].broadcast_to([128, 8, 16, 16]), ALU.mult, [bEQ, bTK], [bEQ])
                dv(lambda e, half=half: e.tensor_reduce(out=IDX[half], in_=EQ.rearrange("p (k a) -> p k a", a=16), axis=AX.X, op=ALU.add), [bEQ], [bIDX])
            G3 = IDX[2].rearrange("p (h k) -> p h k", k=16)
            self.TT("dve", G3, CTOP, CTOP[:, :, 0:1].broadcast_to([128, 8, 16]), ALU.subtract, [bTK], [bIDX])
            self.ACT(IDX[2], IDX[2], AF.Exp, [bIDX], [bIDX])
            dv(lambda e: e.tensor_reduce(out=GSUM, in_=G3, axis=AX.X, op=ALU.add), [bIDX], [bTK])
            dv(lambda e: e.reciprocal(out=GSUM, in_=GSUM), [bTK], [bTK])
            self.TT("dve", G3, G3, GSUM[:, :, None].broadcast_to([128, 8, 16]), ALU.mult, [bIDX, bTK], [bIDX])
            for i in range(3):
                bk = 4 + i
                S.op("pe", lambda e, i=i, bk=bk: e.transpose(self.PSb(bk, 0, 128), IDX[i], self.IDENT[:]), [bIDX, self.bCONST], [bPS[bk]])
                self.CP("act" if i % 2 == 0 else "dve", IDT[i][:, tcs], self.PSb(bk, 0, 128), [bPS[bk]], [bIDT])
        for tb in range(32):
            t0 = tb * 8
            pb = 4 + (tb % 2) * 2
            iob = self.IOTA[:, None, :].broadcast_to([128, 8, 128])
            self.TT("dve", EQ1, iob, IDT[0][:, t0:t0 + 8, None].broadcast_to([128, 8, 128]), ALU.is_equal, [bIDT, self.bCONST], [bE1])
            self.TT("pool", OH1, EQ1, IDT[2][:, t0:t0 + 8, None].broadcast_to([128, 8, 128]), ALU.mult, [bE1, bIDT], [bO1])
            self.TT("dve", OH2, iob, IDT[1][:, t0:t0 + 8, None].broadcast_to([128, 8, 128]), ALU.is_equal, [bIDT, self.bCONST], [bO2])

            def gmm(e, pb=pb):
                ins = None
                for j in range(8):
                    ins = e.matmul(self.PS[:, pb * 512 + j * 128:pb * 512 + (j + 1) * 128], lhsT=OH1[:, j, :], rhs=OH2[:, j, :], start=True, stop=True)
                return ins
            S.op("pe", gmm, [bO1, bO2], [bPS[pb], bPS[pb + 1]])
            self.CP("act", G[:, t0:t0 + 4, :], self.PSb(pb).rearrange("p (a b) -> p a b", a=4), [bPS[pb]], [self.bG])
            self.CP("dve", G[:, t0 + 4:t0 + 8, :], self.PSb(pb + 1).rearrange("p (a b) -> p a b", a=4), [bPS[pb + 1]], [self.bG])
        UTS, VS = self.UTS[l], self.VS[l]

        def load_u(s):
            sl = s % 2
            self.LD(self.UT[sl].rearrange("p a b c -> p (a b c)"), UTS[:, s * 4096:(s + 1) * 4096], self.bUT[sl], r=[self.bUV], w=[self.bUT[sl]])

        def load_v(s):
            sl = s % 2
            self.LD(self.VV[sl].rearrange("p a b -> p (a b)"), VS[:, s * 4096:(s + 1) * 4096], self.bVV[sl], r=[self.bUV], w=[self.bVV[sl]])

        def a_chain(i2):
            s, q = i2 // 4, i2 % 4
            bk = 4 + i2 % 4
            self.chain(self.PSb(bk, 0, 256), [(self.UT[s % 2][:, q, cc, :], HFT[:, cc, :]) for cc in range(8)], [self.bUT[s % 2], self.bHFT], [bPS[bk]])
            if q == 3 and s + 2 < 32:
                load_u(s + 2)
        for bk in range(4):
            self.chain(self.PSb(bk), [(self.ZEROB[:], G[:, 0:4, :].rearrange("p a b -> p (a b)"))], [self.bCONST, self.bG], [bPS[bk]])
        load_u(0); load_v(0); load_u(1); load_v(1)
        a_chain(0)
        a_chain(1)
        for i2 in range(128):
            s, q = i2 // 4, i2 % 4
            if i2 + 2 < 128:
                a_chain(i2 + 2)
            bk = 4 + i2 % 4
            a = i2 % 2
            self.ACT(AG[a], self.PSb(bk, 0, 256), AF.Gelu, [bPS[bk]], [bAG[a]])
            self.TT("dve", WW[a], AG[a], G[:, :, i2], ALU.mult, [bAG[a], self.bG], [bWW[a]])

            def ymm(e, i2=i2, s=s, q=q, a=a):
                ins = None
                for dc in range(8):
                    ins = e.matmul(self.PS[:, dc * 256:(dc + 1) * 256], lhsT=self.VV[s % 2][:, q, dc * 128:(dc + 1) * 128], rhs=WW[a],
                                   start=False, stop=True)
                return ins
            S.op("pe", ymm, [self.bVV[s % 2], bWW[a]], bPS[0:4])
            if q == 3 and s + 2 < 32:
                load_v(s + 2)
        YT = self.cv(AB, 0, [128, 8, 256], F32)
        for dc in range(8):
            if dc % 2 == 0:
                self.TS("dve", YT[:, dc, :], self.PS[:, dc * 256:(dc + 1) * 256], self.mod(5, dc), None, ALU.mult, None, [bPS[dc // 2], self.bCUR], [self.bUT[0]])
            else:
                self.ACT(YT[:, dc, :], self.PS[:, dc * 256:(dc + 1) * 256], AF.Identity, [bPS[dc // 2], self.bCUR], [self.bUT[0]], scale=self.mod(5, dc))
        self.TT("dve", self.Xt[:, :, tsl], self.Xt[:, :, tsl], YT, ALU.add, [self.bUT[0]] + bXa, bXa)


_CACHE = {}


def _prep_weights(w_ada, b_ada, w_in, lb_logits, hg_norm_g, w_up_a, w_up_b, w_o, w_pq, sub_keys, peer_u, peer_v, ln_g, ln_b):
    nl = w_ada.shape[0]
    f = lambda a: np.ascontiguousarray(np.asarray(a, dtype=np.float32))
    d = {}
    d["w_ada"] = f(w_ada); d["w_in"] = f(w_in); d["w_up_a"] = f(w_up_a); d["w_up_b"] = f(w_up_b)
    d["w_o"] = f(w_o); d["w_pq"] = f(w_pq)
    d["bada"] = f(np.asarray(b_ada).reshape(nl, 48, 128).transpose(2, 0, 1))
    d["lbl"] = f(np.asarray(lb_logits).reshape(nl, 4, 128).transpose(2, 0, 1))
    d["hgg"] = f(np.asarray(hg_norm_g).reshape(nl, 4, 128).transpose(2, 0, 1))
    d["lng"] = f(np.asarray(ln_g).reshape(nl, 2, 8, 128).transpose(3, 0, 1, 2))
    d["lnb"] = f(np.asarray(ln_b).reshape(nl, 2, 8, 128).transpose(3, 0, 1, 2))
    d["skT"] = f(np.asarray(sub_keys).transpose(0, 1, 2, 4, 3))
    d["uT"] = f(np.asarray(peer_u).reshape(nl, 128, 128, 8, 128).transpose(0, 4, 2, 3, 1)).reshape(nl, 128, 128 * 1024)
    d["pv"] = f(peer_v).reshape(nl, 128, 128 * 1024)
    return d


def kernel(x, c, w_ada, b_ada, w_in, lb_logits, hg_norm_g, w_up_a, w_up_b, w_o, w_pq, sub_keys, peer_u, peer_v, ln_g, ln_b):
    x = np.asarray(x, dtype=np.float32)
    c = np.asarray(c, dtype=np.float32)
    B = x.shape[0]
    nseq = B // NCORES
    key = ("full", nseq)
    if key not in _CACHE:
        _CACHE[key] = MK(nseq=nseq, nl=DEPTH)
    mk = _CACHE[key]
    wd = _prep_weights(w_ada, b_ada, w_in, lb_logits, hg_norm_g, w_up_a, w_up_b, w_o, w_pq, sub_keys, peer_u, peer_v, ln_g, ln_b)
    in_maps = []
    for i in range(NCORES):
        xs = x[i * nseq:(i + 1) * nseq]
        xT = np.ascontiguousarray(xs.reshape(nseq, S_LEN, 8, 128).transpose(0, 3, 2, 1))
        cs = c[i * nseq:(i + 1) * nseq]
        cT = np.ascontiguousarray(cs.reshape(nseq, 8, 128).transpose(2, 1, 0))
        m = dict(wd)
        m["xT"] = xT
        m["cT"] = cT
        in_maps.append(m)
    res = run_bass_kernel_spmd(mk.nc, in_maps, core_ids=list(range(NCORES)))
    out = np.empty((B, S_LEN, D), dtype=np.float32)
    for i in range(NCORES):
        oT = np.asarray(res.results[i]["outT"])
        out[i * nseq:(i + 1) * nseq] = oT.transpose(0, 3, 2, 1).reshape(nseq, S_LEN, D)
    return out
```

```python
from contextlib import ExitStack
import numpy as np
import concourse.bass as bass
import concourse.mybir as mybir
from concourse.bass_utils import run_bass_kernel_spmd

F32 = mybir.dt.float32
BF16 = mybir.dt.bfloat16
U32 = mybir.dt.uint32
I32 = mybir.dt.int32
U8 = mybir.dt.uint8
AF = mybir.ActivationFunctionType
ALU = mybir.AluOpType
AX = mybir.AxisListType

D = 1024
S_LEN = 2048
NCORES = 8
DEPTH = 4
BATCH = 32
ALPHA = (2.0 * DEPTH) ** 0.25
LN_EPS = 1e-5
RMS_EPS = 1e-6
IN_W = 5632
OFF_HG = 0
OFF_QK = OFF_HG + 4 * 4096
OFF_SV = OFF_QK + 4 * 2048
OFF_MG = OFF_SV + 4096
OFF_WO = OFF_MG + 8 * 3072
OFF_PQ = OFF_WO + 8 * 1024
NWS = OFF_PQ + 8192


class Buf:
    def __init__(self, S, name=""):
        self.name = name
        self.w = None
        self.r = {}
        S.bufs.append(self)


class Sched:
    def __init__(self, nc):
        self.nc = nc
        self.eng = {"pe": nc.tensor, "act": nc.scalar, "dve": nc.vector, "pool": nc.gpsimd, "sp": nc.sync}
        self.same = {"act", "dve", "pool"}
        self.es = ExitStack()
        self.semh = {}
        self.cnt = {}
        self.waited = {e: {} for e in self.eng}
        self.bufs = []
        self.n = 0
        self.pool = [self.es.enter_context(nc.semaphore("sp%d" % i)) for i in range(48)]
        self.npool = 0
        for e in self.eng:
            self._sem(e)

    def sb(self, name, shape, dt):
        return self.es.enter_context(self.nc.sbuf_tensor(name, list(shape), dt))

    def ps(self, name, shape, dt):
        return self.es.enter_context(self.nc.psum_tensor(name, list(shape), dt))

    def _sem(self, key):
        if key not in self.semh:
            self.semh[key] = self.pool[self.npool]
            self.npool += 1
            self.cnt[key] = 0
        return self.semh[key]

    def _waits(self, e, reads, writes):
        waits = {}

        def need(k, v):
            if waits.get(k, 0) < v:
                waits[k] = v
        for b in reads:
            if b.w is not None:
                need(*b.w)
        for b in writes:
            if b.w is not None:
                need(*b.w)
            for k, v in b.r.items():
                if k != e:
                    need(k, v)
        eng = self.eng[e]
        for k, v in waits.items():
            if k == e and e not in self.same:
                continue
            if self.waited[e].get(k, 0) >= v:
                continue
            self.waited[e][k] = v
            eng.wait_ge(self.semh[k], v)
            self.n += 1

    def op(self, e, fn, reads=(), writes=()):
        self._sem(e)
        self._waits(e, reads, writes)
        ins = fn(self.eng[e])
        ins.then_inc(self.semh[e], 1)
        self.cnt[e] += 1
        idx = self.cnt[e]
        for b in reads:
            b.r[e] = idx
        for b in writes:
            b.w = (e, idx)
            b.r = {}
        self.n += 1

    def dma(self, q, fn, key_buf, reads=(), writes=()):
        key = ("dma", id(key_buf))
        self._sem(key)
        self._sem(q)
        self._waits(q, reads, writes)
        ins = fn(self.eng[q])
        ins.then_inc(self.semh[key], 16)
        self.cnt[key] += 16
        val = self.cnt[key]
        for b in reads:
            b.r[key] = val
        for b in writes:
            b.w = (key, val)
            b.r = {}
        self.n += 1

    def sync_all(self):
        nc = self.nc
        for key, h in self.semh.items():
            if isinstance(key, tuple) and self.cnt[key] > 0:
                nc.sync.wait_ge(h, self.cnt[key])
        nc.all_engine_barrier()
        for h in self.pool:
            nc.gpsimd.sem_clear(h)
        nc.all_engine_barrier()
        for k in self.cnt:
            self.cnt[k] = 0
        for e in self.waited:
            self.waited[e] = {}
        for b in self.bufs:
            b.w = None
            b.r = {}


class MK:
    def __init__(self, nseq=4, nl=4, hw_loops=True, ntiles=8, dbg=None, stop_after=None):
        self.nseq, self.nl, self.hw, self.ntiles, self.dbg, self.stop_after = nseq, nl, hw_loops, ntiles, dbg, stop_after
        self.nc = bass.Bass("TRN2", target_bir_lowering=False)
        self.S = Sched(self.nc)
        self.build()

    def B(self, name=""):
        return Buf(self.S, name)

    def chain(self, out, pairs, r, w, first=True, last=True):
        def fn(e):
            n = len(pairs)
            ins = None
            for i, (a, b) in enumerate(pairs):
                ins = e.matmul(out, lhsT=a, rhs=b, start=(first and i == 0), stop=(last and i == n - 1))
            return ins
        self.S.op("pe", fn, r, w)

    def ACT(self, out, in_, func, r, w, bias=None, scale=None):
        kw = {}
        if bias is not None:
            kw["bias"] = bias
        if scale is not None:
            kw["scale"] = scale
        self.S.op("act", lambda e: e.activation(out=out, in_=in_, func=func, **kw), r, w)

    def TT(self, eng, out, in0, in1, op, r, w):
        self.S.op(eng, lambda e: e.tensor_tensor(out=out, in0=in0, in1=in1, op=op), r, w)

    def TS(self, eng, out, in0, s1, s2, op0, op1, r, w):
        if s2 is None:
            self.S.op(eng, lambda e: e.tensor_scalar(out=out, in0=in0, scalar1=s1, scalar2=None, op0=op0), r, w)
        else:
            self.S.op(eng, lambda e: e.tensor_scalar(out=out, in0=in0, scalar1=s1, scalar2=s2, op0=op0, op1=op1), r, w)

    def STT(self, eng, out, in0, scalar, in1, op0, op1, r, w):
        self.S.op(eng, lambda e: e.scalar_tensor_tensor(out=out, in0=in0, scalar=scalar, in1=in1, op0=op0, op1=op1), r, w)

    def CP(self, eng, out, in_, r, w):
        if eng == "act":
            self.S.op("act", lambda e: e.copy(out=out, in_=in_), r, w)
        else:
            self.S.op(eng, lambda e: e.tensor_copy(out=out, in_=in_), r, w)

    def MS(self, eng, ap, val, w):
        self.S.op(eng, lambda e: e.memset(ap, val), (), w)

    def LD(self, out, in_, key, r=(), w=(), q="sp"):
        self.S.dma(q, lambda e: e.dma_start(out=out, in_=in_), key, r, w)

    def cv(self, arena, off, shape, dt):
        nb = {F32: 4, BF16: 2, U32: 4, I32: 4}[dt]
        n = int(np.prod(shape[1:])) * nb
        ap = arena[0:shape[0], off:off + n].bitcast(dt)
        if len(shape) == 3:
            ap = ap.rearrange("p (a b) -> p a b", a=shape[1])
        elif len(shape) == 4:
            ap = ap.rearrange("p (a b c) -> p a b c", a=shape[1], b=shape[2])
        return ap

    def run_loop(self, n, body):
        if self.hw and n > 1:
            with self.nc.Fori(0, n) as i:
                self.S.sync_all()
                body(i)
            self.S.sync_all()
        else:
            for i in range(n):
                self.S.sync_all()
                body(i)
            self.S.sync_all()

    def build(self):
        nc, S, nseq, nl = self.nc, self.S, self.nseq, self.nl
        dram = lambda name, shape, dt, kind: nc.dram_tensor(name, list(shape), dt, kind=kind).ap()
        self.xT = dram("xT", [nseq, 128, 8, S_LEN], F32, "ExternalInput")
        self.cT = dram("cT", [128, 8, nseq], F32, "ExternalInput")
        self.w_ada = dram("w_ada", [nl, D, 6 * D], F32, "ExternalInput")
        self.bada = dram("bada", [128, nl, 48], F32, "ExternalInput")
        self.w_in = dram("w_in", [nl, D, IN_W], F32, "ExternalInput")
        self.lbl = dram("lbl", [128, nl, 4], F32, "ExternalInput")
        self.hgg = dram("hgg", [128, nl, 4], F32, "ExternalInput")
        self.w_up_a = dram("w_up_a", [nl, 512, D], F32, "ExternalInput")
        self.w_up_b = dram("w_up_b", [nl, 512, D], F32, "ExternalInput")
        self.w_o = dram("w_o", [nl, D, D], F32, "ExternalInput")
        self.w_pq = dram("w_pq", [nl, D, D], F32, "ExternalInput")
        self.skT = dram("skT", [nl, 2, 8, 64, 128], F32, "ExternalInput")
        self.uT = dram("uT", [nl, 128, 128 * 1024], F32, "ExternalInput")
        self.pv = dram("pv", [nl, 128, 128 * 1024], F32, "ExternalInput")
        self.lng = dram("lng", [128, nl, 2, 8], F32, "ExternalInput")
        self.lnb = dram("lnb", [128, nl, 2, 8], F32, "ExternalInput")
        self.outT = dram("outT", [nseq, 128, 8, S_LEN], F32, "ExternalOutput")
        if self.dbg:
            self.dbg_out = dram("dbg_out", [128, 8, S_LEN], F32, "ExternalOutput")
        self.WSC = dram("wsc", [nl, 128, NWS], BF16, "Internal")
        self.UTS = dram("uts", [nl, 128, 128 * 1024], BF16, "Internal")
        self.VS = dram("vs", [nl, 128, 128 * 1024], BF16, "Internal")
        self.bWSC = self.B("wsc")
        self.bUV = self.B("uvs")
        self.Xt = S.sb("X", [128, 8, S_LEN], F32)
        self.bX = [self.B("X%d" % i) for i in range(4)]
        self.AA = S.sb("arenaA", [128, 65536], U8)
        self.AB = S.sb("arenaB", [128, 69 * 1024], U8)
        self.PS = S.ps("psum", [128, 4096], F32)
        self.bPS = [self.B("ps%d" % i) for i in range(8)]
        c = lambda n, sh, dt=F32: S.sb(n, sh, dt)
        self.IDENT = c("ident", [128, 128]); self.IOTA = c("iota", [128, 128])
        self.TRI = c("tri", [128, 128]); self.TRIC = c("tric", [128, 128])
        self.ONESD = c("onesd", [128, 128]); self.ONESH = c("onesh", [128, 128])
        self.MASKH = c("maskh", [128, 128]); self.MASKU = c("masku", [128, 128], U32)
        self.ZEROB = c("zerob", [128, 128], BF16)
        self.MODT = c("modt", [128, nseq, nl, 48])
        self.LNG = c("lngs", [128, nl, 2, 8]); self.LNB = c("lnbs", [128, nl, 2, 8])
        self.HGG = c("hggs", [128, nl, 4]); self.LBT = c("lbt", [128, nl, 4, 3])
        self.CUR = c("cur", [128, 48 + 16 + 16 + 4 + 12])
        self.bCUR = self.B("cur")
        self.bCONST = self.B("const")
        self.prologue()
        S.sync_all()
        if self.stop_after == "pro":
            return
        self.run_loop(nseq, self.seq_body)

    def PSb(self, i, lo=0, hi=512):
        return self.PS[:, i * 512 + lo:i * 512 + hi]

    def prologue(self):
        nc, S, nseq, nl = self.nc, self.S, self.nseq, self.nl
        bC = self.bCONST
        io = lambda out, base, cm, pat: S.op("pool", lambda e: e.iota(out, pattern=pat, base=base, channel_multiplier=cm,
                                                                     allow_small_or_imprecise_dtypes=True), (), [bC])
        io(self.IOTA[:], 0, 0, [[1, 128]])
        io(self.IDENT[:], 0, -1, [[1, 128]])
        self.TS("dve", self.IDENT[:], self.IDENT[:], 0.0, None, ALU.is_equal, None, [bC], [bC])
        io(self.TRI[:], 0, 1, [[-1, 128]])
        self.TS("dve", self.TRIC[:], self.TRI[:], 0.0, None, ALU.is_le, None, [bC], [bC])
        self.TS("dve", self.TRI[:], self.TRI[:], 0.0, None, ALU.is_gt, None, [bC], [bC])
        self.MS("pool", self.ZEROB[:], 0.0, [bC])
        self.MS("pool", self.ONESD[:], 1.0 / D, [bC])
        self.MS("pool", self.ONESH[:], 1.0 / 128, [bC])
        io(self.MASKH[:], 0, -1, [[1, 128]])
        self.TS("dve", self.MASKH[:], self.MASKH[:], 0.0, None, ALU.is_ge, None, [bC], [bC])
        self.MS("dve", self.MASKH[0:64, 64:128], 0.0, [bC])
        self.CP("dve", self.MASKU[:], self.MASKH[:], [bC], [bC])
        self.LD(self.LNG[:], self.lng, bC, w=[bC]); self.LD(self.LNB[:], self.lnb, bC, w=[bC])
        self.LD(self.HGG[:], self.hgg, bC, w=[bC])
        STI = [self.AA[:, i * 16384:(i + 1) * 16384].bitcast(F32) for i in range(2)]
        STO = [self.AA[:, 32768 + i * 8192:32768 + (i + 1) * 8192].bitcast(BF16) for i in range(2)]
        bSTI = [self.B("sti0"), self.B("sti1")]
        bSTO = [self.B("sto0"), self.B("sto1")]
        jobs = []
        for l in range(nl):
            W = self.WSC[l]
            win = self.w_in[l].rearrange("(c p) j -> p c j", p=128)
            wua = self.w_up_a[l].rearrange("(h p) j -> p h j", p=128)
            wub = self.w_up_b[l].rearrange("(h p) j -> p h j", p=128)
            wo = self.w_o[l].rearrange("(c p) j -> p c j", p=128)
            wpq = self.w_pq[l].rearrange("(c p) j -> p c j", p=128)

            def part(off, n_c, w, j0, src):
                return (lambda st, off=off, n_c=n_c, w=w, j0=j0: st[:, off:off + n_c * w].rearrange("p (c j) -> p c j", c=n_c)[:, :, j0:j0 + src.shape[2]], src)
            for h in range(4):
                jobs.append((W[:, OFF_HG + h * 4096:OFF_HG + (h + 1) * 4096], 4096,
                             [part(0, 8, 512, k * 128, win[:, :, k * 512 + h * 128:k * 512 + (h + 1) * 128]) for k in range(4)], self.bWSC))
            for hp in range(4):
                jobs.append((W[:, OFF_QK + hp * 2048:OFF_QK + (hp + 1) * 2048], 2048,
                             [part(0, 8, 256, k * 128, win[:, :, 2048 + k * 512 + hp * 128:2048 + k * 512 + (hp + 1) * 128]) for k in range(2)], self.bWSC))
            jobs.append((W[:, OFF_SV:OFF_SV + 4096], 4096, [part(0, 8, 512, 0, win[:, :, 3072:3584])], self.bWSC))
            for j in range(8):
                jobs.append((W[:, OFF_MG + j * 3072:OFF_MG + (j + 1) * 3072], 3072,
                             [part(0, 8, 128, 0, win[:, :, 3584 + j * 128:3584 + (j + 1) * 128]),
                              part(1024, 8, 128, 0, win[:, :, 4608 + j * 128:4608 + (j + 1) * 128]),
                              part(2048, 4, 128, 0, wua[:, :, j * 128:(j + 1) * 128]),
                              part(2560, 4, 128, 0, wub[:, :, j * 128:(j + 1) * 128])], self.bWSC))
            for off, wsrc in ((OFF_WO, wo), (OFF_PQ, wpq)):
                for jj in range(2):
                    jobs.append((W[:, off + jj * 4096:off + (jj + 1) * 4096], 4096,
                                 [part(q * 1024, 8, 128, 0, wsrc[:, :, (jj * 4 + q) * 128:(jj * 4 + q + 1) * 128]) for q in range(4)], self.bWSC))
            if self.dbg not in ("oa", "ob", "ln1", "nouv", "hm", "proj", "ew", "blk"):
                for k in range(32):
                    sl = slice(k * 4096, (k + 1) * 4096)
                    jobs.append((self.UTS[l][:, sl], 4096, [(lambda st: st[:, 0:4096], self.uT[l][:, sl])], self.bUV))
                    jobs.append((self.VS[l][:, sl], 4096, [(lambda st: st[:, 0:4096], self.pv[l][:, sl])], self.bUV))

        def job_load(i):
            dst, n, parts, db = jobs[i]
            for vf, src in parts:
                self.LD(vf(STI[i % 2]), src, bSTI[i % 2], w=[bSTI[i % 2]])

        def job_rest(i):
            dst, n, parts, db = jobs[i]
            eng = ("act", "dve", "pool")[i % 3]
            self.CP(eng, STO[i % 2][:, 0:n], STI[i % 2][:, 0:n], [bSTI[i % 2]], [bSTO[i % 2]])
            self.LD(dst, STO[i % 2][:, 0:n], bSTO[i % 2], r=[bSTO[i % 2]], w=[db])
        if jobs:
            job_load(0)
            for i in range(len(jobs)):
                if i + 1 < len(jobs):
                    job_load(i + 1)
                job_rest(i)
        LBL = self.cv(self.AB, 0, [128, nl, 4], F32)
        LBE = self.cv(self.AB, 256, [128, nl, 4], F32)
        LBM = self.cv(self.AB, 512, [128, 4], F32)
        bT = self.B("lbtmp")
        self.LD(LBL, self.lbl, bT, w=[bT])
        S.op("dve", lambda e: e.tensor_reduce(out=LBM, in_=LBL.rearrange("p l h -> p h l"), axis=AX.X, op=ALU.max), [bT], [bT])
        self.TT("dve", LBE, LBL, LBM[:, None, :].broadcast_to([128, nl, 4]), ALU.subtract, [bT], [bT])
        self.ACT(LBE, LBE, AF.Exp, [bT], [bT])
        S.op("dve", lambda e: e.tensor_reduce(out=LBM, in_=LBE.rearrange("p l h -> p h l"), axis=AX.X, op=ALU.add), [bT], [bT])
        S.op("dve", lambda e: e.reciprocal(out=LBM, in_=LBM), [bT], [bT])
        self.TT("dve", LBE, LBE, LBM[:, None, :].broadcast_to([128, nl, 4]), ALU.mult, [bT], [bT])
        self.MS("dve", self.LBT[:, 0, :, 0], 0.0, [bC])
        for l in range(1, nl):
            self.TT("dve", self.LBT[:, l, :, 0], self.LBT[:, l - 1, :, 0], LBE[:, l, :], ALU.add, [bT, bC], [bC])
        self.TS("dve", self.LBT[:, :, :, 1], self.LBT[:, :, :, 0], -1.0, 1.0, ALU.mult, ALU.add, [bC], [bC])
        self.TS("dve", self.LBT[:, :, :, 2], self.LBT[:, :, :, 0], -1.0, None, ALU.add, None, [bC], [bC])
        COND = self.cv(self.AB, 1024, [128, 8, nseq], F32)
        BADA = self.cv(self.AB, 2048, [128, nl, 48], F32)
        bCo = self.B("cond")
        self.LD(COND, self.cT, bCo, w=[bCo])
        self.LD(BADA, self.bada, bCo, w=[bCo])
        self.ACT(COND, COND, AF.Silu, [bCo], [bCo])
        WA = [self.cv(self.AB, 8192 + i * 16384, [128, 8, 512], F32) for i in range(2)]
        bWA = [self.B("wa0"), self.B("wa1")]
        it = 0
        for l in range(nl):
            wa = self.w_ada[l].rearrange("(c p) j -> p c j", p=128)
            for g in range(12):
                s = it % 2
                self.LD(WA[s], wa[:, :, g * 512:(g + 1) * 512], bWA[s], w=[bWA[s]])
                bank = self.PSb(it % 4, 0, 4 * nseq)
                for jl in range(4):
                    self.chain(bank[:, jl * nseq:(jl + 1) * nseq],
                               [(WA[s][:, cc, jl * 128:(jl + 1) * 128], COND[:, cc, :]) for cc in range(8)],
                               [bWA[s], bCo], [self.bPS[it % 4]])
                self.TT("dve", self.MODT[:, :, l, g * 4:(g + 1) * 4], bank.rearrange("p (j b) -> p b j", b=nseq),
                        BADA[:, l, g * 4:(g + 1) * 4][:, None, :].broadcast_to([128, nseq, 4]), ALU.add,
                        [self.bPS[it % 4], bCo], [bC])
                it += 1
        for k in (1, 4):
            self.TS("dve", self.MODT[:, :, :, k * 8:(k + 1) * 8], self.MODT[:, :, :, k * 8:(k + 1) * 8], 1.0, None, ALU.add, None, [bC], [bC])
        for k in (2, 5):
            self.TS("dve", self.MODT[:, :, :, k * 8:(k + 1) * 8], self.MODT[:, :, :, k * 8:(k + 1) * 8], 1.0 / ALPHA, None, ALU.mult, None, [bC], [bC])

    def seq_body(self, b):
        self.b = b
        for cc in range(8):
            self.LD(self.Xt[:, cc, :], self.xT[b][:, cc, :], self.bX[0], w=self.bX)
        for l in range(self.nl):
            self.S.sync_all()
            self.layer_body(l)
        self.S.sync_all()
        for cc in range(8):
            self.LD(self.outT[b][:, cc, :], self.Xt[:, cc, :], self.bX[0], r=self.bX)
        if self.dbg:
            pass

    def layer_body(self, l):
        self.l = l
        b = self.b
        C = self.CUR
        bc = self.bCUR
        self.CP("pool", C[:, 0:48], self.MODT[:, b, l, :], [self.bCONST], [bc])
        self.CP("dve", C[:, 48:64], self.LNG[:, l].rearrange("p a b -> p (a b)"), [self.bCONST], [bc])
        self.CP("dve", C[:, 64:80], self.LNB[:, l].rearrange("p a b -> p (a b)"), [self.bCONST], [bc])
        self.CP("dve", C[:, 80:84], self.HGG[:, l, :], [self.bCONST], [bc])
        self.CP("dve", C[:, 84:96], self.LBT[:, l].rearrange("p a b -> p (a b)"), [self.bCONST], [bc])
        self.phase1()
        if self.stop_after == "p1":
            return
        self.S.sync_all()
        self.phase2()

    def mod(self, k, cc):
        return self.CUR[:, k * 8 + cc:k * 8 + cc + 1]

    def layer_norm(self, which, base):
        SQ = self.cv(self.AB, base, [128, 8, 512], F32)
        MEAN = self.cv(self.AB, base + 16384, [128, 512], F32)
        M2 = self.cv(self.AB, base + 18432, [128, 512], F32)
        RSTD = self.cv(self.AB, base + 20480, [128, 512], F32)
        bSQ, bM, bM2, bR = self.B(), self.B(), self.B(), self.B()
        for tt in range(4):
            bx = self.bX[tt]
            cs = slice(tt * 512, (tt + 1) * 512)
            Xv = self.Xt[:, :, cs]
            self.ACT(SQ, Xv, AF.Square, [bx], [bSQ])
            pm, pq = (0, 1) if tt % 2 == 0 else (2, 3)
            self.chain(self.PSb(pm), [(self.ONESD[:], self.Xt[:, cc, cs]) for cc in range(8)], [bx, self.bCONST], [self.bPS[pm]])
            self.chain(self.PSb(pq), [(self.ONESD[:], SQ[:, cc, :]) for cc in range(8)], [bSQ, self.bCONST], [self.bPS[pq]])
            self.CP("act", MEAN, self.PSb(pm), [self.bPS[pm]], [bM])
            self.TT("dve", M2, MEAN, MEAN, ALU.mult, [bM], [bM2])
            self.TT("dve", M2, self.PSb(pq), M2, ALU.subtract, [self.bPS[pq], bM2], [bM2])
            self.ACT(M2, M2, AF.Ln, [bM2], [bM2], bias=LN_EPS / (ALPHA * ALPHA))
            self.ACT(RSTD, M2, AF.Exp, [bM2], [bR], scale=-0.5)
            self.TT("dve", Xv, Xv, MEAN[:, None, :].broadcast_to([128, 8, 512]), ALU.subtract, [bx, bM], [bx])
            self.TT("pool", Xv, Xv, RSTD[:, None, :].broadcast_to([128, 8, 512]), ALU.mult, [bx, bR], [bx])
            for cc in range(8):
                g = self.CUR[:, 48 + which * 8 + cc:48 + which * 8 + cc + 1]
                bb = self.CUR[:, 64 + which * 8 + cc:64 + which * 8 + cc + 1]
                self.TS("dve" if cc % 2 == 0 else "pool", self.Xt[:, cc, cs], self.Xt[:, cc, cs], g, bb, ALU.mult, ALU.add, [bx, self.bCUR], [bx])

    def phase1(self):
        nc, S = self.nc, self.S
        l = self.l
        AA, AB = self.AA, self.AB
        HM = self.cv(AA, 0, [128, 8, S_LEN], BF16); bHM = self.B("hm")
        OA = self.cv(AA, 32768, [128, 4, S_LEN], BF16); bOA = self.B("oa")
        OB = self.cv(AA, 49152, [128, 4, S_LEN], BF16); bOB = self.B("ob")
        WS = [AB[:, 0:8192].bitcast(BF16), AB[:, 8192:16384].bitcast(BF16)]
        bWS = [self.B("ws0"), self.B("ws1")]
        self.ws_i = 0
        W = self.WSC[l]
        bPS = self.bPS

        def load_ws(off, n):
            s = self.ws_i % 2
            self.ws_i += 1
            o = 0
            for piece in (4096, 2048, 1024):
                while n - o >= piece:
                    self.LD(WS[s][:, o:o + piece], W[:, off + o:off + o + piece], bWS[s], r=[self.bWSC], w=[bWS[s]])
                    o += piece
            assert o == n
            return WS[s], bWS[s]

        for cc in range(8):
            if cc % 2 == 0:
                self.ACT(HM[:, cc, :], self.Xt[:, cc, :], AF.Identity, self.bX + [self.bCUR], [bHM], bias=self.mod(0, cc), scale=self.mod(1, cc))
            else:
                self.TS("dve", HM[:, cc, :], self.Xt[:, cc, :], self.mod(1, cc), self.mod(0, cc), ALU.mult, ALU.add, self.bX + [self.bCUR], [bHM])

        if self.dbg == "hm":
            self.dump_bf(HM, [bHM], 8)
            return
        SC = 16384
        P = [self.cv(AB, SC + i * 8192, [128, S_LEN], F32) for i in range(4)]
        bP = [self.B("P%d" % i) for i in range(4)]
        VH = self.cv(AB, SC + 32768, [128, 16, 128], F32); bVH = self.B("vh")
        RM = self.cv(AB, SC + 40960, [128, S_LEN], F32); bRM = self.B("rm")
        MI = SC + 49152
        SCM = [self.cv(AB, MI + i * 512, [128, 128], F32) for i in range(2)]; bSCM = [self.B(), self.B()]
        KDT = [self.cv(AB, MI + 1024 + i * 512, [128, 128], F32) for i in range(2)]; bKDT = [self.B(), self.B()]
        ST = [self.cv(AB, MI + 2048 + i * 512, [128, 128], F32) for i in range(2)]; bST = [self.B(), self.B()]
        EBE = self.cv(AB, MI + 3072, [128, 32], F32); bEBE = self.B()
        BMD = self.cv(AB, MI + 3200, [128, 32], F32)
        self.MS("pool", RM, 1.0, [bRM])
        self.MS("pool", RM.rearrange("p (a b) -> p a b", b=64)[:, :, 0:1], 0.0, [bRM])
        SCP = [self.PSb(i, 0, 128) for i in range(2)]; bSCP = [self.bPS[0], self.bPS[1]]
        KTP = [self.PSb(2 + i, 0, 128) for i in range(2)]; bKTP = [self.bPS[2], self.bPS[3]]
        STP = [self.PSb(6 + i, 0, 128) for i in range(2)]; bSTP = [self.bPS[6], self.bPS[7]]
        pr = [0]

        def proj_fm(ws, bws, col0, evac):
            for tt in range(4):
                bk = pr[0] % 4
                pr[0] += 1
                self.chain(self.PSb(bk), [(ws.rearrange("p (c j) -> p c j", c=8)[:, cc, col0:col0 + 128], HM[:, cc, tt * 512:(tt + 1) * 512]) for cc in range(8)],
                           [bws, bHM], [bPS[bk]])
                evac(tt, self.PSb(bk), bPS[bk])

        for h in range(4):
            ws, bws = load_ws(OFF_HG + h * 4096, 4096)
            ws3 = ws[:, 0:4096].rearrange("p (c j) -> p c j", c=8)
            lb = self.CUR[:, 84 + h * 3:84 + h * 3 + 1]
            omlb = self.CUR[:, 84 + h * 3 + 1:84 + h * 3 + 2]
            nomlb = self.CUR[:, 84 + h * 3 + 2:84 + h * 3 + 3]
            wsf = ws[:, 0:4096]
            proj_fm(wsf, bws, 0, lambda tt, ps, bps: self.CP("act", P[0][:, tt * 512:(tt + 1) * 512], ps, [bps], [bP[0]]))
            proj_fm(wsf, bws, 128, lambda tt, ps, bps: self.ACT(P[1][:, tt * 512:(tt + 1) * 512], ps, AF.Sigmoid, [bps], [bP[1]]))
            for g in range(4):
                bk = pr[0] % 4
                pr[0] += 1
                for j in range(4):
                    t = g * 4 + j
                    self.chain(self.PSb(bk, j * 128, (j + 1) * 128), [(HM[:, cc, t * 128:(t + 1) * 128], ws3[:, cc, 256:384]) for cc in range(8)],
                               [bws, bHM], [bPS[bk]])
                self.CP("dve", VH[:, g * 4:(g + 1) * 4, :], self.PSb(bk).rearrange("p (a b) -> p a b", a=4), [bPS[bk]], [bVH])
            if self.dbg == "proj":
                self.S.sync_all()
                for i in range(2):
                    self.LD(self.dbg_out[:, i, :], P[i], bP[i], r=[bP[i]])
                self.LD(self.dbg_out[:, 2, :], VH.rearrange("p a b -> p (a b)"), bVH, r=[bVH])
                return
            self.TS("dve", P[2], P[1], nomlb, omlb, ALU.mult, ALU.add, [bP[1], self.bCUR], [bP[2]])
            self.TS("dve", P[1], P[1], omlb, lb, ALU.mult, ALU.add, [bP[1], self.bCUR], [bP[1]])
            self.ACT(P[1], P[1], AF.Ln, [bP[1]], [bP[1]])
            S.op("dve", lambda e: e.tensor_tensor_scan(out=P[3], data0=RM, data1=P[1], initial=0.0, op0=ALU.mult, op1=ALU.add), [bRM, bP[1]], [bP[3]])
            P3v = P[3].rearrange("p (a b) -> p a b", b=64)
            self.CP("dve", BMD, P3v[:, :, 31], [bP[3]], [bEBE])
            self.TT("dve", P3v, P3v, BMD[:, :, None].broadcast_to([128, 32, 64]), ALU.subtract, [bP[3], bEBE], [bP[3]])
            self.TS("dve", P[3], P[3], -80.0, 80.0, ALU.max, ALU.min, [bP[3]], [bP[3]])
            self.TT("dve", EBE[:, 0:31], P3v[:, 0:31, 63], BMD[:, 1:32], ALU.add, [bP[3], bEBE], [bEBE])
            self.CP("dve", EBE[:, 31:32], P3v[:, 31:32, 63], [bP[3]], [bEBE])
            self.ACT(EBE, EBE, AF.Exp, [bEBE], [bEBE])
            self.ACT(P[1], P[3], AF.Exp, [bP[3]], [bP[1]])
            self.TT("pool", P[0], P[0], P[1], ALU.mult, [bP[0], bP[1]], [bP[0]])
            self.ACT(P[1], P[3], AF.Exp, [bP[3], bP[1]], [bP[1]], scale=-1.0)
            self.TT("pool", P[2], P[2], P[1], ALU.mult, [bP[2], bP[1]], [bP[2]])
            if self.dbg == "ew":
                self.S.sync_all()
                for i in range(4):
                    self.LD(self.dbg_out[:, i, :], P[i], bP[i], r=[bP[i]])
                return
            self.MS("dve", ST[0], 0.0, [bST[0]])
            cur = 0

            def pre(blk):
                import os as _os
                _pm = _os.environ.get("PRE_MODE", "")
                s = blk % 2
                cs = slice(blk * 128, (blk + 1) * 128)
                if "a" in _pm:
                    s = 0
                if "b" in _pm:
                    cs = slice(0, 128)
                sp_, sm_ = s, s
                if "c" in _pm:
                    sm_ = 0
                if "d" in _pm:
                    sp_ = 0
                self.chain(SCP[sp_], [(P[2][:, cs], P[0][:, cs])], [bP[2], bP[0]], [bSCP[sp_]])
                if "t" not in _os.environ.get("BLK_FL", "t"):
                    self.TT("dve", SCM[sm_], SCP[sp_], self.MASKH[:], ALU.mult, [bSCP[sp_], self.bCONST], [bSCM[sm_]])
                    return
                import os as _os
                if "t" in _os.environ.get("BLK_FL", "t"):
                    S.op("pe", lambda e: e.transpose(KTP[s], P[2][:, cs], self.IDENT[:]), [bP[2], self.bCONST], [bKTP[s]])
                    self.CP("act", KDT[s], KTP[s], [bKTP[s]], [bKDT[s]])
                self.MS("pool", SCM[s], 0.0, [bSCM[s]])
                S.op("dve", lambda e: e.copy_predicated(out=SCM[s], mask=self.MASKU[:], data=SCP[s]), [bSCP[s], self.bCONST, bSCM[s]], [bSCM[s]])
            pre(0)
            import os as _os
            _NB = int(_os.environ.get("BLK_N", "16")); _FL = _os.environ.get("BLK_FL", "osu")
            for blk in range(_NB):
                s = blk % 2
                if blk + 1 < 16:
                    pre(blk + 1)
                ob = 4 + (blk // 4) % 2
                for ch in range(2):
                    c0 = blk * 128 + ch * 64
                    oc = (blk % 4) * 128 + ch * 64
                    rows = slice(ch * 64, (ch + 1) * 64)
                    if "o" in _FL:
                        self.chain(self.PSb(ob, oc, oc + 64),
                               [(ST[cur], P[0][:, c0:c0 + 64]), (VH[:, blk, :], SCM[s][:, ch * 64:(ch + 1) * 64])],
                               [bST[cur], bP[0], bVH, bSCM[s]], [bPS[ob]])
                    if "s" not in _FL:
                        continue
                    self.chain(STP[cur], [(self.IDENT[:], ST[cur]), (KDT[s][rows, :], VH[rows, blk, :])],
                               [self.bCONST, bST[cur], bKDT[s], bVH], [bSTP[cur]])
                    eb = EBE[:, 2 * blk + ch:2 * blk + ch + 1]
                    if "u" not in _FL:
                        continue
                    if ch == 0:
                        self.ACT(ST[1 - cur], STP[cur], AF.Identity, [bSTP[cur], bEBE], [bST[1 - cur]], scale=eb)
                    else:
                        self.TS("dve", ST[1 - cur], STP[cur], eb, None, ALU.mult, None, [bSTP[cur], bEBE], [bST[1 - cur]])
                    cur = 1 - cur
                if blk % 4 == 3 and "o" in _FL:
                    tt = blk // 4
                    self.CP("act", P[3][:, tt * 512:(tt + 1) * 512], self.PSb(ob), [bPS[ob]], [bP[3]])
            if self.dbg == "blk":
                self.S.sync_all()
                self.LD(self.dbg_out[:, 0, :], P[3], bP[3], r=[bP[3]])
                return
            self.ACT(P[1], P[3], AF.Square, [bP[3]], [bP[1]])
            for tt in range(4):
                bk = pr[0] % 4
                pr[0] += 1
                cs = slice(tt * 512, (tt + 1) * 512)
                self.chain(self.PSb(bk), [(self.ONESH[:], P[1][:, cs])], [bP[1], self.bCONST], [bPS[bk]])
                self.ACT(P[2][:, cs], self.PSb(bk), AF.Ln, [bPS[bk]], [bP[2]], bias=RMS_EPS)
            self.ACT(P[2], P[2], AF.Exp, [bP[2]], [bP[2]], scale=-0.5)
            proj_fm(wsf, bws, 384, lambda tt, ps, bps: self.ACT(P[0][:, tt * 512:(tt + 1) * 512], ps, AF.Silu, [bps], [bP[0]]))
            self.STT("dve", P[1], P[3], self.CUR[:, 80 + h:81 + h], P[2], ALU.mult, ALU.mult, [bP[3], bP[2], self.bCUR], [bP[1]])
            self.TT("pool", OA[:, h, :], P[1], P[0], ALU.mult, [bP[1], bP[0]], [bOA])

        if self.dbg == "oa":
            self.dump_bf(OA, [bOA], 4)
            return
        QT2 = self.cv(AB, SC, [128, S_LEN], BF16); KT2 = self.cv(AB, SC + 4096, [128, S_LEN], BF16)
        VSB = self.cv(AB, SC + 8192, [128, 16, 512], BF16)
        bQK = bP[0]
        bVS = bP[1]
        bST2 = [bP[2], bP[3]]
        EE = [self.cv(AB, SC + 24576 + i * 2048, [128, 512], F32) for i in range(2)]
        SPp = [self.cv(AB, SC + 28672 + i * 2048, [128, 512], F32) for i in range(2)]
        LW = [self.cv(AB, SC + 32768 + i * 2048, [128, 512], F32) for i in range(2)]
        WT = [self.cv(AB, SC + 36864 + i * 1024, [128, 512], BF16) for i in range(2)]
        bEE = [bVH, bRM]; bSP = [bSCM[0], bSCM[1]]; bLW = [bKDT[0], bKDT[1]]; bWT = [bST[0], bST[1]]
        S.sync_all()
        ws, bws = load_ws(OFF_SV, 4096)
        ws3 = ws[:, 0:4096].rearrange("p (c j) -> p c j", c=8)
        for t in range(16):
            bk = 4 + t % 4
            self.chain(self.PSb(bk), [(HM[:, cc, t * 128:(t + 1) * 128], ws3[:, cc, :]) for cc in range(8)], [bws, bHM], [bPS[bk]])
            self.CP("act" if t % 2 == 0 else "dve", VSB[:, t, :], self.PSb(bk), [bPS[bk]], [bVS])
        step = [0]
        for hp in range(4):
            ws, bws = load_ws(OFF_QK + hp * 2048, 2048)
            wsf = ws[:, 0:2048]
            for k, dst in ((0, QT2), (1, KT2)):
                for tt in range(4):
                    bk = 4 + pr[0] % 2
                    pr[0] += 1
                    self.chain(self.PSb(bk), [(wsf.rearrange("p (c j) -> p c j", c=8)[:, cc, k * 128:(k + 1) * 128], HM[:, cc, tt * 512:(tt + 1) * 512]) for cc in range(8)],
                               [bws, bHM], [bPS[bk]])
                    self.CP("act" if tt % 2 == 0 else "dve", dst[:, tt * 512:(tt + 1) * 512], self.PSb(bk), [bPS[bk]], [bQK])
            for ct in range(4):
                Ah = [self.PSb(2), self.PSb(3)]; bAh = [bPS[2], bPS[3]]
                Oh = [self.PS[0:64, 6 * 512:7 * 512], self.PS[0:64, 7 * 512:8 * 512]]; bOh = [bPS[6], bPS[7]]
                for hh in range(2):
                    self.chain(Ah[hh], [(self.ZEROB[:], HM[:, 0, 0:512])], [self.bCONST, bHM], [bAh[hh]])
                    self.chain(Oh[hh], [(self.ZEROB[:, 0:64], HM[:, 0, 0:512])], [self.bCONST, bHM], [bOh[hh]])
                for kb in range(4 * ct + 3, -1, -1):
                    first = max(kb, 4 * ct)
                    c0, c1 = first * 128, (4 * ct + 4) * 128
                    w = c1 - c0
                    lo = c0 - ct * 512
                    diag = kb >= 4 * ct

                    def mk(hh):
                        h = 2 * hp + hh
                        rows = slice(hh * 64, (hh + 1) * 64)
                        sl = hh
                        A, bA, O, bO = Ah[hh], bAh[hh], Oh[hh], bOh[hh]
                        Z = self.PSb(sl, 0, w); bZ = bPS[sl]

                        def acc(dst, lhsT, rhs_t, r, wb):
                            S.op("pe", lambda e: e.matmul(dst[:, lo:lo + w], lhsT=lhsT, rhs=rhs_t[:, 0:w], start=False, stop=True), r, wb)

                        def st0():
                            self.chain(Z, [(KT2[rows, kb * 128:(kb + 1) * 128], QT2[rows, c0:c1])], [bQK], [bZ])

                        def st1():
                            self.ACT(EE[sl][:, 0:w], Z, AF.Exp, [bZ], [bEE[sl]], scale=0.125)

                        def st2():
                            self.ACT(SPp[sl][:, 0:w], EE[sl][:, 0:w], AF.Ln, [bEE[sl]], [bSP[sl]], bias=1.0)

                        def st3():
                            if diag:
                                S.op("pool", lambda e: e.affine_select(out=SPp[sl][:, 0:128], in_=SPp[sl][:, 0:128], pattern=[[1, 128]],
                                                                       compare_op=ALU.is_gt, fill=0.0, base=0, channel_multiplier=-1), [bSP[sl]], [bSP[sl]])

                        def st4():
                            acc(A, self.TRI[:], SPp[sl], [bSP[sl], self.bCONST], [bA])

                        def st5():
                            self.STT("dve", LW[sl][:, 0:w], Z, 0.125, SPp[sl][:, 0:w], ALU.mult, ALU.subtract, [bZ, bSP[sl]], [bLW[sl]])

                        def st6():
                            self.TT("dve", LW[sl][:, 0:w], LW[sl][:, 0:w], A[:, lo:lo + w], ALU.subtract, [bLW[sl], bA], [bLW[sl]])

                        def st7():
                            self.ACT(WT[sl][:, 0:w], LW[sl][:, 0:w], AF.Exp, [bLW[sl]], [bWT[sl]])

                        def st8():
                            if diag:
                                S.op("pool", lambda e: e.affine_select(out=WT[sl][:, 0:128], in_=WT[sl][:, 0:128], pattern=[[1, 128]],
                                                                       compare_op=ALU.is_gt, fill=0.0, base=0, channel_multiplier=-1), [bWT[sl]], [bWT[sl]])

                        def st9():
                            acc(O, VSB[:, kb, h * 64:(h + 1) * 64], WT[sl], [bVS, bWT[sl]], [bO])
                            if kb > 0:
                                acc(A, self.TRIC[:], SPp[sl], [bSP[sl], self.bCONST], [bA])
                        return [st0, st1, st2, st3, st4, st5, st6, st7, st8, st9]
                    stg = [mk(0), mk(1)]
                    for si in range(10):
                        for hh in range(2):
                            stg[hh][si]()
                for hh in range(2):
                    self.CP("act", OB[hh * 64:(hh + 1) * 64, hp, ct * 512:(ct + 1) * 512], Oh[hh], [bOh[hh]], [bOB])
        if self.dbg == "ob":
            self.dump_bf(OB, [bOB], 4)
            return
        S.sync_all()
        MT = self.cv(AB, SC, [128, 8, S_LEN], BF16); bMT = self.B("mt")
        SG = [self.cv(AB, SC + 32768 + i * 2048, [128, 512], F32) for i in range(4)]
        bSG = [self.B() for _ in range(4)]
        it = 0
        for j in range(8):
            ws, bws = load_ws(OFF_MG + j * 3072, 3072)
            ga = ws[:, 0:1024].rearrange("p (c j) -> p c j", c=8)
            gb = ws[:, 1024:2048].rearrange("p (c j) -> p c j", c=8)
            ua = ws[:, 2048:2560].rearrange("p (c j) -> p c j", c=4)
            ub = ws[:, 2560:3072].rearrange("p (c j) -> p c j", c=4)
            for tt in range(4):
                cs = slice(tt * 512, (tt + 1) * 512)
                pb = (it % 2) * 4
                it += 1
                self.chain(self.PSb(pb), [(ga[:, cc, :], HM[:, cc, cs]) for cc in range(8)], [bws, bHM], [bPS[pb]])
                self.chain(self.PSb(pb + 1), [(ua[:, hh, :], OA[:, hh, cs]) for hh in range(4)], [bws, bOA], [bPS[pb + 1]])
                self.chain(self.PSb(pb + 2), [(gb[:, cc, :], HM[:, cc, cs]) for cc in range(8)], [bws, bHM], [bPS[pb + 2]])
                self.chain(self.PSb(pb + 3), [(ub[:, hh, :], OB[:, hh, cs]) for hh in range(4)], [bws, bOB], [bPS[pb + 3]])
                self.ACT(SG[0], self.PSb(pb), AF.Sigmoid, [bPS[pb]], [bSG[0]])
                self.ACT(SG[1], self.PSb(pb + 2), AF.Sigmoid, [bPS[pb + 2]], [bSG[1]])
                self.TT("dve", SG[2], SG[0], self.PSb(pb + 1), ALU.mult, [bSG[0], bPS[pb + 1]], [bSG[2]])
                self.TT("dve", SG[3], SG[1], self.PSb(pb + 3), ALU.mult, [bSG[1], bPS[pb + 3]], [bSG[3]])
                self.TT("pool", MT[:, j, cs], SG[2], SG[3], ALU.add, [bSG[2], bSG[3]], [bMT])
        it = 0
        for jo in range(8):
            ws, bws = load_ws(OFF_WO + jo * 1024, 1024)
            wo = ws[:, 0:1024].rearrange("p (c j) -> p c j", c=8)
            for tt in range(4):
                cs = slice(tt * 512, (tt + 1) * 512)
                pb = it % 4
                it += 1
                self.chain(self.PSb(pb), [(wo[:, jj, :], MT[:, jj, cs]) for jj in range(8)], [bws, bMT], [bPS[pb]])
                self.STT("dve", self.Xt[:, jo, cs], self.PSb(pb), self.mod(2, jo), self.Xt[:, jo, cs], ALU.mult, ALU.add,
                         [bPS[pb], self.bCUR, self.bX[tt]], [self.bX[tt]])
        S.sync_all()
        self.layer_norm(0, SC)
        if self.dbg == "ln1":
            self.dump_x()

    def dump_x(self):
        for cc in range(8):
            self.LD(self.dbg_out[:, cc, :], self.Xt[:, cc, :], self.bX[0], r=self.bX)

    def dump_bf(self, src, bufs, n):
        T = self.cv(self.AB, 16384, [128, S_LEN], F32)
        bT = self.B()
        self.S.sync_all()
        for i in range(n):
            self.CP("dve", T, src[:, i, :], bufs, [bT])
            self.LD(self.dbg_out[:, i, :], T, bT, r=[bT])

    def phase2(self):
        nc, S = self.nc, self.S
        l = self.l
        AB = self.AB
        W = self.WSC[l]
        self.G = self.cv(self.AA, 0, [128, 256, 128], BF16); self.bG = self.B("G")
        self.UT = [self.cv(AB, i * 8192, [128, 4, 8, 128], BF16) for i in range(2)]
        self.VV = [self.cv(AB, 16384 + i * 8192, [128, 4, 1024], BF16) for i in range(2)]
        self.bUT = [self.B("ut0"), self.B("ut1")]
        self.bVV = [self.B("v0"), self.B("v1")]
        self.HFT = self.cv(AB, 32768, [128, 8, 256], BF16); self.bHFT = self.B("hft")
        self.QT = self.cv(AB, 36864, [128, 8, 256], BF16); self.bQT = self.B("qt")
        self.WQ = self.cv(AB, 16384, [128, 8, 8, 128], BF16)
        self.KBD = self.cv(AB, 40960, [128, 8, 256], BF16); self.bKBD = self.B("kbd")
        KBF = self.cv(AB, 0, [128, 8, 256], F32)
        bKF = self.bUT[0]
        self.MS("pool", KBF, 0.0, [bKF])
        self.LD(KBF[0:64, :, 0:128], self.skT[l, 0].rearrange("h d n -> d h n"), bKF, w=[bKF])
        self.LD(KBF[64:128, :, 128:256], self.skT[l, 1].rearrange("h d n -> d h n"), bKF, w=[bKF])
        self.CP("dve", self.KBD, KBF, [bKF], [self.bKBD])
        S.sync_all()
        self.run_loop(self.ntiles, self.peer_tile)
        if self.dbg == "peer":
            self.dump_x()
            return
        self.layer_norm(1, 0)
        if self.dbg == "ln2":
            self.dump_x()

    def peer_tile(self, tq):
        nc, S = self.nc, self.S
        l = self.l
        AB = self.AB
        bPS = self.bPS
        bXa = self.bX
        tsl = bass.ts(tq, 256)
        HFT, QT, WQ, KBD, G = self.HFT, self.QT, self.WQ, self.KBD, self.G
        XC = self.cv(AB, 8192, [128, 8, 256], F32)
        self.CP("pool", XC, self.Xt[:, :, tsl], bXa, [self.bUT[1]])
        for cc in range(8):
            if cc % 2 == 0:
                self.ACT(HFT[:, cc, :], XC[:, cc, :], AF.Identity, [self.bUT[1], self.bCUR], [self.bHFT], bias=self.mod(3, cc), scale=self.mod(4, cc))
            else:
                self.TS("dve", HFT[:, cc, :], XC[:, cc, :], self.mod(4, cc), self.mod(3, cc), ALU.mult, ALU.add, [self.bUT[1], self.bCUR], [self.bHFT])
        self.LD(WQ.rearrange("p a b c -> p (a b c)"), self.WSC[l][:, OFF_PQ:OFF_PQ + 8192], self.bVV[0], r=[self.bWSC], w=self.bVV)
        for h in range(8):
            bk = 4 + h % 4
            self.chain(self.PSb(bk, 0, 256), [(WQ[:, h, cc, :], HFT[:, cc, :]) for cc in range(8)], self.bVV + [self.bHFT], [bPS[bk]])
            self.CP("act" if h % 2 == 0 else "dve", QT[:, h, :], self.PSb(bk, 0, 256), [bPS[bk]], [self.bQT])
        SS = self.cv(AB, 0, [128, 2048], F32); bSS = self.bUT[0]
        EQ = self.cv(AB, 8192, [128, 2048], F32); bEQ = self.bUT[1]
        T0 = 45056
        TOPV = self.cv(AB, T0, [128, 16, 16], F32)
        TOPI = self.cv(AB, T0 + 1024, [128, 16, 16], U32)
        TOPIF = self.cv(AB, T0 + 2048, [128, 16, 16], F32)
        SR = self.cv(AB, T0 + 3072, [128, 256], F32)
        CTOP = self.cv(AB, T0 + 4096, [128, 8, 16], F32)
        CPOS = self.cv(AB, T0 + 4608, [128, 128], U32)
        CAI = self.cv(AB, T0 + 5120, [128, 128], I32)
        CBI = self.cv(AB, T0 + 5632, [128, 128], I32)
        CAF = self.cv(AB, T0 + 6144, [128, 128], F32)
        CBF = self.cv(AB, T0 + 6656, [128, 128], F32)
        IDX = [self.cv(AB, T0 + 7168 + i * 512, [128, 128], F32) for i in range(3)]
        GSUM = self.cv(AB, T0 + 8704, [128, 8], F32)
        IDT = [self.cv(AB, T0 + 9216 + i * 1024, [128, 256], F32) for i in range(3)]
        OH0 = T0 + 12288
        EQ1 = [self.cv(AB, OH0 + i * 6144, [128, 8, 128], BF16) for i in range(2)]
        OH1 = [self.cv(AB, OH0 + 2048 + i * 6144, [128, 8, 128], BF16) for i in range(2)]
        OH2 = [self.cv(AB, OH0 + 4096 + i * 6144, [128, 8, 128], BF16) for i in range(2)]
        AG0 = OH0 + 6144
        AG = [self.cv(AB, AG0 + i * 1024, [128, 256], F32) for i in range(2)]
        WW = [self.cv(AB, AG0 + 2048 + i * 512, [128, 256], BF16) for i in range(2)]
        bTK = self.B("topk"); bSR = self.B("sr"); bIDX = self.B("idx"); bIDT = self.B("idt")
        bE1, bO1, bO2 = [self.B(), self.B()], [self.B(), self.B()], [self.B(), self.B()]
        bAG = [self.B(), self.B()]; bWW = [self.B(), self.B()]
        dv = lambda fn, r, w: S.op("dve", fn, r, w)
        for st in range(2):
            tcs = slice(st * 128, (st + 1) * 128)
            for h in range(8):
                bk = 4 + h // 2
                self.chain(self.PSb(bk, (h % 2) * 256, (h % 2) * 256 + 256), [(QT[:, h, tcs], KBD[:, h, :])], [self.bQT, self.bKBD], [bPS[bk]])
            for k in range(4):
                self.CP("act" if k % 2 == 0 else "dve", SS[:, k * 512:(k + 1) * 512], self.PSb(4 + k), [bPS[4 + k]], [bSS])
            SR16 = EQ.rearrange("p (g n) -> p g n", g=16)
            gb = [[self.B() for _ in range(16)] for _ in range(5)]
            vs = [SS[:, g * 128:(g + 1) * 128] for g in range(16)]
            for g in range(16):
                dv(lambda e, g=g: e.max(out=TOPV[:, g, 0:8], in_=vs[g]), [bSS, bTK], [gb[0][g]])
            for g in range(16):
                dv(lambda e, g=g: e.match_replace(out=SR16[:, g, :], in_to_replace=TOPV[:, g, 0:8], in_values=vs[g], imm_value=-1e30), [bSS, gb[0][g], bEQ], [gb[1][g]])
            for g in range(16):
                dv(lambda e, g=g: e.max(out=TOPV[:, g, 8:16], in_=SR16[:, g, :]), [gb[1][g]], [gb[2][g]])
            for g in range(16):
                dv(lambda e, g=g: e.max_index(out=TOPI[:, g, 0:8], in_max=TOPV[:, g, 0:8], in_values=vs[g]), [bSS, gb[0][g]], [gb[3][g]])
                dv(lambda e, g=g: e.max_index(out=TOPI[:, g, 8:16], in_max=TOPV[:, g, 8:16], in_values=vs[g]), [bSS, gb[2][g]], [gb[4][g]])
            allg = [x for row in gb for x in row]
            self.CP("dve", TOPIF, TOPI, allg, [bTK] + allg)
            self.CP("dve", TOPIF, TOPI, [bTK], [bTK])
            TV4 = TOPV.rearrange("p (h two) k -> p h two k", two=2)
            TI4 = TOPIF.rearrange("p (h two) k -> p h two k", two=2)
            self.TT("dve", SS.rearrange("p (h a b) -> p h a b", h=8, a=16),
                    TV4[:, :, 0, :, None].broadcast_to([128, 8, 16, 16]), TV4[:, :, 1, None, :].broadcast_to([128, 8, 16, 16]), ALU.add, [bTK], [bSS])
            SRC = EQ.rearrange("p (h n) -> p h n", h=8)
            hb = [[self.B() for _ in range(8)] for _ in range(5)]
            cvs = [SS[:, h * 256:(h + 1) * 256] for h in range(8)]
            for h in range(8):
                dv(lambda e, h=h: e.max(out=CTOP[:, h, 0:8], in_=cvs[h]), [bSS, bTK], [hb[0][h]])
            for h in range(8):
                dv(lambda e, h=h: e.match_replace(out=SRC[:, h, :], in_to_replace=CTOP[:, h, 0:8], in_values=cvs[h], imm_value=-1e30), [bSS, hb[0][h], bEQ], [hb[1][h]])
            for h in range(8):
                dv(lambda e, h=h: e.max(out=CTOP[:, h, 8:16], in_=SRC[:, h, :]), [hb[1][h]], [hb[2][h]])
            for h in range(8):
                dv(lambda e, h=h: e.max_index(out=CPOS[:, h * 16:h * 16 + 8], in_max=CTOP[:, h, 0:8], in_values=cvs[h]), [bSS, hb[0][h]], [hb[3][h]])
                dv(lambda e, h=h: e.max_index(out=CPOS[:, h * 16 + 8:h * 16 + 16], in_max=CTOP[:, h, 8:16], in_values=cvs[h]), [bSS, hb[2][h]], [hb[4][h]])
            allh = [x for row in hb for x in row]
            S.op("dve", lambda e: e.tensor_single_scalar(out=CAI, in_=CPOS.bitcast(I32), scalar=4, op=ALU.arith_shift_right), allh, [bTK, bEQ] + allh)
            dv(lambda e: e.tensor_single_scalar(out=CBI, in_=CPOS.bitcast(I32), scalar=15, op=ALU.bitwise_and), [bTK], [bTK])
            self.CP("dve", CAF, CAI, [bTK], [bTK])
            self.CP("dve", CBF, CBI, [bTK], [bTK])
            for half, src in ((0, CAF), (1, CBF)):
                self.TT("dve", EQ.rearrange("p (k a) -> p k a", a=16), src[:, :, None].broadcast_to([128, 128, 16]),
                        self.IOTA[:, None, 0:16].broadcast_to([128, 128, 16]), ALU.is_equal, [bTK, self.bCONST], [bEQ])
                self.TT("dve", EQ.rearrange("p (h k a) -> p h k a", h=8, k=16), EQ.rearrange("p (h k a) -> p h k a", h=8, k=16),
                        TI4[:, :, half, None, :].broadcast_to([128, 8, 16, 16]), ALU.mult, [bEQ, bTK], [bEQ])
                dv(lambda e, half=half: e.tensor_reduce(out=IDX[half], in_=EQ.rearrange("p (k a) -> p k a", a=16), axis=AX.X, op=ALU.add), [bEQ], [bIDX])
            G3 = IDX[2].rearrange("p (h k) -> p h k", k=16)
            self.TT("dve", G3, CTOP, CTOP[:, :, 0:1].broadcast_to([128, 8, 16]), ALU.subtract, [bTK], [bIDX])
            self.ACT(IDX[2], IDX[2], AF.Exp, [bIDX], [bIDX])
            dv(lambda e: e.tensor_reduce(out=GSUM, in_=G3, axis=AX.X, op=ALU.add), [bIDX], [bTK])
            dv(lambda e: e.reciprocal(out=GSUM, in_=GSUM), [bTK], [bTK])
            self.TT("dve", G3, G3, GSUM[:, :, None].broadcast_to([128, 8, 16]), ALU.mult, [bIDX, bTK], [bIDX])
            for i in range(3):
                bk = 4 + i
                S.op("pe", lambda e, i=i, bk=bk: e.transpose(self.PSb(bk, 0, 128), IDX[i], self.IDENT[:]), [bIDX, self.bCONST], [bPS[bk]])
                self.CP("act" if i % 2 == 0 else "dve", IDT[i][:, tcs], self.PSb(bk, 0, 128), [bPS[bk]], [bIDT])
        for tb in range(32):
            t0 = tb * 8
            k = tb % 2
            pb = 4 + k * 2
            iob = self.IOTA[:, None, :].broadcast_to([128, 8, 128])
            self.TT("dve", EQ1[k], iob, IDT[0][:, t0:t0 + 8, None].broadcast_to([128, 8, 128]), ALU.is_equal, [bIDT, self.bCONST], [bE1[k]])
            self.TT("pool", OH1[k], EQ1[k], IDT[2][:, t0:t0 + 8, None].broadcast_to([128, 8, 128]), ALU.mult, [bE1[k], bIDT], [bO1[k]])
            self.TT("dve", OH2[k], iob, IDT[1][:, t0:t0 + 8, None].broadcast_to([128, 8, 128]), ALU.is_equal, [bIDT, self.bCONST], [bO2[k]])

            def gmm(e, pb=pb, k=k):
                ins = None
                for j in range(8):
                    ins = e.matmul(self.PS[:, pb * 512 + j * 128:pb * 512 + (j + 1) * 128], lhsT=OH1[k][:, j, :], rhs=OH2[k][:, j, :], start=True, stop=True)
                return ins
            S.op("pe", gmm, [bO1[k], bO2[k]], [bPS[pb], bPS[pb + 1]])
            self.CP("act", G[:, t0:t0 + 4, :], self.PSb(pb).rearrange("p (a b) -> p a b", a=4), [bPS[pb]], [self.bG])
            self.CP("act", G[:, t0 + 4:t0 + 8, :], self.PSb(pb + 1).rearrange("p (a b) -> p a b", a=4), [bPS[pb + 1]], [self.bG])
        UTS, VS = self.UTS[l], self.VS[l]

        def load_u(s):
            sl = s % 2
            self.LD(self.UT[sl].rearrange("p a b c -> p (a b c)"), UTS[:, s * 4096:(s + 1) * 4096], self.bUT[sl], r=[self.bUV], w=[self.bUT[sl]])

        def load_v(s):
            sl = s % 2
            self.LD(self.VV[sl].rearrange("p a b -> p (a b)"), VS[:, s * 4096:(s + 1) * 4096], self.bVV[sl], r=[self.bUV], w=[self.bVV[sl]])

        def a_chain(i2):
            s, q = i2 // 4, i2 % 4
            bk = 4 + i2 % 4
            self.chain(self.PSb(bk, 0, 256), [(self.UT[s % 2][:, q, cc, :], HFT[:, cc, :]) for cc in range(8)], [self.bUT[s % 2], self.bHFT], [bPS[bk]])
            if q == 3 and s + 2 < 32:
                load_u(s + 2)
        for bk in range(4):
            self.chain(self.PSb(bk), [(self.ZEROB[:], G[:, 0:4, :].rearrange("p a b -> p (a b)"))], [self.bCONST, self.bG], [bPS[bk]])
        load_u(0); load_v(0); load_u(1); load_v(1)
        a_chain(0)
        a_chain(1)
        for i2 in range(128):
            s, q = i2 // 4, i2 % 4
            if i2 + 2 < 128:
                a_chain(i2 + 2)
            bk = 4 + i2 % 4
            a = i2 % 2
            self.ACT(AG[a], self.PSb(bk, 0, 256), AF.Gelu, [bPS[bk]], [bAG[a]])
            self.TT("dve", WW[a], AG[a], G[:, :, i2], ALU.mult, [bAG[a], self.bG], [bWW[a]])

            def ymm(e, i2=i2, s=s, q=q, a=a):
                ins = None
                for dc in range(8):
                    ins = e.matmul(self.PS[:, dc * 256:(dc + 1) * 256], lhsT=self.VV[s % 2][:, q, dc * 128:(dc + 1) * 128], rhs=WW[a],
                                   start=False, stop=True)
                return ins
            S.op("pe", ymm, [self.bVV[s % 2], bWW[a]], bPS[0:4])
            if q == 3 and s + 2 < 32:
                load_v(s + 2)
        YT = self.cv(AB, 0, [128, 8, 256], F32)
        for dc in range(8):
            if dc % 2 == 0:
                self.TS("dve", YT[:, dc, :], self.PS[:, dc * 256:(dc + 1) * 256], self.mod(5, dc), None, ALU.mult, None, [bPS[dc // 2], self.bCUR], [self.bUT[0]])
            else:
                self.ACT(YT[:, dc, :], self.PS[:, dc * 256:(dc + 1) * 256], AF.Identity, [bPS[dc // 2], self.bCUR], [self.bUT[0]], scale=self.mod(5, dc))
        self.TT("dve", self.Xt[:, :, tsl], self.Xt[:, :, tsl], YT, ALU.add, [self.bUT[0]] + bXa, bXa)


_CACHE = {}


def _prep_weights(w_ada, b_ada, w_in, lb_logits, hg_norm_g, w_up_a, w_up_b, w_o, w_pq, sub_keys, peer_u, peer_v, ln_g, ln_b):
    nl = w_ada.shape[0]
    f = lambda a: np.ascontiguousarray(np.asarray(a, dtype=np.float32))
    d = {}
    d["w_ada"] = f(w_ada); d["w_in"] = f(w_in); d["w_up_a"] = f(w_up_a); d["w_up_b"] = f(w_up_b)
    d["w_o"] = f(w_o); d["w_pq"] = f(w_pq)
    d["bada"] = f(np.asarray(b_ada).reshape(nl, 48, 128).transpose(2, 0, 1))
    d["lbl"] = f(np.asarray(lb_logits).reshape(nl, 4, 128).transpose(2, 0, 1))
    d["hgg"] = f(np.asarray(hg_norm_g).reshape(nl, 4, 128).transpose(2, 0, 1))
    d["lng"] = f(np.asarray(ln_g).reshape(nl, 2, 8, 128).transpose(3, 0, 1, 2))
    d["lnb"] = f(np.asarray(ln_b).reshape(nl, 2, 8, 128).transpose(3, 0, 1, 2))
    d["skT"] = f(np.asarray(sub_keys).transpose(0, 1, 2, 4, 3))
    d["uT"] = f(np.asarray(peer_u).reshape(nl, 128, 128, 8, 128).transpose(0, 4, 2, 3, 1)).reshape(nl, 128, 128 * 1024)
    d["pv"] = f(peer_v).reshape(nl, 128, 128 * 1024)
    return d


def kernel(x, c, w_ada, b_ada, w_in, lb_logits, hg_norm_g, w_up_a, w_up_b, w_o, w_pq, sub_keys, peer_u, peer_v, ln_g, ln_b):
    x = np.asarray(x, dtype=np.float32)
    c = np.asarray(c, dtype=np.float32)
    B = x.shape[0]
    nseq = B // NCORES
    key = ("full", nseq)
    if key not in _CACHE:
        _CACHE[key] = MK(nseq=nseq, nl=DEPTH)
    mk = _CACHE[key]
    wd = _prep_weights(w_ada, b_ada, w_in, lb_logits, hg_norm_g, w_up_a, w_up_b, w_o, w_pq, sub_keys, peer_u, peer_v, ln_g, ln_b)
    in_maps = []
    for i in range(NCORES):
        xs = x[i * nseq:(i + 1) * nseq]
        xT = np.ascontiguousarray(xs.reshape(nseq, S_LEN, 8, 128).transpose(0, 3, 2, 1))
        cs = c[i * nseq:(i + 1) * nseq]
        cT = np.ascontiguousarray(cs.reshape(nseq, 8, 128).transpose(2, 1, 0))
        m = dict(wd)
        m["xT"] = xT
        m["cT"] = cT
        in_maps.append(m)
    res = run_bass_kernel_spmd(mk.nc, in_maps, core_ids=list(range(NCORES)))
    out = np.empty((B, S_LEN, D), dtype=np.float32)
    for i in range(NCORES):
        oT = np.asarray(res.results[i]["outT"])
        out[i * nseq:(i + 1) * nseq] = oT.transpose(0, 3, 2, 1).reshape(nseq, S_LEN, D)
    return out
```

```python
from contextlib import ExitStack
import numpy as np
import concourse.bass as bass
import concourse.mybir as mybir
from concourse.bass_utils import run_bass_kernel_spmd

F32 = mybir.dt.float32
BF16 = mybir.dt.bfloat16
U32 = mybir.dt.uint32
I32 = mybir.dt.int32
U8 = mybir.dt.uint8
AF = mybir.ActivationFunctionType
ALU = mybir.AluOpType
AX = mybir.AxisListType

D = 1024
S_LEN = 2048
NCORES = 8
DEPTH = 4
BATCH = 32
ALPHA = (2.0 * DEPTH) ** 0.25
LN_EPS = 1e-5
RMS_EPS = 1e-6
IN_W = 5632
OFF_HG = 0
OFF_QK = OFF_HG + 4 * 4096
OFF_SV = OFF_QK + 4 * 2048
OFF_MG = OFF_SV + 4096
OFF_WO = OFF_MG + 8 * 3072
OFF_PQ = OFF_WO + 8 * 1024
NWS = OFF_PQ + 8192


class Buf:
    def __init__(self, S, name=""):
        self.name = name
        self.w = None
        self.r = {}
        S.bufs.append(self)


class Sched:
    def __init__(self, nc):
        self.nc = nc
        self.eng = {"pe": nc.tensor, "act": nc.scalar, "dve": nc.vector, "pool": nc.gpsimd, "sp": nc.sync}
        self.same = {"act", "dve", "pool"}
        self.es = ExitStack()
        self.semh = {}
        self.cnt = {}
        self.waited = {e: {} for e in self.eng}
        self.bufs = []
        self.n = 0
        self.pool = [self.es.enter_context(nc.semaphore("sp%d" % i)) for i in range(48)]
        self.npool = 0
        for e in self.eng:
            self._sem(e)

    def sb(self, name, shape, dt):
        return self.es.enter_context(self.nc.sbuf_tensor(name, list(shape), dt))

    def ps(self, name, shape, dt):
        return self.es.enter_context(self.nc.psum_tensor(name, list(shape), dt))

    def _sem(self, key):
        if key not in self.semh:
            self.semh[key] = self.pool[self.npool]
            self.npool += 1
            self.cnt[key] = 0
        return self.semh[key]

    def _waits(self, e, reads, writes):
        waits = {}

        def need(k, v):
            if waits.get(k, 0) < v:
                waits[k] = v
        for b in reads:
            if b.w is not None:
                need(*b.w)
        for b in writes:
            if b.w is not None:
                need(*b.w)
            for k, v in b.r.items():
                if k != e:
                    need(k, v)
        eng = self.eng[e]
        for k, v in waits.items():
            if k == e and e not in self.same:
                continue
            if self.waited[e].get(k, 0) >= v:
                continue
            self.waited[e][k] = v
            eng.wait_ge(self.semh[k], v)
            self.n += 1

    def op(self, e, fn, reads=(), writes=()):
        self._sem(e)
        self._waits(e, reads, writes)
        ins = fn(self.eng[e])
        ins.then_inc(self.semh[e], 1)
        self.cnt[e] += 1
        idx = self.cnt[e]
        for b in reads:
            b.r[e] = idx
        for b in writes:
            b.w = (e, idx)
            b.r = {}
        self.n += 1

    def dma(self, q, fn, key_buf, reads=(), writes=()):
        key = ("dma", id(key_buf))
        self._sem(key)
        self._sem(q)
        self._waits(q, reads, writes)
        ins = fn(self.eng[q])
        ins.then_inc(self.semh[key], 16)
        self.cnt[key] += 16
        val = self.cnt[key]
        for b in reads:
            b.r[key] = val
        for b in writes:
            b.w = (key, val)
            b.r = {}
        self.n += 1

    def sync_all(self):
        nc = self.nc
        for key, h in self.semh.items():
            if isinstance(key, tuple) and self.cnt[key] > 0:
                nc.sync.wait_ge(h, self.cnt[key])
        nc.all_engine_barrier()
        for h in self.pool:
            nc.gpsimd.sem_clear(h)
        nc.all_engine_barrier()
        for k in self.cnt:
            self.cnt[k] = 0
        for e in self.waited:
            self.waited[e] = {}
        for b in self.bufs:
            b.w = None
            b.r = {}


class MK:
    def __init__(self, nseq=4, nl=4, hw_loops=True, ntiles=8, dbg=None, stop_after=None):
        self.nseq, self.nl, self.hw, self.ntiles, self.dbg, self.stop_after = nseq, nl, hw_loops, ntiles, dbg, stop_after
        self.nc = bass.Bass("TRN2", target_bir_lowering=False)
        self.S = Sched(self.nc)
        self.build()

    def B(self, name=""):
        return Buf(self.S, name)

    def chain(self, out, pairs, r, w, first=True, last=True):
        def fn(e):
            n = len(pairs)
            ins = None
            for i, (a, b) in enumerate(pairs):
                ins = e.matmul(out, lhsT=a, rhs=b, start=(first and i == 0), stop=(last and i == n - 1))
            return ins
        self.S.op("pe", fn, r, w)

    def ACT(self, out, in_, func, r, w, bias=None, scale=None):
        kw = {}
        if bias is not None:
            kw["bias"] = bias
        if scale is not None:
            kw["scale"] = scale
        self.S.op("act", lambda e: e.activation(out=out, in_=in_, func=func, **kw), r, w)

    def TT(self, eng, out, in0, in1, op, r, w):
        self.S.op(eng, lambda e: e.tensor_tensor(out=out, in0=in0, in1=in1, op=op), r, w)

    def TS(self, eng, out, in0, s1, s2, op0, op1, r, w):
        if s2 is None:
            self.S.op(eng, lambda e: e.tensor_scalar(out=out, in0=in0, scalar1=s1, scalar2=None, op0=op0), r, w)
        else:
            self.S.op(eng, lambda e: e.tensor_scalar(out=out, in0=in0, scalar1=s1, scalar2=s2, op0=op0, op1=op1), r, w)

    def STT(self, eng, out, in0, scalar, in1, op0, op1, r, w):
        self.S.op(eng, lambda e: e.scalar_tensor_tensor(out=out, in0=in0, scalar=scalar, in1=in1, op0=op0, op1=op1), r, w)

    def CP(self, eng, out, in_, r, w):
        if eng == "act":
            self.S.op("act", lambda e: e.copy(out=out, in_=in_), r, w)
        else:
            self.S.op(eng, lambda e: e.tensor_copy(out=out, in_=in_), r, w)

    def MS(self, eng, ap, val, w):
        self.S.op(eng, lambda e: e.memset(ap, val), (), w)

    def LD(self, out, in_, key, r=(), w=(), q="sp"):
        self.S.dma(q, lambda e: e.dma_start(out=out, in_=in_), key, r, w)

    def cv(self, arena, off, shape, dt):
        nb = {F32: 4, BF16: 2, U32: 4, I32: 4}[dt]
        n = int(np.prod(shape[1:])) * nb
        ap = arena[0:shape[0], off:off + n].bitcast(dt)
        if len(shape) == 3:
            ap = ap.rearrange("p (a b) -> p a b", a=shape[1])
        elif len(shape) == 4:
            ap = ap.rearrange("p (a b c) -> p a b c", a=shape[1], b=shape[2])
        return ap

    def run_loop(self, n, body):
        if self.hw and n > 1:
            with self.nc.Fori(0, n) as i:
                self.S.sync_all()
                body(i)
            self.S.sync_all()
        else:
            for i in range(n):
                self.S.sync_all()
                body(i)
            self.S.sync_all()

    def build(self):
        nc, S, nseq, nl = self.nc, self.S, self.nseq, self.nl
        dram = lambda name, shape, dt, kind: nc.dram_tensor(name, list(shape), dt, kind=kind).ap()
        self.xT = dram("xT", [nseq, 128, 8, S_LEN], F32, "ExternalInput")
        self.cT = dram("cT", [128, 8, nseq], F32, "ExternalInput")
        self.w_ada = dram("w_ada", [nl, D, 6 * D], F32, "ExternalInput")
        self.bada = dram("bada", [128, nl, 48], F32, "ExternalInput")
        self.w_in = dram("w_in", [nl, D, IN_W], F32, "ExternalInput")
        self.lbl = dram("lbl", [128, nl, 4], F32, "ExternalInput")
        self.hgg = dram("hgg", [128, nl, 4], F32, "ExternalInput")
        self.w_up_a = dram("w_up_a", [nl, 512, D], F32, "ExternalInput")
        self.w_up_b = dram("w_up_b", [nl, 512, D], F32, "ExternalInput")
        self.w_o = dram("w_o", [nl, D, D], F32, "ExternalInput")
        self.w_pq = dram("w_pq", [nl, D, D], F32, "ExternalInput")
        self.skT = dram("skT", [nl, 2, 8, 64, 128], F32, "ExternalInput")
        self.uT = dram("uT", [nl, 128, 128 * 1024], F32, "ExternalInput")
        self.pv = dram("pv", [nl, 128, 128 * 1024], F32, "ExternalInput")
        self.lng = dram("lng", [128, nl, 2, 8], F32, "ExternalInput")
        self.lnb = dram("lnb", [128, nl, 2, 8], F32, "ExternalInput")
        self.outT = dram("outT", [nseq, 128, 8, S_LEN], F32, "ExternalOutput")
        if self.dbg:
            self.dbg_out = dram("dbg_out", [128, 8, S_LEN], F32, "ExternalOutput")
        self.WSC = dram("wsc", [nl, 128, NWS], BF16, "Internal")
        self.UTS = dram("uts", [nl, 128, 128 * 1024], BF16, "Internal")
        self.VS = dram("vs", [nl, 128, 128 * 1024], BF16, "Internal")
        self.bWSC = self.B("wsc")
        self.bUV = self.B("uvs")
        self.Xt = S.sb("X", [128, 8, S_LEN], F32)
        self.bX = [self.B("X%d" % i) for i in range(4)]
        self.AA = S.sb("arenaA", [128, 65536], U8)
        self.AB = S.sb("arenaB", [128, 69 * 1024], U8)
        self.PS = S.ps("psum", [128, 4096], F32)
        self.bPS = [self.B("ps%d" % i) for i in range(8)]
        c = lambda n, sh, dt=F32: S.sb(n, sh, dt)
        self.IDENT = c("ident", [128, 128]); self.IOTA = c("iota", [128, 128])
        self.TRI = c("tri", [128, 128]); self.TRIC = c("tric", [128, 128])
        self.ONESD = c("onesd", [128, 128]); self.ONESH = c("onesh", [128, 128])
        self.MASKH = c("maskh", [128, 128]); self.MASKU = c("masku", [128, 128], U32)
        self.ZEROB = c("zerob", [128, 128], BF16)
        self.MODT = c("modt", [128, nseq, nl, 48])
        self.LNG = c("lngs", [128, nl, 2, 8]); self.LNB = c("lnbs", [128, nl, 2, 8])
        self.HGG = c("hggs", [128, nl, 4]); self.LBT = c("lbt", [128, nl, 4, 3])
        self.CUR = c("cur", [128, 48 + 16 + 16 + 4 + 12])
        self.bCUR = self.B("cur")
        self.bCONST = self.B("const")
        self.prologue()
        S.sync_all()
        if self.stop_after == "pro":
            return
        self.run_loop(nseq, self.seq_body)

    def PSb(self, i, lo=0, hi=512):
        return self.PS[:, i * 512 + lo:i * 512 + hi]

    def prologue(self):
        nc, S, nseq, nl = self.nc, self.S, self.nseq, self.nl
        bC = self.bCONST
        io = lambda out, base, cm, pat: S.op("pool", lambda e: e.iota(out, pattern=pat, base=base, channel_multiplier=cm,
                                                                     allow_small_or_imprecise_dtypes=True), (), [bC])
        io(self.IOTA[:], 0, 0, [[1, 128]])
        io(self.IDENT[:], 0, -1, [[1, 128]])
        self.TS("dve", self.IDENT[:], self.IDENT[:], 0.0, None, ALU.is_equal, None, [bC], [bC])
        io(self.TRI[:], 0, 1, [[-1, 128]])
        self.TS("dve", self.TRIC[:], self.TRI[:], 0.0, None, ALU.is_le, None, [bC], [bC])
        self.TS("dve", self.TRI[:], self.TRI[:], 0.0, None, ALU.is_gt, None, [bC], [bC])
        self.MS("pool", self.ZEROB[:], 0.0, [bC])
        self.MS("pool", self.ONESD[:], 1.0 / D, [bC])
        self.MS("pool", self.ONESH[:], 1.0 / 128, [bC])
        io(self.MASKH[:], 0, -1, [[1, 128]])
        self.TS("dve", self.MASKH[:], self.MASKH[:], 0.0, None, ALU.is_ge, None, [bC], [bC])
        self.MS("dve", self.MASKH[0:64, 64:128], 0.0, [bC])
        self.CP("dve", self.MASKU[:], self.MASKH[:], [bC], [bC])
        self.LD(self.LNG[:], self.lng, bC, w=[bC]); self.LD(self.LNB[:], self.lnb, bC, w=[bC])
        self.LD(self.HGG[:], self.hgg, bC, w=[bC])
        STI = [self.AA[:, i * 16384:(i + 1) * 16384].bitcast(F32) for i in range(2)]
        STO = [self.AA[:, 32768 + i * 8192:32768 + (i + 1) * 8192].bitcast(BF16) for i in range(2)]
        bSTI = [self.B("sti0"), self.B("sti1")]
        bSTO = [self.B("sto0"), self.B("sto1")]
        jobs = []
        for l in range(nl):
            W = self.WSC[l]
            win = self.w_in[l].rearrange("(c p) j -> p c j", p=128)
            wua = self.w_up_a[l].rearrange("(h p) j -> p h j", p=128)
            wub = self.w_up_b[l].rearrange("(h p) j -> p h j", p=128)
            wo = self.w_o[l].rearrange("(c p) j -> p c j", p=128)
            wpq = self.w_pq[l].rearrange("(c p) j -> p c j", p=128)

            def part(off, n_c, w, j0, src):
                return (lambda st, off=off, n_c=n_c, w=w, j0=j0: st[:, off:off + n_c * w].rearrange("p (c j) -> p c j", c=n_c)[:, :, j0:j0 + src.shape[2]], src)
            for h in range(4):
                jobs.append((W[:, OFF_HG + h * 4096:OFF_HG + (h + 1) * 4096], 4096,
                             [part(0, 8, 512, k * 128, win[:, :, k * 512 + h * 128:k * 512 + (h + 1) * 128]) for k in range(4)], self.bWSC))
            for hp in range(4):
                jobs.append((W[:, OFF_QK + hp * 2048:OFF_QK + (hp + 1) * 2048], 2048,
                             [part(0, 8, 256, k * 128, win[:, :, 2048 + k * 512 + hp * 128:2048 + k * 512 + (hp + 1) * 128]) for k in range(2)], self.bWSC))
            jobs.append((W[:, OFF_SV:OFF_SV + 4096], 4096, [part(0, 8, 512, 0, win[:, :, 3072:3584])], self.bWSC))
            for j in range(8):
                jobs.append((W[:, OFF_MG + j * 3072:OFF_MG + (j + 1) * 3072], 3072,
                             [part(0, 8, 128, 0, win[:, :, 3584 + j * 128:3584 + (j + 1) * 128]),
                              part(1024, 8, 128, 0, win[:, :, 4608 + j * 128:4608 + (j + 1) * 128]),
                              part(2048, 4, 128, 0, wua[:, :, j * 128:(j + 1) * 128]),
                              part(2560, 4, 128, 0, wub[:, :, j * 128:(j + 1) * 128])], self.bWSC))
            for off, wsrc in ((OFF_WO, wo), (OFF_PQ, wpq)):
                for jj in range(2):
                    jobs.append((W[:, off + jj * 4096:off + (jj + 1) * 4096], 4096,
                                 [part(q * 1024, 8, 128, 0, wsrc[:, :, (jj * 4 + q) * 128:(jj * 4 + q + 1) * 128]) for q in range(4)], self.bWSC))
            if self.dbg not in ("oa", "ob", "ln1", "nouv", "hm", "proj", "ew", "blk"):
                for k in range(32):
                    sl = slice(k * 4096, (k + 1) * 4096)
                    jobs.append((self.UTS[l][:, sl], 4096, [(lambda st: st[:, 0:4096], self.uT[l][:, sl])], self.bUV))
                    jobs.append((self.VS[l][:, sl], 4096, [(lambda st: st[:, 0:4096], self.pv[l][:, sl])], self.bUV))

        def job_load(i):
            dst, n, parts, db = jobs[i]
            for vf, src in parts:
                self.LD(vf(STI[i % 2]), src, bSTI[i % 2], w=[bSTI[i % 2]])

        def job_rest(i):
            dst, n, parts, db = jobs[i]
            eng = ("act", "dve", "pool")[i % 3]
            self.CP(eng, STO[i % 2][:, 0:n], STI[i % 2][:, 0:n], [bSTI[i % 2]], [bSTO[i % 2]])
            self.LD(dst, STO[i % 2][:, 0:n], bSTO[i % 2], r=[bSTO[i % 2]], w=[db])
        if jobs:
            job_load(0)
            for i in range(len(jobs)):
                if i + 1 < len(jobs):
                    job_load(i + 1)
                job_rest(i)
        LBL = self.cv(self.AB, 0, [128, nl, 4], F32)
        LBE = self.cv(self.AB, 256, [128, nl, 4], F32)
        LBM = self.cv(self.AB, 512, [128, 4], F32)
        bT = self.B("lbtmp")
        self.LD(LBL, self.lbl, bT, w=[bT])
        S.op("dve", lambda e: e.tensor_reduce(out=LBM, in_=LBL.rearrange("p l h -> p h l"), axis=AX.X, op=ALU.max), [bT], [bT])
        self.TT("dve", LBE, LBL, LBM[:, None, :].broadcast_to([128, nl, 4]), ALU.subtract, [bT], [bT])
        self.ACT(LBE, LBE, AF.Exp, [bT], [bT])
        S.op("dve", lambda e: e.tensor_reduce(out=LBM, in_=LBE.rearrange("p l h -> p h l"), axis=AX.X, op=ALU.add), [bT], [bT])
        S.op("dve", lambda e: e.reciprocal(out=LBM, in_=LBM), [bT], [bT])
        self.TT("dve", LBE, LBE, LBM[:, None, :].broadcast_to([128, nl, 4]), ALU.mult, [bT], [bT])
        self.MS("dve", self.LBT[:, 0, :, 0], 0.0, [bC])
        for l in range(1, nl):
            self.TT("dve", self.LBT[:, l, :, 0], self.LBT[:, l - 1, :, 0], LBE[:, l, :], ALU.add, [bT, bC], [bC])
        self.TS("dve", self.LBT[:, :, :, 1], self.LBT[:, :, :, 0], -1.0, 1.0, ALU.mult, ALU.add, [bC], [bC])
        self.TS("dve", self.LBT[:, :, :, 2], self.LBT[:, :, :, 0], -1.0, None, ALU.add, None, [bC], [bC])
        COND = self.cv(self.AB, 1024, [128, 8, nseq], F32)
        BADA = self.cv(self.AB, 2048, [128, nl, 48], F32)
        bCo = self.B("cond")
        self.LD(COND, self.cT, bCo, w=[bCo])
        self.LD(BADA, self.bada, bCo, w=[bCo])
        self.ACT(COND, COND, AF.Silu, [bCo], [bCo])
        WA = [self.cv(self.AB, 8192 + i * 16384, [128, 8, 512], F32) for i in range(2)]
        bWA = [self.B("wa0"), self.B("wa1")]
        it = 0
        for l in range(nl):
            wa = self.w_ada[l].rearrange("(c p) j -> p c j", p=128)
            for g in range(12):
                s = it % 2
                self.LD(WA[s], wa[:, :, g * 512:(g + 1) * 512], bWA[s], w=[bWA[s]])
                bank = self.PSb(it % 4, 0, 4 * nseq)
                for jl in range(4):
                    self.chain(bank[:, jl * nseq:(jl + 1) * nseq],
                               [(WA[s][:, cc, jl * 128:(jl + 1) * 128], COND[:, cc, :]) for cc in range(8)],
                               [bWA[s], bCo], [self.bPS[it % 4]])
                self.TT("dve", self.MODT[:, :, l, g * 4:(g + 1) * 4], bank.rearrange("p (j b) -> p b j", b=nseq),
                        BADA[:, l, g * 4:(g + 1) * 4][:, None, :].broadcast_to([128, nseq, 4]), ALU.add,
                        [self.bPS[it % 4], bCo], [bC])
                it += 1
        for k in (1, 4):
            self.TS("dve", self.MODT[:, :, :, k * 8:(k + 1) * 8], self.MODT[:, :, :, k * 8:(k + 1) * 8], 1.0, None, ALU.add, None, [bC], [bC])
        for k in (2, 5):
            self.TS("dve", self.MODT[:, :, :, k * 8:(k + 1) * 8], self.MODT[:, :, :, k * 8:(k + 1) * 8], 1.0 / ALPHA, None, ALU.mult, None, [bC], [bC])

    def seq_body(self, b):
        self.b = b
        for cc in range(8):
            self.LD(self.Xt[:, cc, :], self.xT[b][:, cc, :], self.bX[0], w=self.bX)
        for l in range(self.nl):
            self.S.sync_all()
            self.layer_body(l)
        self.S.sync_all()
        for cc in range(8):
            self.LD(self.outT[b][:, cc, :], self.Xt[:, cc, :], self.bX[0], r=self.bX)
        if self.dbg:
            pass

    def layer_body(self, l):
        self.l = l
        b = self.b
        C = self.CUR
        bc = self.bCUR
        self.CP("pool", C[:, 0:48], self.MODT[:, b, l, :], [self.bCONST], [bc])
        self.CP("dve", C[:, 48:64], self.LNG[:, l].rearrange("p a b -> p (a b)"), [self.bCONST], [bc])
        self.CP("dve", C[:, 64:80], self.LNB[:, l].rearrange("p a b -> p (a b)"), [self.bCONST], [bc])
        self.CP("dve", C[:, 80:84], self.HGG[:, l, :], [self.bCONST], [bc])
        self.CP("dve", C[:, 84:96], self.LBT[:, l].rearrange("p a b -> p (a b)"), [self.bCONST], [bc])
        self.phase1()
        if self.stop_after == "p1":
            return
        self.S.sync_all()
        self.phase2()

    def mod(self, k, cc):
        return self.CUR[:, k * 8 + cc:k * 8 + cc + 1]

    def layer_norm(self, which, base):
        SQ = self.cv(self.AB, base, [128, 8, 512], F32)
        MEAN = self.cv(self.AB, base + 16384, [128, 512], F32)
        M2 = self.cv(self.AB, base + 18432, [128, 512], F32)
        RSTD = self.cv(self.AB, base + 20480, [128, 512], F32)
        bSQ, bM, bM2, bR = self.B(), self.B(), self.B(), self.B()
        for tt in range(4):
            bx = self.bX[tt]
            cs = slice(tt * 512, (tt + 1) * 512)
            Xv = self.Xt[:, :, cs]
            self.ACT(SQ, Xv, AF.Square, [bx], [bSQ])
            pm, pq = (0, 1) if tt % 2 == 0 else (2, 3)
            self.chain(self.PSb(pm), [(self.ONESD[:], self.Xt[:, cc, cs]) for cc in range(8)], [bx, self.bCONST], [self.bPS[pm]])
            self.chain(self.PSb(pq), [(self.ONESD[:], SQ[:, cc, :]) for cc in range(8)], [bSQ, self.bCONST], [self.bPS[pq]])
            self.CP("act", MEAN, self.PSb(pm), [self.bPS[pm]], [bM])
            self.TT("dve", M2, MEAN, MEAN, ALU.mult, [bM], [bM2])
            self.TT("dve", M2, self.PSb(pq), M2, ALU.subtract, [self.bPS[pq], bM2], [bM2])
            self.ACT(M2, M2, AF.Ln, [bM2], [bM2], bias=LN_EPS / (ALPHA * ALPHA))
            self.ACT(RSTD, M2, AF.Exp, [bM2], [bR], scale=-0.5)
            self.TT("dve", Xv, Xv, MEAN[:, None, :].broadcast_to([128, 8, 512]), ALU.subtract, [bx, bM], [bx])
            self.TT("pool", Xv, Xv, RSTD[:, None, :].broadcast_to([128, 8, 512]), ALU.mult, [bx, bR], [bx])
            for cc in range(8):
                g = self.CUR[:, 48 + which * 8 + cc:48 + which * 8 + cc + 1]
                bb = self.CUR[:, 64 + which * 8 + cc:64 + which * 8 + cc + 1]
                self.TS("dve" if cc % 2 == 0 else "pool", self.Xt[:, cc, cs], self.Xt[:, cc, cs], g, bb, ALU.mult, ALU.add, [bx, self.bCUR], [bx])

    def phase1(self):
        nc, S = self.nc, self.S
        l = self.l
        AA, AB = self.AA, self.AB
        HM = self.cv(AA, 0, [128, 8, S_LEN], BF16); bHM = self.B("hm")
        OA = self.cv(AA, 32768, [128, 4, S_LEN], BF16); bOA = self.B("oa")
        OB = self.cv(AA, 49152, [128, 4, S_LEN], BF16); bOB = self.B("ob")
        WS = [AB[:, 0:8192].bitcast(BF16), AB[:, 8192:16384].bitcast(BF16)]
        bWS = [self.B("ws0"), self.B("ws1")]
        self.ws_i = 0
        W = self.WSC[l]
        bPS = self.bPS

        def load_ws(off, n):
            s = self.ws_i % 2
            self.ws_i += 1
            o = 0
            for piece in (4096, 2048, 1024):
                while n - o >= piece:
                    self.LD(WS[s][:, o:o + piece], W[:, off + o:off + o + piece], bWS[s], r=[self.bWSC], w=[bWS[s]])
                    o += piece
            assert o == n
            return WS[s], bWS[s]

        for cc in range(8):
            if cc % 2 == 0:
                self.ACT(HM[:, cc, :], self.Xt[:, cc, :], AF.Identity, self.bX + [self.bCUR], [bHM], bias=self.mod(0, cc), scale=self.mod(1, cc))
            else:
                self.TS("dve", HM[:, cc, :], self.Xt[:, cc, :], self.mod(1, cc), self.mod(0, cc), ALU.mult, ALU.add, self.bX + [self.bCUR], [bHM])

        if self.dbg == "hm":
            self.dump_bf(HM, [bHM], 8)
            return
        SC = 16384
        P = [self.cv(AB, SC + i * 8192, [128, S_LEN], F32) for i in range(4)]
        bP = [self.B("P%d" % i) for i in range(4)]
        VH = self.cv(AB, SC + 32768, [128, 16, 128], F32); bVH = self.B("vh")
        RM = self.cv(AB, SC + 40960, [128, S_LEN], F32); bRM = self.B("rm")
        MI = SC + 49152
        SCM = [self.cv(AB, MI + i * 512, [128, 128], F32) for i in range(2)]; bSCM = [self.B(), self.B()]
        KDT = [self.cv(AB, MI + 1024 + i * 512, [128, 128], F32) for i in range(2)]; bKDT = [self.B(), self.B()]
        ST = [self.cv(AB, MI + 2048 + i * 512, [128, 128], F32) for i in range(2)]; bST = [self.B(), self.B()]
        EBE = self.cv(AB, MI + 3072, [128, 32], F32); bEBE = self.B()
        BMD = self.cv(AB, MI + 3200, [128, 32], F32)
        self.MS("pool", RM, 1.0, [bRM])
        self.MS("pool", RM.rearrange("p (a b) -> p a b", b=64)[:, :, 0:1], 0.0, [bRM])
        SCP = [self.PSb(i, 0, 128) for i in range(2)]; bSCP = [self.bPS[0], self.bPS[1]]
        KTP = [self.PSb(2 + i, 0, 128) for i in range(2)]; bKTP = [self.bPS[2], self.bPS[3]]
        STP = [self.PSb(6 + i, 0, 128) for i in range(2)]; bSTP = [self.bPS[6], self.bPS[7]]
        pr = [0]

        def proj_fm(ws, bws, col0, evac):
            for tt in range(4):
                bk = pr[0] % 4
                pr[0] += 1
                self.chain(self.PSb(bk), [(ws.rearrange("p (c j) -> p c j", c=8)[:, cc, col0:col0 + 128], HM[:, cc, tt * 512:(tt + 1) * 512]) for cc in range(8)],
                           [bws, bHM], [bPS[bk]])
                evac(tt, self.PSb(bk), bPS[bk])

        for h in range(4):
            ws, bws = load_ws(OFF_HG + h * 4096, 4096)
            ws3 = ws[:, 0:4096].rearrange("p (c j) -> p c j", c=8)
            lb = self.CUR[:, 84 + h * 3:84 + h * 3 + 1]
            omlb = self.CUR[:, 84 + h * 3 + 1:84 + h * 3 + 2]
            nomlb = self.CUR[:, 84 + h * 3 + 2:84 + h * 3 + 3]
            wsf = ws[:, 0:4096]
            proj_fm(wsf, bws, 0, lambda tt, ps, bps: self.CP("act", P[0][:, tt * 512:(tt + 1) * 512], ps, [bps], [bP[0]]))
            proj_fm(wsf, bws, 128, lambda tt, ps, bps: self.ACT(P[1][:, tt * 512:(tt + 1) * 512], ps, AF.Sigmoid, [bps], [bP[1]]))
            for g in range(4):
                bk = pr[0] % 4
                pr[0] += 1
                for j in range(4):
                    t = g * 4 + j
                    self.chain(self.PSb(bk, j * 128, (j + 1) * 128), [(HM[:, cc, t * 128:(t + 1) * 128], ws3[:, cc, 256:384]) for cc in range(8)],
                               [bws, bHM], [bPS[bk]])
                self.CP("dve", VH[:, g * 4:(g + 1) * 4, :], self.PSb(bk).rearrange("p (a b) -> p a b", a=4), [bPS[bk]], [bVH])
            if self.dbg == "proj":
                self.S.sync_all()
                for i in range(2):
                    self.LD(self.dbg_out[:, i, :], P[i], bP[i], r=[bP[i]])
                self.LD(self.dbg_out[:, 2, :], VH.rearrange("p a b -> p (a b)"), bVH, r=[bVH])
                return
            self.TS("dve", P[2], P[1], nomlb, omlb, ALU.mult, ALU.add, [bP[1], self.bCUR], [bP[2]])
            self.TS("dve", P[1], P[1], omlb, lb, ALU.mult, ALU.add, [bP[1], self.bCUR], [bP[1]])
            self.ACT(P[1], P[1], AF.Ln, [bP[1]], [bP[1]])
            S.op("dve", lambda e: e.tensor_tensor_scan(out=P[3], data0=RM, data1=P[1], initial=0.0, op0=ALU.mult, op1=ALU.add), [bRM, bP[1]], [bP[3]])
            P3v = P[3].rearrange("p (a b) -> p a b", b=64)
            self.CP("dve", BMD, P3v[:, :, 31], [bP[3]], [bEBE])
            self.TT("dve", P3v, P3v, BMD[:, :, None].broadcast_to([128, 32, 64]), ALU.subtract, [bP[3], bEBE], [bP[3]])
            self.TS("dve", P[3], P[3], -80.0, 80.0, ALU.max, ALU.min, [bP[3]], [bP[3]])
            self.TT("dve", EBE[:, 0:31], P3v[:, 0:31, 63], BMD[:, 1:32], ALU.add, [bP[3], bEBE], [bEBE])
            self.CP("dve", EBE[:, 31:32], P3v[:, 31:32, 63], [bP[3]], [bEBE])
            self.ACT(EBE, EBE, AF.Exp, [bEBE], [bEBE])
            self.ACT(P[1], P[3], AF.Exp, [bP[3]], [bP[1]])
            self.TT("pool", P[0], P[0], P[1], ALU.mult, [bP[0], bP[1]], [bP[0]])
            self.ACT(P[1], P[3], AF.Exp, [bP[3], bP[1]], [bP[1]], scale=-1.0)
            self.TT("pool", P[2], P[2], P[1], ALU.mult, [bP[2], bP[1]], [bP[2]])
            if self.dbg == "ew":
                self.S.sync_all()
                for i in range(4):
                    self.LD(self.dbg_out[:, i, :], P[i], bP[i], r=[bP[i]])
                return
            self.MS("dve", ST[0], 0.0, [bST[0]])
            cur = 0

            def pre(blk):
                import os as _os
                _pm = _os.environ.get("PRE_MODE", "")
                s = blk % 2
                cs = slice(blk * 128, (blk + 1) * 128)
                if "a" in _pm:
                    s = 0
                if "b" in _pm:
                    cs = slice(0, 128)
                sp_, sm_ = s, s
                if "c" in _pm:
                    sm_ = 0
                if "d" in _pm:
                    sp_ = 0
                self.chain(SCP[sp_], [(P[2][:, cs], P[0][:, cs])], [bP[2], bP[0]], [bSCP[sp_]])
                if "t" not in _os.environ.get("BLK_FL", "t"):
                    self.TT("dve", SCM[sm_], SCP[sp_], self.MASKH[:], ALU.mult, [bSCP[sp_], self.bCONST], [bSCM[sm_]])
                    return
                import os as _os
                if "t" in _os.environ.get("BLK_FL", "t"):
                    S.op("pe", lambda e: e.transpose(KTP[s], P[2][:, cs], self.IDENT[:]), [bP[2], self.bCONST], [bKTP[s]])
                    self.CP("act", KDT[s], KTP[s], [bKTP[s]], [bKDT[s]])
                self.MS("pool", SCM[s], 0.0, [bSCM[s]])
                S.op("dve", lambda e: e.copy_predicated(out=SCM[s], mask=self.MASKU[:], data=SCP[s]), [bSCP[s], self.bCONST, bSCM[s]], [bSCM[s]])
            pre(0)
            import os as _os
            _NB = int(_os.environ.get("BLK_N", "16")); _FL = _os.environ.get("BLK_FL", "osu")
            for blk in range(_NB):
                s = blk % 2
                if blk + 1 < 16:
                    pre(blk + 1)
                ob = 4 + (blk // 4) % 2
                for ch in range(2):
                    c0 = blk * 128 + ch * 64
                    oc = (blk % 4) * 128 + ch * 64
                    rows = slice(ch * 64, (ch + 1) * 64)
                    if "o" in _FL:
                        self.chain(self.PSb(ob, oc, oc + 64),
                               [(ST[cur], P[0][:, c0:c0 + 64]), (VH[:, blk, :], SCM[s][:, ch * 64:(ch + 1) * 64])],
                               [bST[cur], bP[0], bVH, bSCM[s]], [bPS[ob]])
                    if "s" not in _FL:
                        continue
                    self.chain(STP[cur], [(self.IDENT[:], ST[cur]), (KDT[s][rows, :], VH[rows, blk, :])],
                               [self.bCONST, bST[cur], bKDT[s], bVH], [bSTP[cur]])
                    eb = EBE[:, 2 * blk + ch:2 * blk + ch + 1]
                    if "u" not in _FL:
                        continue
                    if ch == 0:
                        self.ACT(ST[1 - cur], STP[cur], AF.Identity, [bSTP[cur], bEBE], [bST[1 - cur]], scale=eb)
                    else:
                        self.TS("dve", ST[1 - cur], STP[cur], eb, None, ALU.mult, None, [bSTP[cur], bEBE], [bST[1 - cur]])
                    cur = 1 - cur
                if blk % 4 == 3 and "o" in _FL:
                    tt = blk // 4
                    self.CP("act", P[3][:, tt * 512:(tt + 1) * 512], self.PSb(ob), [bPS[ob]], [bP[3]])
            if self.dbg == "blk":
                self.S.sync_all()
                self.LD(self.dbg_out[:, 0, :], P[3], bP[3], r=[bP[3]])
                return
            self.ACT(P[1], P[3], AF.Square, [bP[3]], [bP[1]])
            for tt in range(4):
                bk = pr[0] % 4
                pr[0] += 1
                cs = slice(tt * 512, (tt + 1) * 512)
                self.chain(self.PSb(bk), [(self.ONESH[:], P[1][:, cs])], [bP[1], self.bCONST], [bPS[bk]])
                self.ACT(P[2][:, cs], self.PSb(bk), AF.Ln, [bPS[bk]], [bP[2]], bias=RMS_EPS)
            self.ACT(P[2], P[2], AF.Exp, [bP[2]], [bP[2]], scale=-0.5)
            proj_fm(wsf, bws, 384, lambda tt, ps, bps: self.ACT(P[0][:, tt * 512:(tt + 1) * 512], ps, AF.Silu, [bps], [bP[0]]))
            self.STT("dve", P[1], P[3], self.CUR[:, 80 + h:81 + h], P[2], ALU.mult, ALU.mult, [bP[3], bP[2], self.bCUR], [bP[1]])
            self.TT("pool", OA[:, h, :], P[1], P[0], ALU.mult, [bP[1], bP[0]], [bOA])

        if self.dbg == "oa":
            self.dump_bf(OA, [bOA], 4)
            return
        QT2 = self.cv(AB, SC, [128, S_LEN], BF16); KT2 = self.cv(AB, SC + 4096, [128, S_LEN], BF16)
        VSB = self.cv(AB, SC + 8192, [128, 16, 512], BF16)
        bQK = bP[0]
        bVS = bP[1]
        bST2 = [bP[2], bP[3]]
        EE = [self.cv(AB, SC + 24576 + i * 2048, [128, 512], F32) for i in range(2)]
        SPp = [self.cv(AB, SC + 28672 + i * 2048, [128, 512], F32) for i in range(2)]
        LW = [self.cv(AB, SC + 32768 + i * 2048, [128, 512], F32) for i in range(2)]
        WT = [self.cv(AB, SC + 36864 + i * 1024, [128, 512], BF16) for i in range(2)]
        bEE = [bVH, bRM]; bSP = [bSCM[0], bSCM[1]]; bLW = [bKDT[0], bKDT[1]]; bWT = [bST[0], bST[1]]
        S.sync_all()
        ws, bws = load_ws(OFF_SV, 4096)
        ws3 = ws[:, 0:4096].rearrange("p (c j) -> p c j", c=8)
        for t in range(16):
            bk = 4 + t % 4
            self.chain(self.PSb(bk), [(HM[:, cc, t * 128:(t + 1) * 128], ws3[:, cc, :]) for cc in range(8)], [bws, bHM], [bPS[bk]])
            self.CP("act" if t % 2 == 0 else "dve", VSB[:, t, :], self.PSb(bk), [bPS[bk]], [bVS])
        step = [0]
        for hp in range(4):
            ws, bws = load_ws(OFF_QK + hp * 2048, 2048)
            wsf = ws[:, 0:2048]
            for k, dst in ((0, QT2), (1, KT2)):
                for tt in range(4):
                    bk = 4 + pr[0] % 2
                    pr[0] += 1
                    self.chain(self.PSb(bk), [(wsf.rearrange("p (c j) -> p c j", c=8)[:, cc, k * 128:(k + 1) * 128], HM[:, cc, tt * 512:(tt + 1) * 512]) for cc in range(8)],
                               [bws, bHM], [bPS[bk]])
                    self.CP("act" if tt % 2 == 0 else "dve", dst[:, tt * 512:(tt + 1) * 512], self.PSb(bk), [bPS[bk]], [bQK])
            for ct in range(4):
                Ah = [self.PSb(2), self.PSb(3)]; bAh = [bPS[2], bPS[3]]
                Oh = [self.PS[0:64, 6 * 512:7 * 512], self.PS[0:64, 7 * 512:8 * 512]]; bOh = [bPS[6], bPS[7]]
                for hh in range(2):
                    self.chain(Ah[hh], [(self.ZEROB[:], HM[:, 0, 0:512])], [self.bCONST, bHM], [bAh[hh]])
                    self.chain(Oh[hh], [(self.ZEROB[:, 0:64], HM[:, 0, 0:512])], [self.bCONST, bHM], [bOh[hh]])
                for kb in range(4 * ct + 3, -1, -1):
                    first = max(kb, 4 * ct)
                    c0, c1 = first * 128, (4 * ct + 4) * 128
                    w = c1 - c0
                    lo = c0 - ct * 512
                    diag = kb >= 4 * ct

                    def mk(hh):
                        h = 2 * hp + hh
                        rows = slice(hh * 64, (hh + 1) * 64)
                        sl = hh
                        A, bA, O, bO = Ah[hh], bAh[hh], Oh[hh], bOh[hh]
                        Z = self.PSb(sl, 0, w); bZ = bPS[sl]

                        def acc(dst, lhsT, rhs_t, r, wb):
                            S.op("pe", lambda e: e.matmul(dst[:, lo:lo + w], lhsT=lhsT, rhs=rhs_t[:, 0:w], start=False, stop=True), r, wb)

                        def st0():
                            self.chain(Z, [(KT2[rows, kb * 128:(kb + 1) * 128], QT2[rows, c0:c1])], [bQK], [bZ])

                        def st1():
                            self.ACT(EE[sl][:, 0:w], Z, AF.Exp, [bZ], [bEE[sl]], scale=0.125)

                        def st2():
                            self.ACT(SPp[sl][:, 0:w], EE[sl][:, 0:w], AF.Ln, [bEE[sl]], [bSP[sl]], bias=1.0)

                        def st3():
                            if diag:
                                S.op("pool", lambda e: e.affine_select(out=SPp[sl][:, 0:128], in_=SPp[sl][:, 0:128], pattern=[[1, 128]],
                                                                       compare_op=ALU.is_gt, fill=0.0, base=0, channel_multiplier=-1), [bSP[sl]], [bSP[sl]])

                        def st4():
                            acc(A, self.TRI[:], SPp[sl], [bSP[sl], self.bCONST], [bA])

                        def st5():
                            self.STT("dve", LW[sl][:, 0:w], Z, 0.125, SPp[sl][:, 0:w], ALU.mult, ALU.subtract, [bZ, bSP[sl]], [bLW[sl]])

                        def st6():
                            self.TT("dve", LW[sl][:, 0:w], LW[sl][:, 0:w], A[:, lo:lo + w], ALU.subtract, [bLW[sl], bA], [bLW[sl]])

                        def st7():
                            self.ACT(WT[sl][:, 0:w], LW[sl][:, 0:w], AF.Exp, [bLW[sl]], [bWT[sl]])

                        def st8():
                            if diag:
                                S.op("pool", lambda e: e.affine_select(out=WT[sl][:, 0:128], in_=WT[sl][:, 0:128], pattern=[[1, 128]],
                                                                       compare_op=ALU.is_gt, fill=0.0, base=0, channel_multiplier=-1), [bWT[sl]], [bWT[sl]])

                        def st9():
                            acc(O, VSB[:, kb, h * 64:(h + 1) * 64], WT[sl], [bVS, bWT[sl]], [bO])
                            if kb > 0:
                                acc(A, self.TRIC[:], SPp[sl], [bSP[sl], self.bCONST], [bA])
                        return [st0, st1, st2, st3, st4, st5, st6, st7, st8, st9]
                    stg = [mk(0), mk(1)]
                    for si in range(10):
                        for hh in range(2):
                            stg[hh][si]()
                for hh in range(2):
                    self.CP("act", OB[hh * 64:(hh + 1) * 64, hp, ct * 512:(ct + 1) * 512], Oh[hh], [bOh[hh]], [bOB])
        if self.dbg == "ob":
            self.dump_bf(OB, [bOB], 4)
            return
        S.sync_all()
        MT = self.cv(AB, SC, [128, 8, S_LEN], BF16); bMT = self.B("mt")
        SG = [self.cv(AB, SC + 32768 + i * 2048, [128, 512], F32) for i in range(4)]
        bSG = [self.B() for _ in range(4)]
        it = 0
        for j in range(8):
            ws, bws = load_ws(OFF_MG + j * 3072, 3072)
            ga = ws[:, 0:1024].rearrange("p (c j) -> p c j", c=8)
            gb = ws[:, 1024:2048].rearrange("p (c j) -> p c j", c=8)
            ua = ws[:, 2048:2560].rearrange("p (c j) -> p c j", c=4)
            ub = ws[:, 2560:3072].rearrange("p (c j) -> p c j", c=4)
            for tt in range(4):
                cs = slice(tt * 512, (tt + 1) * 512)
                pb = (it % 2) * 4
                it += 1
                self.chain(self.PSb(pb), [(ga[:, cc, :], HM[:, cc, cs]) for cc in range(8)], [bws, bHM], [bPS[pb]])
                self.chain(self.PSb(pb + 1), [(ua[:, hh, :], OA[:, hh, cs]) for hh in range(4)], [bws, bOA], [bPS[pb + 1]])
                self.chain(self.PSb(pb + 2), [(gb[:, cc, :], HM[:, cc, cs]) for cc in range(8)], [bws, bHM], [bPS[pb + 2]])
                self.chain(self.PSb(pb + 3), [(ub[:, hh, :], OB[:, hh, cs]) for hh in range(4)], [bws, bOB], [bPS[pb + 3]])
                self.ACT(SG[0], self.PSb(pb), AF.Sigmoid, [bPS[pb]], [bSG[0]])
                self.ACT(SG[1], self.PSb(pb + 2), AF.Sigmoid, [bPS[pb + 2]], [bSG[1]])
                self.TT("dve", SG[2], SG[0], self.PSb(pb + 1), ALU.mult, [bSG[0], bPS[pb + 1]], [bSG[2]])
                self.TT("dve", SG[3], SG[1], self.PSb(pb + 3), ALU.mult, [bSG[1], bPS[pb + 3]], [bSG[3]])
                self.TT("pool", MT[:, j, cs], SG[2], SG[3], ALU.add, [bSG[2], bSG[3]], [bMT])
        it = 0
        for jo in range(8):
            ws, bws = load_ws(OFF_WO + jo * 1024, 1024)
            wo = ws[:, 0:1024].rearrange("p (c j) -> p c j", c=8)
            for tt in range(4):
                cs = slice(tt * 512, (tt + 1) * 512)
                pb = it % 4
                it += 1
                self.chain(self.PSb(pb), [(wo[:, jj, :], MT[:, jj, cs]) for jj in range(8)], [bws, bMT], [bPS[pb]])
                self.STT("dve", self.Xt[:, jo, cs], self.PSb(pb), self.mod(2, jo), self.Xt[:, jo, cs], ALU.mult, ALU.add,
                         [bPS[pb], self.bCUR, self.bX[tt]], [self.bX[tt]])
        S.sync_all()
        self.layer_norm(0, SC)
        if self.dbg == "ln1":
            self.dump_x()

    def dump_x(self):
        for cc in range(8):
            self.LD(self.dbg_out[:, cc, :], self.Xt[:, cc, :], self.bX[0], r=self.bX)

    def dump_bf(self, src, bufs, n):
        T = self.cv(self.AB, 16384, [128, S_LEN], F32)
        bT = self.B()
        self.S.sync_all()
        for i in range(n):
            self.CP("dve", T, src[:, i, :], bufs, [bT])
            self.LD(self.dbg_out[:, i, :], T, bT, r=[bT])

    def phase2(self):
        nc, S = self.nc, self.S
        l = self.l
        AB = self.AB
        W = self.WSC[l]
        self.G = self.cv(self.AA, 0, [128, 256, 128], BF16); self.bG = self.B("G")
        self.UT = [self.cv(AB, i * 8192, [128, 4, 8, 128], BF16) for i in range(2)]
        self.VV = [self.cv(AB, 16384 + i * 8192, [128, 4, 1024], BF16) for i in range(2)]
        self.bUT = [self.B("ut0"), self.B("ut1")]
        self.bVV = [self.B("v0"), self.B("v1")]
        self.HFT = self.cv(AB, 32768, [128, 8, 256], BF16); self.bHFT = self.B("hft")
        self.QT = self.cv(AB, 36864, [128, 8, 256], BF16); self.bQT = self.B("qt")
        self.WQ = self.cv(AB, 16384, [128, 8, 8, 128], BF16)
        self.KBD = self.cv(AB, 40960, [128, 8, 256], BF16); self.bKBD = self.B("kbd")
        KBF = self.cv(AB, 0, [128, 8, 256], F32)
        bKF = self.bUT[0]
        self.MS("pool", KBF, 0.0, [bKF])
        self.LD(KBF[0:64, :, 0:128], self.skT[l, 0].rearrange("h d n -> d h n"), bKF, w=[bKF])
        self.LD(KBF[64:128, :, 128:256], self.skT[l, 1].rearrange("h d n -> d h n"), bKF, w=[bKF])
        self.CP("dve", self.KBD, KBF, [bKF], [self.bKBD])
        S.sync_all()
        self.run_loop(self.ntiles, self.peer_tile)
        if self.dbg == "peer":
            self.dump_x()
            return
        self.layer_norm(1, 0)
        if self.dbg == "ln2":
            self.dump_x()

    def peer_tile(self, tq):
        nc, S = self.nc, self.S
        l = self.l
        AB = self.AB
        bPS = self.bPS
        bXa = self.bX
        tsl = bass.ts(tq, 256)
        HFT, QT, WQ, KBD, G = self.HFT, self.QT, self.WQ, self.KBD, self.G
        XC = self.cv(AB, 8192, [128, 8, 256], F32)
        self.CP("pool", XC, self.Xt[:, :, tsl], bXa, [self.bUT[1]])
        for cc in range(8):
            if cc % 2 == 0:
                self.ACT(HFT[:, cc, :], XC[:, cc, :], AF.Identity, [self.bUT[1], self.bCUR], [self.bHFT], bias=self.mod(3, cc), scale=self.mod(4, cc))
            else:
                self.TS("dve", HFT[:, cc, :], XC[:, cc, :], self.mod(4, cc), self.mod(3, cc), ALU.mult, ALU.add, [self.bUT[1], self.bCUR], [self.bHFT])
        self.LD(WQ.rearrange("p a b c -> p (a b c)"), self.WSC[l][:, OFF_PQ:OFF_PQ + 8192], self.bVV[0], r=[self.bWSC], w=self.bVV)
        for h in range(8):
            bk = 4 + h % 4
            self.chain(self.PSb(bk, 0, 256), [(WQ[:, h, cc, :], HFT[:, cc, :]) for cc in range(8)], self.bVV + [self.bHFT], [bPS[bk]])
            self.CP("act" if h % 2 == 0 else "dve", QT[:, h, :], self.PSb(bk, 0, 256), [bPS[bk]], [self.bQT])
        SS = self.cv(AB, 0, [128, 2048], F32); bSS = self.bUT[0]
        EQ = self.cv(AB, 8192, [128, 2048], F32); bEQ = self.bUT[1]
        T0 = 45056
        TOPV = self.cv(AB, T0, [128, 16, 16], F32)
        TOPI = self.cv(AB, T0 + 1024, [128, 16, 16], U32)
        TOPIF = self.cv(AB, T0 + 2048, [128, 16, 16], F32)
        SR = self.cv(AB, T0 + 3072, [128, 256], F32)
        CTOP = self.cv(AB, T0 + 4096, [128, 8, 16], F32)
        CPOS = self.cv(AB, T0 + 4608, [128, 128], U32)
        CAI = self.cv(AB, T0 + 5120, [128, 128], I32)
        CBI = self.cv(AB, T0 + 5632, [128, 128], I32)
        CAF = self.cv(AB, T0 + 6144, [128, 128], F32)
        CBF = self.cv(AB, T0 + 6656, [128, 128], F32)
        IDX = [self.cv(AB, T0 + 7168 + i * 512, [128, 128], F32) for i in range(3)]
        GSUM = self.cv(AB, T0 + 8704, [128, 8], F32)
        IDT = [self.cv(AB, T0 + 9216 + i * 1024, [128, 256], F32) for i in range(3)]
        OH0 = T0 + 12288
        EQ1 = [self.cv(AB, OH0 + i * 6144, [128, 8, 128], BF16) for i in range(2)]
        OH1 = [self.cv(AB, OH0 + 2048 + i * 6144, [128, 8, 128], BF16) for i in range(2)]
        OH2 = [self.cv(AB, OH0 + 4096 + i * 6144, [128, 8, 128], BF16) for i in range(2)]
        AG0 = OH0 + 6144
        AG = [self.cv(AB, AG0 + i * 1024, [128, 256], F32) for i in range(2)]
        WW = [self.cv(AB, AG0 + 2048 + i * 512, [128, 256], BF16) for i in range(2)]
        bTK = self.B("topk"); bSR = self.B("sr"); bIDX = self.B("idx"); bIDT = self.B("idt")
        bE1, bO1, bO2 = [self.B(), self.B()], [self.B(), self.B()], [self.B(), self.B()]
        bAG = [self.B(), self.B()]; bWW = [self.B(), self.B()]
        dv = lambda fn, r, w: S.op("dve", fn, r, w)
        for st in range(2):
            tcs = slice(st * 128, (st + 1) * 128)
            for h in range(8):
                bk = 4 + h // 2
                self.chain(self.PSb(bk, (h % 2) * 256, (h % 2) * 256 + 256), [(QT[:, h, tcs], KBD[:, h, :])], [self.bQT, self.bKBD], [bPS[bk]])
            for k in range(4):
                self.CP("act" if k % 2 == 0 else "dve", SS[:, k * 512:(k + 1) * 512], self.PSb(4 + k), [bPS[4 + k]], [bSS])
            SR16 = EQ.rearrange("p (g n) -> p g n", g=16)
            gb = [[self.B() for _ in range(16)] for _ in range(5)]
            vs = [SS[:, g * 128:(g + 1) * 128] for g in range(16)]
            for g in range(16):
                dv(lambda e, g=g: e.max(out=TOPV[:, g, 0:8], in_=vs[g]), [bSS, bTK], [gb[0][g]])
            for g in range(16):
                dv(lambda e, g=g: e.match_replace(out=SR16[:, g, :], in_to_replace=TOPV[:, g, 0:8], in_values=vs[g], imm_value=-1e30), [bSS, gb[0][g], bEQ], [gb[1][g]])
            for g in range(16):
                dv(lambda e, g=g: e.max(out=TOPV[:, g, 8:16], in_=SR16[:, g, :]), [gb[1][g]], [gb[2][g]])
            for g in range(16):
                dv(lambda e, g=g: e.max_index(out=TOPI[:, g, 0:8], in_max=TOPV[:, g, 0:8], in_values=vs[g]), [bSS, gb[0][g]], [gb[3][g]])
                dv(lambda e, g=g: e.max_index(out=TOPI[:, g, 8:16], in_max=TOPV[:, g, 8:16], in_values=vs[g]), [bSS, gb[2][g]], [gb[4][g]])
            allg = [x for row in gb for x in row]
            self.CP("dve", TOPIF, TOPI, allg, [bTK] + allg)
            self.CP("dve", TOPIF, TOPI, [bTK], [bTK])
            TV4 = TOPV.rearrange("p (h two) k -> p h two k", two=2)
            TI4 = TOPIF.rearrange("p (h two) k -> p h two k", two=2)
            self.TT("dve", SS.rearrange("p (h a b) -> p h a b", h=8, a=16),
                    TV4[:, :, 0, :, None].broadcast_to([128, 8, 16, 16]), TV4[:, :, 1, None, :].broadcast_to([128, 8, 16, 16]), ALU.add, [bTK], [bSS])
            SRC = EQ.rearrange("p (h n) -> p h n", h=8)
            hb = [[self.B() for _ in range(8)] for _ in range(5)]
            cvs = [SS[:, h * 256:(h + 1) * 256] for h in range(8)]
            for h in range(8):
                dv(lambda e, h=h: e.max(out=CTOP[:, h, 0:8], in_=cvs[h]), [bSS, bTK], [hb[0][h]])
            for h in range(8):
                dv(lambda e, h=h: e.match_replace(out=SRC[:, h, :], in_to_replace=CTOP[:, h, 0:8], in_values=cvs[h], imm_value=-1e30), [bSS, hb[0][h], bEQ], [hb[1][h]])
            for h in range(8):
                dv(lambda e, h=h: e.max(out=CTOP[:, h, 8:16], in_=SRC[:, h, :]), [hb[1][h]], [hb[2][h]])
            for h in range(8):
                dv(lambda e, h=h: e.max_index(out=CPOS[:, h * 16:h * 16 + 8], in_max=CTOP[:, h, 0:8], in_values=cvs[h]), [bSS, hb[0][h]], [hb[3][h]])
                dv(lambda e, h=h: e.max_index(out=CPOS[:, h * 16 + 8:h * 16 + 16], in_max=CTOP[:, h, 8:16], in_values=cvs[h]), [bSS, hb[2][h]], [hb[4][h]])
            allh = [x for row in hb for x in row]
            S.op("dve", lambda e: e.tensor_single_scalar(out=CAI, in_=CPOS.bitcast(I32), scalar=4, op=ALU.arith_shift_right), allh, [bTK, bEQ] + allh)
            dv(lambda e: e.tensor_single_scalar(out=CBI, in_=CPOS.bitcast(I32), scalar=15, op=ALU.bitwise_and), [bTK], [bTK])
            self.CP("dve", CAF, CAI, [bTK], [bTK])
            self.CP("dve", CBF, CBI, [bTK], [bTK])
            for half, src in ((0, CAF), (1, CBF)):
                self.TT("dve", EQ.rearrange("p (k a) -> p k a", a=16), src[:, :, None].broadcast_to([128, 128, 16]),
                        self.IOTA[:, None, 0:16].broadcast_to([128, 128, 16]), ALU.is_equal, [bTK, self.bCONST], [bEQ])
                self.TT("dve", EQ.rearrange("p (h k a) -> p h k a", h=8, k=16), EQ.rearrange("p (h k a) -> p h k a", h=8, k=16),
                        TI4[:, :, half, None, :].broadcast_to([128, 8, 16, 16]), ALU.mult, [bEQ, bTK], [bEQ])
                dv(lambda e, half=half: e.tensor_reduce(out=IDX[half], in_=EQ.rearrange("p (k a) -> p k a", a=16), axis=AX.X, op=ALU.add), [bEQ], [bIDX])
            G3 = IDX[2].rearrange("p (h k) -> p h k", k=16)
            self.TT("dve", G3, CTOP, CTOP[:, :, 0:1].broadcast_to([128, 8, 16]), ALU.subtract, [bTK], [bIDX])
            self.ACT(IDX[2], IDX[2], AF.Exp, [bIDX], [bIDX])
            dv(lambda e: e.tensor_reduce(out=GSUM, in_=G3, axis=AX.X, op=ALU.add), [bIDX], [bTK])
            dv(lambda e: e.reciprocal(out=GSUM, in_=GSUM), [bTK], [bTK])
            self.TT("dve", G3, G3, GSUM[:, :, None].broadcast_to([128, 8, 16]), ALU.mult, [bIDX, bTK], [bIDX])
            for i in range(3):
                bk = 4 + i
                S.op("pe", lambda e, i=i, bk=bk: e.transpose(self.PSb(bk, 0, 128), IDX[i], self.IDENT[:]), [bIDX, self.bCONST], [bPS[bk]])
                self.CP("act" if i % 2 == 0 else "dve", IDT[i][:, tcs], self.PSb(bk, 0, 128), [bPS[bk]], [bIDT])
        for tb in range(32):
            t0 = tb * 8
            k = tb % 2
            pb = 4 + k * 2
            iob = self.IOTA[:, None, :].broadcast_to([128, 8, 128])
            self.TT("dve", EQ1[k], iob, IDT[0][:, t0:t0 + 8, None].broadcast_to([128, 8, 128]), ALU.is_equal, [bIDT, self.bCONST], [bE1[k]])
            self.TT("pool", OH1[k], EQ1[k], IDT[2][:, t0:t0 + 8, None].broadcast_to([128, 8, 128]), ALU.mult, [bE1[k], bIDT], [bO1[k]])
            self.TT("dve", OH2[k], iob, IDT[1][:, t0:t0 + 8, None].broadcast_to([128, 8, 128]), ALU.is_equal, [bIDT, self.bCONST], [bO2[k]])

            def gmm(e, pb=pb, k=k):
                ins = None
                for j in range(8):
                    ins = e.matmul(self.PS[:, pb * 512 + j * 128:pb * 512 + (j + 1) * 128], lhsT=OH1[k][:, j, :], rhs=OH2[k][:, j, :], start=True, stop=True)
                return ins
            S.op("pe", gmm, [bO1[k], bO2[k]], [bPS[pb], bPS[pb + 1]])
            self.CP("act", G[:, t0:t0 + 4, :], self.PSb(pb).rearrange("p (a b) -> p a b", a=4), [bPS[pb]], [self.bG])
            self.CP("act", G[:, t0 + 4:t0 + 8, :], self.PSb(pb + 1).rearrange("p (a b) -> p a b", a=4), [bPS[pb + 1]], [self.bG])
        UTS, VS = self.UTS[l], self.VS[l]

        def load_u(s):
            sl = s % 2
            self.LD(self.UT[sl].rearrange("p a b c -> p (a b c)"), UTS[:, s * 4096:(s + 1) * 4096], self.bUT[sl], r=[self.bUV], w=[self.bUT[sl]])

        def load_v(s):
            sl = s % 2
            self.LD(self.VV[sl].rearrange("p a b -> p (a b)"), VS[:, s * 4096:(s + 1) * 4096], self.bVV[sl], r=[self.bUV], w=[self.bVV[sl]])

        def a_chain(i2):
            s, q = i2 // 4, i2 % 4
            bk = 4 + i2 % 4
            self.chain(self.PSb(bk, 0, 256), [(self.UT[s % 2][:, q, cc, :], HFT[:, cc, :]) for cc in range(8)], [self.bUT[s % 2], self.bHFT], [bPS[bk]])
            if q == 3 and s + 2 < 32:
                load_u(s + 2)
        for bk in range(4):
            self.chain(self.PSb(bk), [(self.ZEROB[:], G[:, 0:4, :].rearrange("p a b -> p (a b)"))], [self.bCONST, self.bG], [bPS[bk]])
        load_u(0); load_v(0); load_u(1); load_v(1)
        a_chain(0)
        a_chain(1)
        for i2 in range(128):
            s, q = i2 // 4, i2 % 4
            if i2 + 2 < 128:
                a_chain(i2 + 2)
            bk = 4 + i2 % 4
            a = i2 % 2
            self.ACT(AG[a], self.PSb(bk, 0, 256), AF.Gelu, [bPS[bk]], [bAG[a]])
            self.TT("dve", WW[a], AG[a], G[:, :, i2], ALU.mult, [bAG[a], self.bG], [bWW[a]])

            def ymm(e, i2=i2, s=s, q=q, a=a):
                ins = None
                for th in range(2):
                    for dh in range(2):
                        ins = e.matmul(self.PSb(th * 2 + dh), lhsT=WW[a][:, th * 128:(th + 1) * 128], rhs=self.VV[s % 2][:, q, dh * 512:(dh + 1) * 512],
                                       start=False, stop=True)
                return ins
            S.op("pe", ymm, [self.bVV[s % 2], bWW[a]], bPS[0:4])
            if q == 3 and s + 2 < 32:
                load_v(s + 2)
        YT = self.cv(AB, 0, [128, 4, 512], F32)
        XO = self.cv(AB, 8192, [128, 8, 256], F32)
        for k in range(4):
            self.CP("act" if k % 2 == 0 else "dve", YT[:, k, :], self.PSb(k), [bPS[k]], [self.bUT[0]])
        for dc in range(8):
            bk = 4 + dc // 2
            for th in range(2):
                S.op("pe", lambda e, dc=dc, th=th, bk=bk: e.transpose(self.PSb(bk, (dc % 2) * 256 + th * 128, (dc % 2) * 256 + th * 128 + 128),
                                                                   YT[:, th * 2 + dc // 4, (dc % 4) * 128:(dc % 4 + 1) * 128], self.IDENT[:]),
                     [self.bUT[0], self.bCONST], [bPS[bk]])
        for dc in range(8):
            bk = 4 + dc // 2
            src = self.PSb(bk, (dc % 2) * 256, (dc % 2) * 256 + 256)
            if dc % 2 == 0:
                self.TS("dve", XO[:, dc, :], src, self.mod(5, dc), None, ALU.mult, None, [bPS[bk], self.bCUR], [self.bUT[1]])
            else:
                self.ACT(XO[:, dc, :], src, AF.Identity, [bPS[bk], self.bCUR], [self.bUT[1]], scale=self.mod(5, dc))
        self.TT("dve", self.Xt[:, :, tsl], self.Xt[:, :, tsl], XO, ALU.add, [self.bUT[1]] + bXa, bXa)


_CACHE = {}


def _prep_weights(w_ada, b_ada, w_in, lb_logits, hg_norm_g, w_up_a, w_up_b, w_o, w_pq, sub_keys, peer_u, peer_v, ln_g, ln_b):
    nl = w_ada.shape[0]
    f = lambda a: np.ascontiguousarray(np.asarray(a, dtype=np.float32))
    d = {}
    d["w_ada"] = f(w_ada); d["w_in"] = f(w_in); d["w_up_a"] = f(w_up_a); d["w_up_b"] = f(w_up_b)
    d["w_o"] = f(w_o); d["w_pq"] = f(w_pq)
    d["bada"] = f(np.asarray(b_ada).reshape(nl, 48, 128).transpose(2, 0, 1))
    d["lbl"] = f(np.asarray(lb_logits).reshape(nl, 4, 128).transpose(2, 0, 1))
    d["hgg"] = f(np.asarray(hg_norm_g).reshape(nl, 4, 128).transpose(2, 0, 1))
    d["lng"] = f(np.asarray(ln_g).reshape(nl, 2, 8, 128).transpose(3, 0, 1, 2))
    d["lnb"] = f(np.asarray(ln_b).reshape(nl, 2, 8, 128).transpose(3, 0, 1, 2))
    d["skT"] = f(np.asarray(sub_keys).transpose(0, 1, 2, 4, 3))
    d["uT"] = f(np.asarray(peer_u).reshape(nl, 128, 128, 8, 128).transpose(0, 4, 2, 3, 1)).reshape(nl, 128, 128 * 1024)
    d["pv"] = f(peer_v).reshape(nl, 128, 128 * 1024)
    return d


def kernel(x, c, w_ada, b_ada, w_in, lb_logits, hg_norm_g, w_up_a, w_up_b, w_o, w_pq, sub_keys, peer_u, peer_v, ln_g, ln_b):
    x = np.asarray(x, dtype=np.float32)
    c = np.asarray(c, dtype=np.float32)
    B = x.shape[0]
    nseq = B // NCORES
    key = ("full", nseq)
    if key not in _CACHE:
        _CACHE[key] = MK(nseq=nseq, nl=DEPTH)
    mk = _CACHE[key]
    wd = _prep_weights(w_ada, b_ada, w_in, lb_logits, hg_norm_g, w_up_a, w_up_b, w_o, w_pq, sub_keys, peer_u, peer_v, ln_g, ln_b)
    in_maps = []
    for i in range(NCORES):
        xs = x[i * nseq:(i + 1) * nseq]
        xT = np.ascontiguousarray(xs.reshape(nseq, S_LEN, 8, 128).transpose(0, 3, 2, 1))
        cs = c[i * nseq:(i + 1) * nseq]
        cT = np.ascontiguousarray(cs.reshape(nseq, 8, 128).transpose(2, 1, 0))
        m = dict(wd)
        m["xT"] = xT
        m["cT"] = cT
        in_maps.append(m)
    res = run_bass_kernel_spmd(mk.nc, in_maps, core_ids=list(range(NCORES)))
    out = np.empty((B, S_LEN, D), dtype=np.float32)
    for i in range(NCORES):
        oT = np.asarray(res.results[i]["outT"])
        out[i * nseq:(i + 1) * nseq] = oT.transpose(0, 3, 2, 1).reshape(nseq, S_LEN, D)
    return out
```
